# Optimizing a Trainium2 kernel written in Bass

```python
import math
import jax
import jax.numpy as jnp
from jax import lax
import numpy as np

D_MODEL = 1024
BATCH = 4
SEQ = 8192
DEPTH = 1

D_MIX = D_MODEL
ML_HEADS = 4
ML_DH = D_MIX // (2 * ML_HEADS)
ML_W = ML_HEADS * ML_DH
ML_CHUNK = 128
CONV_K = 4
DA_HEADS = 4
DA_DH = D_MIX // (4 * DA_HEADS)
DA_W = DA_HEADS * 2 * DA_DH
Q_BLOCK = 128
N_EXPERTS = 256
TOP_K = 8
N_GROUPS = 8
TOPK_GROUPS = 4
D_EXPERT = D_MODEL // 4
D_SHARED = D_EXPERT
ROUTED_SCALE = 2.5
MOE_BLOCK = 128
EPS = 1e-6
COL_SPLITS = (2 * ML_W, 3 * ML_W, 4 * ML_W, 4 * ML_W + ML_HEADS, 4 * ML_W + 2 * ML_HEADS,
              4 * ML_W + 2 * ML_HEADS + DA_W, 4 * ML_W + 2 * ML_HEADS + 2 * DA_W)
D_IN = 4 * ML_W + 2 * ML_HEADS + 3 * DA_W

kernel_name = 'hybrid_mlstm_diffattn_moe_layer'


def rms_norm(x, eps=EPS):
    xf = x.astype(jnp.float32)
    return (xf * lax.rsqrt(jnp.mean(xf * xf, axis=-1, keepdims=True) + eps)).astype(x.dtype)


def lambda_init(layer):
    return 0.8 - 0.6 * math.exp(-0.3 * layer)


def alibi_slopes(n):
    return 2.0 ** (-8.0 * jnp.arange(1, n + 1, dtype=jnp.float32) / n)


def causal_depthwise_conv(u, w, b):
    S = u.shape[1]
    up = jnp.pad(u, ((0, 0), (CONV_K - 1, 0), (0, 0)))
    out = b
    for j in range(CONV_K):
        out = out + w[j] * up[:, j:j + S, :]
    return out


def mlstm_chunkwise(q, k, v, i_pre, f_pre):
    B, S, H, DH = q.shape
    L = ML_CHUNK
    NC = S // L

    def to_chunks(t):
        t = t.astype(jnp.float32).reshape((B, NC, L, H) + t.shape[3:])
        return jnp.moveaxis(t, (1, 3), (0, 2))

    qc = to_chunks(q)
    kc = to_chunks(k) * (DH ** -0.5)
    vc = to_chunks(v)
    ic = to_chunks(i_pre)
    lfc = jax.nn.log_sigmoid(to_chunks(f_pre))
    causal = jnp.tril(jnp.ones((L, L), dtype=bool))

    def step(carry, xs):
        C, n, m = carry
        qb, kb, vb, ib, lfb = xs
        b = jnp.cumsum(lfb, axis=-1)
        d_intra = jnp.where(causal, b[..., :, None] - b[..., None, :] + ib[..., None, :], -jnp.inf)
        d_inter = b + m[..., None]
        m_t = jnp.maximum(d_inter, jnp.max(d_intra, axis=-1))
        w_intra = jnp.exp(d_intra - m_t[..., None])
        w_inter = jnp.exp(d_inter - m_t)
        s = jnp.einsum('bhtd,bhsd->bhts', qb, kb) * w_intra
        num = (w_inter[..., None] * jnp.einsum('bhtd,bhde->bhte', qb, C)
               + jnp.einsum('bhts,bhse->bhte', s, vb))
        den = w_inter * jnp.einsum('bhtd,bhd->bht', qb, n) + jnp.sum(s, axis=-1)
        h = num / jnp.maximum(jnp.abs(den), jnp.exp(-m_t))[..., None]
        b_last = b[..., -1]
        d_state = b_last[..., None] - b + ib
        m_new = jnp.maximum(b_last + m, jnp.max(d_state, axis=-1))
        carry_scale = jnp.exp(b_last + m - m_new)
        wk = jnp.exp(d_state - m_new[..., None])
        C_new = carry_scale[..., None, None] * C + jnp.einsum('bhs,bhsd,bhse->bhde', wk, kb, vb)
        n_new = carry_scale[..., None] * n + jnp.einsum('bhs,bhsd->bhd', wk, kb)
        return (C_new, n_new, m_new), h

    init = (jnp.zeros((B, H, DH, DH), jnp.float32),
            jnp.zeros((B, H, DH), jnp.float32),
            jnp.zeros((B, H), jnp.float32))
    _, h = lax.scan(step, init, (qc, kc, vc, ic, lfc))
    return jnp.moveaxis(h, (0, 2), (1, 3)).reshape(B, S, H, DH)


def diff_attention(q, k, v, lam, slopes):
    B, S, H, _, d = q.shape
    nq = S // Q_BLOCK
    qh = jnp.transpose(q, (0, 2, 3, 1, 4))
    kh = jnp.transpose(k, (0, 2, 3, 1, 4))
    vh = jnp.transpose(v, (0, 2, 1, 3))
    q_blocks = jnp.moveaxis(qh.reshape(B, H, 2, nq, Q_BLOCK, d), 3, 0)
    kpos = jnp.arange(S, dtype=jnp.int32)
    scale = d ** -0.5

    def one_block(args):
        qb, qi = args
        qpos = qi * Q_BLOCK + jnp.arange(Q_BLOCK, dtype=jnp.int32)
        dist = (qpos[:, None] - kpos[None, :]).astype(jnp.float32)
        s = jnp.einsum('bhcqd,bhckd->bhcqk', qb, kh).astype(jnp.float32) * scale
        s = jnp.where(dist >= 0, s - slopes[None, :, None, None, None] * dist, -jnp.inf)
        p = jax.nn.softmax(s, axis=-1)
        a = p[:, :, 0] - lam * p[:, :, 1]
        return jnp.einsum('bhqk,bhke->bhqe', a.astype(vh.dtype), vh)

    o = lax.map(one_block, (q_blocks, jnp.arange(nq, dtype=jnp.int32)))
    return jnp.transpose(o, (1, 0, 3, 2, 4)).reshape(B, S, H, 2 * d)


def swiglu(h, w1, w3, w2):
    return (jax.nn.silu(h @ w1) * (h @ w3)) @ w2


def routed_moe(h, w_router, router_bias, w1, w3, w2, ws1, ws3, ws2):
    T, D = h.shape
    s = jax.nn.sigmoid((h @ w_router).astype(jnp.float32))
    sel = s + router_bias
    g = sel.reshape(T, N_GROUPS, N_EXPERTS // N_GROUPS)
    group_score = jnp.sum(lax.top_k(g, 2)[0], axis=-1)
    _, top_groups = lax.top_k(group_score, TOPK_GROUPS)
    group_mask = jnp.sum(jax.nn.one_hot(top_groups, N_GROUPS, dtype=jnp.float32), axis=1) > 0
    expert_mask = jnp.repeat(group_mask, N_EXPERTS // N_GROUPS, axis=-1)
    _, idx = lax.top_k(jnp.where(expert_mask, sel, -jnp.inf), TOP_K)
    gate = jnp.take_along_axis(s, idx, axis=-1)
    gate = gate / jnp.sum(gate, axis=-1, keepdims=True) * ROUTED_SCALE

    TK = T * TOP_K
    flat_e = idx.reshape(-1)
    flat_tok = jnp.repeat(jnp.arange(T, dtype=jnp.int32), TOP_K)
    flat_w = gate.reshape(-1).astype(h.dtype)
    order = jnp.argsort(flat_e)
    sorted_e = flat_e[order]
    counts = jnp.bincount(flat_e, length=N_EXPERTS)
    padded = (counts + MOE_BLOCK - 1) // MOE_BLOCK * MOE_BLOCK
    pad_end = jnp.cumsum(padded)
    pad_start = pad_end - padded
    start = jnp.cumsum(counts) - counts
    dest = pad_start[sorted_e] + jnp.arange(TK, dtype=jnp.int32) - start[sorted_e]
    n_pad = -(-TK // MOE_BLOCK) * MOE_BLOCK + N_EXPERTS * MOE_BLOCK
    n_blocks = n_pad // MOE_BLOCK
    slot_tok = jnp.full((n_pad,), T, jnp.int32).at[dest].set(flat_tok[order])
    slot_w = jnp.zeros((n_pad,), h.dtype).at[dest].set(flat_w[order])
    block_e = jnp.minimum(
        jnp.searchsorted(pad_end, jnp.arange(n_blocks, dtype=jnp.int32) * MOE_BLOCK, side='right'),
        N_EXPERTS - 1)
    h_pad = jnp.concatenate([h, jnp.zeros((1, D), h.dtype)], axis=0)

    def expert_block(args):
        tok, wt, e = args
        xb = h_pad[tok]
        return swiglu(xb, w1[e], w3[e], w2[e]) * wt[:, None]

    out = lax.map(expert_block, (slot_tok.reshape(n_blocks, MOE_BLOCK),
                                 slot_w.reshape(n_blocks, MOE_BLOCK), block_e))
    routed = jnp.zeros((T + 1, D), h.dtype).at[slot_tok].add(out.reshape(n_pad, D))[:T]
    return routed + swiglu(h, ws1, ws3, ws2)


def setup_inputs(seed: int = 0) -> dict:
    key = jax.random.key(seed)
    ks = jax.random.split(key, 25)
    f32 = jnp.float32
    L = DEPTH

    def nrm(k, shape, scale):
        return jax.random.normal(k, shape, f32) * scale

    f_bias = jnp.linspace(3.0, 6.0, ML_HEADS, dtype=f32)
    gate_b = jnp.concatenate([nrm(ks[7], (L, ML_HEADS), 0.1),
                              f_bias[None, :] + nrm(ks[8], (L, ML_HEADS), 0.1)], axis=-1)
    return {
        'x': nrm(ks[0], (BATCH, SEQ, D_MODEL), 1.0),
        'c': nrm(ks[1], (BATCH, D_MODEL), 1.0),
        'w_ada': nrm(ks[2], (L, D_MODEL, 6 * D_MODEL), 0.5 * D_MODEL ** -0.5),
        'b_ada': nrm(ks[3], (L, 6 * D_MODEL), 0.02),
        'w_in': nrm(ks[4], (L, D_MODEL, D_IN), D_MODEL ** -0.5),
        'conv_w': nrm(ks[5], (L, CONV_K, 2 * ML_W), CONV_K ** -0.5),
        'conv_b': nrm(ks[6], (L, 2 * ML_W), 0.02),
        'gate_b': gate_b,
        'ml_norm_g': 1.0 + nrm(ks[9], (L, ML_DH), 0.02),
        'da_q_norm_g': 1.0 + nrm(ks[10], (L, DA_DH), 0.02),
        'da_k_norm_g': 1.0 + nrm(ks[11], (L, DA_DH), 0.02),
        'lambda_q1': nrm(ks[12], (L, DA_DH), 0.1),
        'lambda_k1': nrm(ks[13], (L, DA_DH), 0.1),
        'lambda_q2': nrm(ks[14], (L, DA_DH), 0.1),
        'lambda_k2': nrm(ks[15], (L, DA_DH), 0.1),
        'da_norm_g': 1.0 + nrm(ks[16], (L, 2 * DA_DH), 0.02),
        'w_out': nrm(ks[17], (L, D_MIX, D_MODEL), D_MIX ** -0.5),
        'w_router': nrm(ks[18], (L, D_MODEL, N_EXPERTS), D_MODEL ** -0.5),
        'router_bias': nrm(ks[19], (L, N_EXPERTS), 0.01),
        'w1': nrm(ks[20], (L, N_EXPERTS, D_MODEL, D_EXPERT), D_MODEL ** -0.5),
        'w3': nrm(ks[21], (L, N_EXPERTS, D_MODEL, D_EXPERT), D_MODEL ** -0.5),
        'w2': nrm(ks[22], (L, N_EXPERTS, D_EXPERT, D_MODEL), D_EXPERT ** -0.5),
        'ws1': nrm(ks[23], (L, D_MODEL, D_SHARED), D_MODEL ** -0.5),
        'ws3': nrm(ks[24], (L, D_MODEL, D_SHARED), D_MODEL ** -0.5),
        'ws2': nrm(jax.random.fold_in(ks[24], 1), (L, D_SHARED, D_MODEL), D_SHARED ** -0.5),
    }


def reference(x, c, w_ada, b_ada, w_in, conv_w, conv_b, gate_b, ml_norm_g, da_q_norm_g, da_k_norm_g,
              lambda_q1, lambda_k1, lambda_q2, lambda_k2, da_norm_g, w_out, w_router, router_bias,
              w1, w3, w2, ws1, ws3, ws2):
    B, S, D = x.shape
    slopes = alibi_slopes(DA_HEADS)
    for l in range(DEPTH):
        lam_init = lambda_init(l)
        mod = (jax.nn.silu(c) @ w_ada[l] + b_ada[l]).reshape(B, 6, 1, D)
        shift_a, scale_a, gate_a = mod[:, 0], mod[:, 1], mod[:, 2]
        shift_f, scale_f, gate_f = mod[:, 3], mod[:, 4], mod[:, 5]

        h = rms_norm(x) * (1.0 + scale_a) + shift_a
        proj = h @ w_in[l]
        qk_raw, mv, mo, mi, mf, dq, dk, dv = jnp.split(proj, COL_SPLITS, axis=-1)

        qk = jax.nn.silu(causal_depthwise_conv(qk_raw, conv_w[l], conv_b[l]))
        mq, mk = jnp.split(qk, 2, axis=-1)
        hm = mlstm_chunkwise(mq.reshape(B, S, ML_HEADS, ML_DH),
                             mk.reshape(B, S, ML_HEADS, ML_DH),
                             mv.reshape(B, S, ML_HEADS, ML_DH),
                             mi + gate_b[l, :ML_HEADS],
                             mf + gate_b[l, ML_HEADS:])
        hm = rms_norm(hm) * ml_norm_g[l] * jax.nn.sigmoid(mo.reshape(B, S, ML_HEADS, ML_DH).astype(jnp.float32))

        dq = rms_norm(dq.reshape(B, S, DA_HEADS, 2, DA_DH)) * da_q_norm_g[l]
        dk = rms_norm(dk.reshape(B, S, DA_HEADS, 2, DA_DH)) * da_k_norm_g[l]
        lam = (jnp.exp(jnp.sum(lambda_q1[l].astype(jnp.float32) * lambda_k1[l].astype(jnp.float32)))
               - jnp.exp(jnp.sum(lambda_q2[l].astype(jnp.float32) * lambda_k2[l].astype(jnp.float32)))
               + lam_init)
        hd = diff_attention(dq, dk, dv.reshape(B, S, DA_HEADS, 2 * DA_DH), lam, slopes)
        hd = rms_norm(hd) * da_norm_g[l] * (1.0 - lam_init)

        mix = jnp.concatenate([hm.reshape(B, S, ML_W).astype(x.dtype),
                               hd.reshape(B, S, DA_W).astype(x.dtype)], axis=-1) @ w_out[l]
        x = x + gate_a * mix

        h2 = rms_norm(x) * (1.0 + scale_f) + shift_f
        y = routed_moe(h2.reshape(B * S, D), w_router[l], router_bias[l], w1[l], w3[l], w2[l],
                       ws1[l], ws3[l], ws2[l]).reshape(B, S, D)
        x = x + gate_f * y
    return x
```

```python
import numpy as np
import ml_dtypes
from contextlib import ExitStack
import concourse.bass as bass
import concourse.mybir as mybir
from concourse.bass_utils import run_bass_kernel_spmd

F32 = mybir.dt.float32
BF16 = mybir.dt.bfloat16
I32 = mybir.dt.int32
ALU = mybir.AluOpType
AF = mybir.ActivationFunctionType
AX = mybir.AxisListType

D = 1024
DIN = 3592
NE = 256
EPS = 1e-6
SAME_ENGINE_RAW_WAIT = True
STOP = 99


class Prog:
    def __init__(self, nc, es):
        self.nc = nc
        self.es = es
        self.E = {'pe': nc.tensor, 'act': nc.scalar, 'dve': nc.vector, 'pool': nc.gpsimd, 'sp': nc.sync}
        self.sems = {}
        self.val = {}
        for e in self.E:
            self._sem(e)
        self.known = {e: {} for e in self.E}
        self.lastw = {}
        self.readers = {}
        self.n = 0

    def _sem(self, name):
        if name not in self.sems:
            self.sems[name] = self.es.enter_context(self.nc.semaphore("s_" + name))
            self.val[name] = 0
        return self.sems[name]

    def _wait(self, eng, ev):
        sn, v, snap = ev
        kn = self.known[eng]
        if kn.get(sn, 0) >= v:
            return
        self.E[eng].wait_ge(self.sems[sn], v)
        kn[sn] = v
        for k, vv in snap.items():
            if kn.get(k, 0) < vv:
                kn[k] = vv

    def op(self, eng, fn, r=(), w=(), dma=None):
        self.n += 1
        for res in r:
            ev = self.lastw.get(res)
            if ev is not None:
                if ev[0] == eng and not SAME_ENGINE_RAW_WAIT:
                    continue
                if ev[0] == 'pe' and eng == 'pe':
                    continue
                self._wait(eng, ev)
        for res in w:
            ev = self.lastw.get(res)
            if ev is not None and ev[0] != eng:
                self._wait(eng, ev)
            for sn, ev2 in self.readers.get(res, {}).items():
                if sn != eng:
                    self._wait(eng, ev2)
        ins = fn()
        kn = self.known[eng]
        if dma is None:
            self.val[eng] += 1
            ins.then_inc(self.sems[eng], 1)
            ev = (eng, self.val[eng], dict(kn))
        else:
            self._sem(dma)
            self.val[dma] += 16
            ins.then_inc(self.sems[dma], 16)
            ev = (dma, self.val[dma], dict(kn))
        for res in w:
            self.lastw[res] = ev
            self.readers[res] = {}
        for res in r:
            self.readers.setdefault(res, {})[ev[0]] = ev
        return ev

    def barrier(self):
        for e in self.E:
            for sn, v in self.val.items():
                if v > 0 and sn != e:
                    self._wait(e, (sn, v, {}))
            if self.val[e] > 0:
                self._wait(e, (e, self.val[e], {}))
        self.lastw = {}
        self.readers = {}

    def finish(self):
        self.barrier()


def build_nc(S, dbg=False):
    NBLK = S // 128
    NP = NBLK // 2
    TOWN = S // 2
    NSB = S // 512
    NB = TOWN * 8 // 128 + 256
    assert NB <= 512
    nc = bass.Bass("TRN2", target_bir_lowering=False)

    def din(name, shape, dt=F32):
        return nc.dram_tensor(name, list(shape), dt, kind="ExternalInput").ap()

    def dscr(name, shape, dt):
        return nc.dram_tensor(name, list(shape), dt).ap()

    x = din("x", [S, D])
    xo = din("xo", [TOWN, D])
    ccol = din("ccol", [128, 8])
    w_ada = din("w_ada", [D, 6 * D])
    b_ada = din("b_ada", [128, 6 * D])
    w_in = din("w_in", [D, DIN])
    convw = din("convw", [128, 8, 4])
    convb = din("convb", [128, 8])
    gate_b = din("gate_b", [128, 8])
    mlg = din("mlg", [128, 512])
    qg = din("qg", [128, 64])
    kg = din("kg", [128, 64])
    lamv = din("lamv", [128, 4, 64])
    dag = din("dag", [128, 128])
    w_out = din("w_out", [D, D])
    w_router = din("w_router", [D, NE])
    rbias = din("rbias", [128, NE])
    wexp = din("wexp", [NE * 128, 6144])
    ws1 = din("ws1", [D, 256])
    ws3 = din("ws3", [D, 256])
    ws2 = din("ws2", [256, D])
    c_identb = din("c_identb", [128, 128], BF16)
    c_identf = din("c_identf", [128, 128])
    c_tri = din("c_tri", [128, 128])
    c_tristrict = din("c_tristrict", [128, 128], BF16)
    c_maskml = din("c_maskml", [128, 128])
    c_masku = din("c_masku", [128, 2, 128], BF16)
    c_alibi = din("c_alibi", [128, 4, NBLK + 1])
    c_sel = din("c_sel", [128, 2])
    c_pidx = din("c_pidx", [128, 1])
    c_iota = din("c_iota", [128, 512])
    c_thr = din("c_thr", [128, 32])
    out = nc.dram_tensor("out", [TOWN, D], F32, kind="ExternalOutput").ap()

    KT_s = dscr("KT_s", [4, 128, S], BF16)
    QT_s = dscr("QT_s", [4, 128, TOWN], BF16)
    V_s = dscr("V_s", [4, 128, NBLK, 129], BF16)
    MIX_s = dscr("MIX_s", [128, 8, TOWN], BF16)
    H2_s = dscr("H2_s", [TOWN, D], BF16)
    ACC_s = dscr("ACC_s", [TOWN, D], F32)
    XS_s = dscr("XS_s", [NB * 128, D], BF16)
    YS_s = dscr("YS_s", [NB * 128, D], F32)
    dbgs = {}
    if dbg:
        dbgs['mod'] = nc.dram_tensor("d_mod", [128, 6 * D], F32, kind="ExternalOutput").ap()
        dbgs['mix'] = nc.dram_tensor("d_mix", [128, 8, TOWN], BF16, kind="ExternalOutput").ap()
        dbgs['acc'] = nc.dram_tensor("d_acc", [TOWN, D], F32, kind="ExternalOutput").ap()
        dbgs['h2'] = nc.dram_tensor("d_h2", [TOWN, D], BF16, kind="ExternalOutput").ap()
        dbgs['gd'] = nc.dram_tensor("d_gd", [128, NP, NE], F32, kind="ExternalOutput").ap()

    with ExitStack() as es:
        P = Prog(nc, es)
        op = P.op

        def sb(name, shape, dt=F32, st=es):
            return st.enter_context(nc.sbuf_tensor(name, list(shape), dt))

        def ps(name, shape, dt=F32, st=es):
            return st.enter_context(nc.psum_tensor(name, list(shape), dt))

        V = nc.vector
        A = nc.scalar
        G = nc.gpsimd
        T = nc.tensor
        SP = nc.sync

        def load(dst, src, key, dsem, eng='sp'):
            return op(eng, lambda: P.E[eng].dma_start(out=dst, in_=src), r=(), w=(key,), dma=dsem)

        identb = sb("identb", [128, 128], BF16)
        identf = sb("identf", [128, 128])
        tri = sb("tri", [128, 128])
        tristrict = sb("tristrict", [128, 128], BF16)
        onesf = sb("onesf", [128, 128])
        onesb = sb("onesb", [128, 128], BF16)
        maskml = sb("maskml", [128, 128])
        masku = sb("masku", [128, 2, 128], BF16)
        alibi = sb("alibi", [128, 4, NBLK + 1])
        sel = sb("sel", [128, 2])
        pidx = sb("pidx", [128, 1])
        iota = sb("iota", [128, 512])
        thr = sb("thr", [128, 32])
        mod = sb("mod", [128, 6 * D])
        lam = sb("lam", [128, 4])
        dagb = sb("dagb", [128, 128])
        for t_, s_, k_ in ((identb, c_identb, 'identb'), (identf, c_identf, 'identf'), (tri, c_tri, 'tri'),
                           (tristrict, c_tristrict, 'tristrict'), (maskml, c_maskml, 'maskml'),
                           (masku, c_masku, 'masku'), (alibi, c_alibi, 'alibi'), (sel, c_sel, 'sel'),
                           (pidx, c_pidx, 'pidx'), (iota, c_iota, 'iota'), (thr, c_thr, 'thr'),
                           (dagb, dag, 'dagb')):
            load(t_[:], s_, k_, 'd_const')
        op('dve', lambda: V.memset(onesf[:], 1.0), w=('onesf',))
        op('dve', lambda: V.memset(onesb[:], 1.0), w=('onesb',))

        with ExitStack() as p0:
            cc = sb("cc", [128, 8], st=p0)
            sc = sb("sc", [128, 8], st=p0)
            scb = sb("scb", [128, 8, 128], st=p0)
            bada = sb("bada", [128, 6 * D], st=p0)
            wst = [sb("wst%d" % i, [128, 8, 512], st=p0) for i in range(2)]
            lv = sb("lv", [128, 4, 64], st=p0)
            lt = sb("lt", [128, 2, 64], st=p0)
            ls = sb("ls", [128, 2], st=p0)
            psm = [ps("psm%d" % i, [128, 512], st=p0) for i in range(2)]
            load(cc[:], ccol, 'cc', 'd_c0')
            load(bada[:], b_ada, 'bada', 'd_c0')
            load(lv[:], lamv, 'lv', 'd_c0')
            op('act', lambda: A.activation(out=sc[:], in_=cc[:], func=AF.Silu), r=('cc',), w=('sc',))
            for k in range(8):
                op('dve', lambda k=k: V.tensor_copy(out=scb[:, k, :], in_=sc[:, k:k + 1].to_broadcast([128, 128])),
                   r=('sc',), w=('scb',))
            wv = w_ada.rearrange("(k p) n -> p k n", p=128)
            for g in range(12):
                b = g % 2
                load(wst[b][:], wv[:, :, g * 512:(g + 1) * 512], 'wst%d' % b, 'd_wst%d' % b)
                for k in range(8):
                    op('pe', lambda k=k, b=b: T.matmul(psm[b][:], lhsT=scb[:, k, :], rhs=wst[b][:, k, :],
                                                       start=(k == 0), stop=(k == 7)),
                       r=('scb', 'wst%d' % b), w=('psm%d' % b,))
                op('dve', lambda g=g, b=b: V.tensor_tensor(out=mod[:, g * 512:(g + 1) * 512], in0=psm[b][:],
                                                           in1=bada[:, g * 512:(g + 1) * 512], op=ALU.add),
                   r=('psm%d' % b, 'bada'), w=('mod',))
            for c0 in (1024, 4096):
                op('dve', lambda c0=c0: V.tensor_scalar(out=mod[:, c0:c0 + 1024], in0=mod[:, c0:c0 + 1024],
                                                        scalar1=1.0, scalar2=None, op0=ALU.add),
                   r=('mod',), w=('mod',))
            op('dve', lambda: V.tensor_tensor(out=lt[:, 0, :], in0=lv[:, 0, :], in1=lv[:, 1, :], op=ALU.mult),
               r=('lv',), w=('lt',))
            op('dve', lambda: V.tensor_tensor(out=lt[:, 1, :], in0=lv[:, 2, :], in1=lv[:, 3, :], op=ALU.mult),
               r=('lv',), w=('lt',))
            op('dve', lambda: V.tensor_reduce(out=ls[:], in_=lt[:], axis=AX.X, op=ALU.add), r=('lt',), w=('ls',))
            op('act', lambda: A.activation(out=ls[:], in_=ls[:], func=AF.Exp), r=('ls',), w=('ls',))
            op('dve', lambda: V.tensor_tensor(out=lam[:, 0:1], in0=ls[:, 0:1], in1=ls[:, 1:2], op=ALU.subtract),
               r=('ls',), w=('lam',))
            op('dve', lambda: V.tensor_scalar(out=lam[:, 0:1], in0=lam[:, 0:1], scalar1=0.2, scalar2=None,
                                              op0=ALU.add), r=('lam',), w=('lam',))
            op('dve', lambda: V.tensor_scalar(out=lam[:, 1:2], in0=lam[:, 0:1], scalar1=-1.0, scalar2=None,
                                              op0=ALU.mult), r=('lam',), w=('lam',))
            op('dve', lambda: V.tensor_scalar(out=dagb[:], in0=dagb[:], scalar1=0.8, scalar2=None, op0=ALU.mult),
               r=('dagb',), w=('dagb',))
            if dbg:
                op('sp', lambda: SP.dma_start(out=dbgs['mod'], in_=mod[:]), r=('mod',), w=('dbgmod',), dma='d_dbg')
            P.barrier()

        if STOP >= 1:
            PHASES(nc, P, locals())
        P.finish()
    return nc


def PHASES(nc, P, L):
    es = L['es']; op = P.op; sb = L['sb']; ps = L['ps']; load = L['load']
    V = nc.vector; A = nc.scalar; G = nc.gpsimd; T = nc.tensor; SP = nc.sync
    S = L['S']; NBLK = L['NBLK']; NP = L['NP']; TOWN = L['TOWN']; NSB = L['NSB']; NB = L['NB']
    dbg = L['dbg']; dbgs = L['dbgs']
    identb, identf, tri, tristrict, onesf, onesb = L['identb'], L['identf'], L['tri'], L['tristrict'], L['onesf'], L['onesb']
    maskml, masku, alibi, sel, pidx, iota, thr, mod, lam, dagb = (L['maskml'], L['masku'], L['alibi'], L['sel'],
                                                                  L['pidx'], L['iota'], L['thr'], L['mod'], L['lam'], L['dagb'])
    x, xo, w_in, out = L['x'], L['xo'], L['w_in'], L['out']
    KT_s, QT_s, V_s, MIX_s, H2_s, ACC_s, XS_s, YS_s = (L['KT_s'], L['QT_s'], L['V_s'], L['MIX_s'], L['H2_s'],
                                                       L['ACC_s'], L['XS_s'], L['YS_s'])
    SHIFT_A, SCALE_A, GATE_A, SHIFT_F, SCALE_F, GATE_F = [mod[:, i * D:(i + 1) * D] for i in range(6)]

    def rstd_from_ss(ssap, n, outap, rkey, wkey, eng='dve'):
        op(eng, lambda: V.tensor_scalar(out=outap, in0=ssap, scalar1=1.0 / n, scalar2=EPS, op0=ALU.mult, op1=ALU.add),
           r=(rkey,), w=(wkey,))
        op('act', lambda: A.activation(out=outap, in_=outap, func=AF.Sqrt), r=(wkey,), w=(wkey,))
        op(eng, lambda: V.reciprocal(out=outap, in_=outap), r=(wkey,), w=(wkey,))

    with ExitStack() as p1:
        winb = sb("winb", [128, 8, DIN], BF16, st=p1)
        with ExitStack() as p1w:
            wstg = [sb("wstg%d" % i, [128, DIN], st=p1w) for i in range(2)]
            for k in range(8):
                b = k % 2
                load(wstg[b][:], w_in[k * 128:(k + 1) * 128, :], 'wstg%d' % b, 'd_wstg%d' % b)
                e_ = 'dve' if b == 0 else 'pool'
                op(e_, lambda k=k, b=b, e_=e_: P.E[e_].tensor_copy(out=winb[:, k, :], in_=wstg[b][:]),
                   r=('wstg%d' % b,), w=('winb',))
            P.barrier()
        cw = sb("cw", [128, 8, 4], st=p1); cb = sb("cb", [128, 8], st=p1); gb = sb("gb", [128, 8], st=p1)
        mlgb = sb("mlgb", [128, 512], st=p1); qgb = sb("qgb", [128, 64], st=p1); kgb = sb("kgb", [128, 64], st=p1)
        load(cw[:], L['convw'], 'cw', 'd_c1'); load(cb[:], L['convb'], 'cb', 'd_c1'); load(gb[:], L['gate_b'], 'gb', 'd_c1')
        load(mlgb[:], L['mlg'], 'mlgb', 'd_c1'); load(qgb[:], L['qg'], 'qgb', 'd_c1'); load(kgb[:], L['kg'], 'kgb', 'd_c1')
        op('dve', lambda: V.tensor_scalar(out=qgb[:], in0=qgb[:], scalar1=0.125, scalar2=None, op0=ALU.mult),
           r=('qgb',), w=('qgb',))
        xb = [sb("xb%d" % i, [128, D], st=p1) for i in range(2)]
        junk = sb("junk", [128, D], BF16, st=p1)
        ss = sb("ss", [128, 8], st=p1)
        hn = sb("hn", [128, D], st=p1)
        hb = sb("hb", [128, D], BF16, st=p1)
        hT = sb("hT", [128, 8, 512], BF16, st=p1)
        raw = sb("raw", [128, 8, 516], st=p1)
        cacc = sb("cacc", [128, 512], st=p1)
        qkT_all = sb("qkT", [128, 2, 8, 512], BF16, st=p1)
        vaug_all = sb("vaug", [128, 2, 4, 4 * 129], BF16, st=p1)
        gsig_all = sb("gsig", [128, 2, 4, 512], st=p1)
        gcol_all = sb("gcol", [128, 2, 4, 8], st=p1)
        lf = sb("lf", [128, 4, 4], st=p1)
        gg = sb("gg", [128, 16], st=p1); eg = sb("eg", [128, 16], st=p1); eb = sb("eb", [128, 16], st=p1)
        wk = sb("wk", [128, 16], st=p1); carry = sb("carry", [128, 16], st=p1)
        tq = sb("tq", [128, 512], st=p1); tq2 = sb("tq2", [128, 512], st=p1)
        qn = sb("qn", [128, 512], BF16, st=p1); kn = sb("kn", [128, 512], BF16, st=p1)
        ss8 = sb("ss8", [128, 16], st=p1)
        qTp = sb("qTp", [128, 2, 4, 128], BF16, st=p1)
        qTo = sb("qTo", [128, 4, 128], BF16, st=p1)
        qTt = sb("qTt", [128, 4, 128], st=p1)
        kTs = sb("kTs", [128, 4, 128], BF16, st=p1)
        vda = sb("vda", [128, 4, 129], BF16, st=p1)
        Cst = sb("Cst", [128, 4, 129], st=p1)
        Cbf = sb("Cbf", [128, 4, 129], BF16, st=p1)
        sTb2 = sb("sTb", [128, 2, 128], BF16, st=p1)
        kwb2 = sb("kwb", [128, 2, 128], BF16, st=p1)
        sm2 = sb("sm", [128, 2, 8], st=p1)
        hbuf2 = sb("hbuf", [128, 2, 128], st=p1)
        junkB = sb("junkB", [128, 2, 128], BF16, st=p1)
        hm = sb("hm", [128, 2, 512], st=p1)
        hmo = sb("hmo", [128, 512], st=p1)
        hmb = sb("hmb", [128, 512], BF16, st=p1)
        hmT = sb("hmT", [128, 4, 128], BF16, st=p1)
        pT = ps("pT", [128, 1024], BF16, st=p1)
        pfm = [ps("pfm%d" % i, [128, 512], st=p1) for i in range(2)]
        ptm = [ps("ptm%d" % i, [128, 512], st=p1) for i in range(2)]
        pml = ps("pml", [128, 512], st=p1)
        pn = ps("pn", [128, 512], st=p1)
        pc = ps("pc", [128, 512], st=p1)
        op('dve', lambda: V.memset(raw[:], 0.0), w=tuple('raw%d' % g for g in range(8)))
        op('dve', lambda: V.memset(Cst[:], 0.0), w=tuple('Cst%d' % g for g in range(4)))
        op('dve', lambda: V.memset(Cbf[:], 0.0), w=tuple('Cbf%d' % g for g in range(4)))
        op('dve', lambda: V.memset(vaug_all[:], 1.0), w=tuple('vaug%d_%d' % (g, q) for g in range(4) for q in range(2)))
        op('dve', lambda: V.memset(vda[:], 1.0), w=('vda',))
        nxb = 0

        def stageA(sbi):
            nonlocal nxb
            bp = sbi % 2; kp = '_%d' % bp
            qkT = qkT_all[:, bp]; gsig = gsig_all[:, bp]; gcol = gcol_all[:, bp]
            vaug = vaug_all[:, bp].rearrange("p a (h e) -> p a h e", h=4)
            for bi in range(4):
                blk = sbi * 4 + bi
                xt = xb[nxb % 2]; xk = 'xb%d' % (nxb % 2); nxb += 1
                load(xt[:], x[blk * 128:(blk + 1) * 128, :], xk, 'd_' + xk)
                op('act', lambda xt=xt: A.activation(out=junk[:], in_=xt[:], func=AF.Square, accum_out=ss[:, 0:1]),
                   r=(xk,), w=('junk', 'ss'))
                rstd_from_ss(ss[:, 0:1], D, ss[:, 1:2], 'ss', 'ss1')
                op('dve', lambda xt=xt: V.scalar_tensor_tensor(out=hn[:], in0=xt[:], scalar=ss[:, 1:2], in1=SCALE_A,
                                                               op0=ALU.mult, op1=ALU.mult), r=(xk, 'ss1', 'mod'), w=('hn',))
                op('pool', lambda: G.tensor_tensor(out=hb[:], in0=hn[:], in1=SHIFT_A, op=ALU.add), r=('hn', 'mod'), w=('hb',))
                for k in range(8):
                    op('pe', lambda k=k: T.transpose(out=pT[:, k * 128:(k + 1) * 128], in_=hb[:, k * 128:(k + 1) * 128],
                                                     identity=identb[:]), r=('hb', 'identb'), w=('pT', 'pTk_s0', 'pTk_s1'))
                op('act', lambda bi=bi: A.activation(out=hT[:, :, bi * 128:(bi + 1) * 128],
                                                     in_=pT[:].rearrange("p (k t) -> p k t", k=8), func=AF.Copy),
                   r=('pT', 'pTk_s0', 'pTk_s1'), w=('hT',))
                yield
            for g in range(8):
                pf = pfm[g % 2]; pk = 'pfm%d' % (g % 2)
                for k in range(8):
                    op('pe', lambda g=g, k=k, pf=pf: T.matmul(pf[:], lhsT=winb[:, k, g * 128:(g + 1) * 128], rhs=hT[:, k, :],
                                                              start=(k == 0), stop=(k == 7)), r=('winb', 'hT'), w=(pk,))
                op('act', lambda g=g, pf=pf: A.activation(out=raw[:, g, 3:515], in_=pf[:], func=AF.Copy), r=(pk,), w=('raw%d' % g,))
                op('dve', lambda g=g: V.tensor_scalar(out=cacc[:], in0=raw[:, g, 3:515], scalar1=cw[:, g, 3:4],
                                                      scalar2=cb[:, g:g + 1], op0=ALU.mult, op1=ALU.add),
                   r=('raw%d' % g, 'cw', 'cb'), w=('cacc',))
                for j in range(3):
                    op('dve', lambda g=g, j=j: V.scalar_tensor_tensor(out=cacc[:], in0=raw[:, g, j:j + 512],
                                                                      scalar=cw[:, g, j:j + 1], in1=cacc[:],
                                                                      op0=ALU.mult, op1=ALU.add),
                       r=('raw%d' % g, 'cw', 'cacc'), w=('cacc',))
                op('act', lambda g=g: A.activation(out=qkT[:, g, :], in_=cacc[:], func=AF.Silu), r=('cacc',), w=('qkT%d' % g + kp,))
                op('pool', lambda g=g: G.tensor_copy(out=raw[:, g, 0:3], in_=raw[:, g, 512:515]), r=('raw%d' % g,), w=('raw%d' % g,))
                yield
            groups = [(1024, 512), (1536, 512), (2048, 8), (2056, 512), (2568, 512), (3080, 512)]
            npt = 0
            for bi in range(4):
                blk = sbi * 4 + bi
                par = blk % 2
                pair = blk // 2

                def proj(gi):
                    nonlocal npt
                    c0, wd = groups[gi]
                    pt_ = ptm[npt % 2]; pk_ = 'ptm%d' % (npt % 2); npt += 1
                    for k in range(8):
                        op('pe', lambda k=k: T.matmul(pt_[:, 0:wd], lhsT=hT[:, k, bi * 128:(bi + 1) * 128],
                                                      rhs=winb[:, k, c0:c0 + wd], start=(k == 0), stop=(k == 7)),
                           r=('hT', 'winb'), w=(pk_,))
                    return pt_, pk_
                pt_, pk_ = proj(0)
                op('act', lambda pt_=pt_: A.activation(out=vaug[:, bi, :, 0:128], in_=pt_[:].rearrange("p (h e) -> p h e", h=4),
                                                       func=AF.Copy), r=(pk_,), w=('vaug%d' % bi + kp,))
                pt_, pk_ = proj(1)
                op('act', lambda pt_=pt_: A.activation(out=gsig[:, bi, :], in_=pt_[:], func=AF.Sigmoid), r=(pk_,), w=('gsig%d' % bi + kp,))
                op('pool', lambda: G.tensor_tensor(out=gsig[:, bi, :], in0=gsig[:, bi, :], in1=mlgb[:], op=ALU.mult),
                   r=('gsig%d' % bi + kp, 'mlgb'), w=('gsig%d' % bi + kp,))
                pt_, pk_ = proj(2)
                op('dve', lambda pt_=pt_: V.tensor_tensor(out=gcol[:, bi, :], in0=pt_[:, 0:8], in1=gb[:], op=ALU.add),
                   r=(pk_, 'gb'), w=('gcol' + kp,))
                for which, gi, gn, dst in (('q', 3, qgb, qn), ('k', 4, kgb, kn)):
                    pt_, pk_ = proj(gi)
                    so = 0 if which == 'q' else 8
                    op('act', lambda pt_=pt_: A.activation(out=tq[:], in_=pt_[:], func=AF.Square), r=(pk_,), w=('tq',))
                    op('dve', lambda so=so: V.tensor_reduce(out=ss8[:, so:so + 8], in_=tq[:].rearrange("p (a d) -> p a d", d=64),
                                                            axis=AX.X, op=ALU.add), r=('tq',), w=('ss8',))
                    rstd_from_ss(ss8[:, so:so + 8], 64, ss8[:, so:so + 8], 'ss8', 'ss8')
                    op('dve', lambda pt_=pt_, so=so: V.tensor_tensor(
                        out=tq2[:].rearrange("p (a d) -> p a d", d=64), in0=pt_[:].rearrange("p (a d) -> p a d", d=64),
                        in1=ss8[:, so:so + 8].rearrange("p (a o) -> p a o", o=1).to_broadcast([128, 8, 64]), op=ALU.mult),
                       r=(pk_, 'ss8'), w=('tq2',))
                    op('pool', lambda gn=gn, dst=dst: G.tensor_tensor(
                        out=dst[:].rearrange("p (a d) -> p a d", d=64), in0=tq2[:].rearrange("p (a d) -> p a d", d=64),
                        in1=gn[:].rearrange("p (o d) -> p o d", o=1).to_broadcast([128, 8, 64]), op=ALU.mult),
                       r=('tq2', 'qgb', 'kgb'), w=(which + 'n',))
                    for h in range(4):
                        op('pe', lambda h=h, dst=dst: T.transpose(out=pT[:, h * 128:(h + 1) * 128], in_=dst[:, h * 128:(h + 1) * 128],
                                                                  identity=identb[:]), r=(which + 'n', 'identb'), w=('pT',))
                    if which == 'q':
                        op('act', lambda: A.activation(out=qTp[:, par, :, :], in_=pT[:, 0:512].rearrange("p (h t) -> p h t", h=4),
                                                       func=AF.Copy), r=('pT',), w=('qTp',))
                    else:
                        op('act', lambda: A.activation(out=kTs[:], in_=pT[:, 0:512].rearrange("p (h t) -> p h t", h=4),
                                                       func=AF.Copy), r=('pT',), w=('kTs',))
                        op('sp', lambda: SP.dma_start(out=KT_s[:, :, blk * 128:(blk + 1) * 128].rearrange("h p t -> p h t"),
                                                      in_=kTs[:]), r=('kTs',), w=('KT_s',), dma='d_kts')
                if par == 1:
                    op('dve', lambda: V.tensor_scalar(out=qTt[:], in0=qTp[:, 0, :, :], scalar1=sel[:, 0:1], scalar2=None,
                                                      op0=ALU.mult), r=('qTp', 'sel'), w=('qTt',))
                    op('dve', lambda: V.scalar_tensor_tensor(out=qTo[:], in0=qTp[:, 1, :, :], scalar=sel[:, 1:2], in1=qTt[:],
                                                             op0=ALU.mult, op1=ALU.add), r=('qTp', 'sel', 'qTt'), w=('qTo',))
                    op('sp', lambda: SP.dma_start(out=QT_s[:, :, pair * 128:(pair + 1) * 128].rearrange("h p t -> p h t"),
                                                  in_=qTo[:]), r=('qTo',), w=('QT_s',), dma='d_qts')
                pt_, pk_ = proj(5)
                op('act', lambda pt_=pt_: A.activation(out=vda[:, :, 0:128], in_=pt_[:].rearrange("p (h e) -> p h e", h=4),
                                                       func=AF.Copy), r=(pk_,), w=('vda',))
                op('sp', lambda: SP.dma_start(out=V_s[:, :, blk, :].rearrange("h p e -> p h e"), in_=vda[:]),
                   r=('vda',), w=('V_s',), dma='d_vs')
                yield
        def stageB(sbi):
            bp = sbi % 2; kp = '_%d' % bp
            qkT = qkT_all[:, bp]; gsig = gsig_all[:, bp]; gcol = gcol_all[:, bp]
            vaug = vaug_all[:, bp].rearrange("p a (h e) -> p a h e", h=4)
            fpre = gcol[:, :, 4:8]
            op('act', lambda: A.activation(out=lf[:], in_=fpre, func=AF.Exp, scale=-1.0), r=('gcol' + kp,), w=('lf',))
            op('act', lambda: A.activation(out=lf[:], in_=lf[:], func=AF.Ln, bias=1.0), r=('lf',), w=('lf',))
            op('dve', lambda: V.tensor_scalar(out=lf[:], in0=lf[:], scalar1=-1.0, scalar2=None, op0=ALU.mult), r=('lf',), w=('lf',))
            lf2 = lf[:].rearrange("p a h -> p (a h)")
            op('pe', lambda: T.matmul(pn[:, 400:416], lhsT=tri[:], rhs=lf2, start=True, stop=True), r=('tri', 'lf'), w=('bk1',))
            op('pe', lambda: T.matmul(pn[:, 416:432], lhsT=onesf[:], rhs=lf2, start=True, stop=True), r=('onesf', 'lf'), w=('bk1',))
            op('dve', lambda: V.tensor_tensor(out=gg[:].rearrange("p (a h) -> p a h", h=4), in0=gcol[:, :, 0:4],
                                              in1=pn[:, 400:416].rearrange("p (a h) -> p a h", h=4), op=ALU.subtract),
               r=('gcol' + kp, 'bk1'), w=('gg',))
            op('act', lambda: A.activation(out=eg[:], in_=gg[:], func=AF.Exp), r=('gg',), w=('eg',))
            op('act', lambda: A.activation(out=eb[:], in_=pn[:, 400:416], func=AF.Exp), r=('bk1',), w=('eb',))
            op('act', lambda: A.activation(out=carry[:], in_=pn[:, 416:432], func=AF.Exp), r=('bk1',), w=('carry',))
            op('dve', lambda: V.tensor_tensor(out=wk[:], in0=gg[:], in1=pn[:, 416:432], op=ALU.add), r=('gg', 'bk1'), w=('wk',))
            op('act', lambda: A.activation(out=wk[:], in_=wk[:], func=AF.Exp, bias=-2.4260151319598084), r=('wk',), w=('wk',))
            yield
            for bi in range(4):
                blk = sbi * 4 + bi
                par = blk % 2
                pair = blk // 2
                tsl = slice(bi * 128, (bi + 1) * 128)
                def headgen(h, sl):
                    col = bi * 4 + h
                    qT_ = qkT[:, h, tsl]
                    kT_ = qkT[:, 4 + h, tsl]
                    ks = '_s%d' % sl
                    sTb_ = sTb2[:, sl, :]; kwb_ = kwb2[:, sl, :]; sm_ = sm2[:, sl, :]; hbuf_ = hbuf2[:, sl, :]
                    bank_ = pml if sl == 0 else pn
                    bk = 'bk%d' % sl
                    pS_ = bank_[:, 0:128]
                    pK_ = pT[:, 512 + sl * 128:512 + (sl + 1) * 128]
                    pn_ = bank_[:, 128:257]
                    pc_ = bank_[:, 260:389]
                    op('pe', lambda: T.matmul(pS_, lhsT=kT_, rhs=qT_, start=True, stop=True),
                       r=('qkT%d' % h + kp, 'qkT%d' % (4 + h) + kp), w=(bk,))
                    op('pe', lambda: T.transpose(out=pK_, in_=kT_, identity=identb[:]), r=('qkT%d' % (4 + h) + kp, 'identb'), w=('pT',))
                    yield
                    op('dve', lambda: V.scalar_tensor_tensor(out=sTb_, in0=pS_, scalar=eg[:, col:col + 1], in1=maskml[:],
                                                             op0=ALU.mult, op1=ALU.mult), r=(bk, 'eg', 'maskml'), w=('sTb' + ks,))
                    op('act', lambda: A.activation(out=kwb_, in_=pK_, func=AF.Copy, scale=wk[:, col:col + 1]),
                       r=('pT', 'wk'), w=('kwb' + ks,))
                    yield
                    op('pe', lambda: T.matmul(pn_, lhsT=sTb_, rhs=vaug[:, bi, h, :], start=True, stop=False),
                       r=('sTb' + ks, 'vaug%d' % bi + kp), w=(bk,))
                    op('pe', lambda: T.matmul(pn_, lhsT=qT_, rhs=Cbf[:, h, :], start=False, stop=True),
                       r=('qkT%d' % h + kp, 'Cbf%d' % h), w=(bk,))
                    op('pe', lambda: T.matmul(pc_, lhsT=kwb_, rhs=vaug[:, bi, h, :], start=True, stop=True),
                       r=('kwb' + ks, 'vaug%d' % bi + kp), w=(bk,))
                    yield
                    op('dve', lambda: V.tensor_scalar(out=sm_[:, 0:1], in0=pn_[:, 128:129], scalar1=eb[:, col:col + 1], scalar2=None,
                                                      op0=ALU.mult), r=(bk, 'eb'), w=('sm' + ks,))
                    op('dve', lambda: V.scalar_tensor_tensor(out=Cst[:, h, :], in0=Cst[:, h, :], scalar=carry[:, col:col + 1],
                                                             in1=pc_, op0=ALU.mult, op1=ALU.add),
                       r=('Cst%d' % h, 'carry', bk), w=('Cst%d' % h,))
                    op('pool', lambda: G.tensor_copy(out=Cbf[:, h, :], in_=Cst[:, h, :]), r=('Cst%d' % h,), w=('Cbf%d' % h,))
                    yield
                    op('dve', lambda: V.scalar_tensor_tensor(out=sm_[:, 1:2], in0=sm_[:, 0:1], scalar=-1.0, in1=sm_[:, 0:1],
                                                             op0=ALU.mult, op1=ALU.max), r=('sm' + ks,), w=('sm' + ks,))
                    yield
                    op('dve', lambda: V.tensor_scalar(out=sm_[:, 1:2], in0=sm_[:, 1:2], scalar1=1.0, scalar2=None,
                                                      op0=ALU.max), r=('sm' + ks,), w=('sm' + ks,))
                    yield
                    op('dve', lambda: V.reciprocal(out=sm_[:, 1:2], in_=sm_[:, 1:2]), r=('sm' + ks,), w=('sm' + ks,))
                    yield
                    op('dve', lambda: V.tensor_tensor(out=sm_[:, 2:3], in0=sm_[:, 1:2], in1=eb[:, col:col + 1], op=ALU.mult),
                       r=('sm' + ks, 'eb'), w=('sm' + ks,))
                    yield
                    op('act', lambda: A.activation(out=hbuf_, in_=pn_[:, 0:128], func=AF.Copy, scale=sm_[:, 2:3]),
                       r=(bk, 'sm' + ks), w=('hbuf' + ks,))
                    yield
                    op('act', lambda: A.activation(out=junkB[:, sl, :], in_=hbuf_, func=AF.Square, accum_out=sm_[:, 3:4]),
                       r=('hbuf' + ks,), w=('junkB' + ks, 'sm3' + ks))
                    yield
                    op('dve', lambda: V.tensor_scalar(out=sm_[:, 4:5], in0=sm_[:, 3:4], scalar1=1.0 / 128, scalar2=EPS, op0=ALU.mult, op1=ALU.add),
                       r=('sm3' + ks,), w=('sm4' + ks,))
                    yield
                    op('act', lambda: A.activation(out=sm_[:, 4:5], in_=sm_[:, 4:5], func=AF.Sqrt), r=('sm4' + ks,), w=('sm4' + ks,))
                    yield
                    op('dve', lambda: V.reciprocal(out=sm_[:, 4:5], in_=sm_[:, 4:5]), r=('sm4' + ks,), w=('sm4' + ks,))
                    yield
                    op('dve', lambda: V.scalar_tensor_tensor(out=hm[:, par, h * 128:(h + 1) * 128], in0=hbuf_, scalar=sm_[:, 4:5],
                                                             in1=gsig[:, bi, h * 128:(h + 1) * 128], op0=ALU.mult, op1=ALU.mult),
                       r=('hbuf' + ks, 'sm4' + ks, 'gsig%d' % bi + kp), w=('hm',))

                for hp in range(2):
                    gens = [headgen(2 * hp, 0), headgen(2 * hp + 1, 1)]
                    nst = 0
                    while gens:
                        for g_ in list(gens):
                            try:
                                next(g_)
                            except StopIteration:
                                gens.remove(g_)
                        nst += 1
                        if nst % 4 == 0:
                            yield
                    yield
                if par == 1:
                    op('dve', lambda: V.tensor_scalar(out=hmo[:], in0=hm[:, 0, :], scalar1=sel[:, 0:1], scalar2=None, op0=ALU.mult),
                       r=('hm', 'sel'), w=('hmo',))
                    op('dve', lambda: V.scalar_tensor_tensor(out=hmb[:], in0=hm[:, 1, :], scalar=sel[:, 1:2], in1=hmo[:],
                                                             op0=ALU.mult, op1=ALU.add), r=('hm', 'sel', 'hmo'), w=('hmb',))
                    for h in range(4):
                        op('pe', lambda h=h: T.transpose(out=pT[:, h * 128:(h + 1) * 128], in_=hmb[:, h * 128:(h + 1) * 128],
                                                         identity=identb[:]), r=('hmb', 'identb'), w=('pT',))
                    op('act', lambda: A.activation(out=hmT[:], in_=pT[:, 0:512].rearrange("p (h t) -> p h t", h=4), func=AF.Copy),
                       r=('pT',), w=('hmT',))
                    op('sp', lambda: SP.dma_start(out=MIX_s[:, 0:4, pair * 128:(pair + 1) * 128], in_=hmT[:]),
                       r=('hmT',), w=('MIX_s',), dma='d_mixs')
                yield

        def run_il(ga, gb_):
            alive = [g_ for g_ in (ga, gb_) if g_ is not None]
            while alive:
                for g_ in list(alive):
                    try:
                        next(g_)
                    except StopIteration:
                        alive.remove(g_)
        run_il(stageA(0), None)
        for sbi in range(NSB):
            run_il(stageA(sbi + 1) if sbi + 1 < NSB else None, stageB(sbi))
        P.barrier()

    if STOP < 2:
        return
    with ExitStack() as p2:
        KT = [sb("KT%d" % i, [128, S], BF16, st=p2) for i in range(2)]
        VV = [sb("VV%d" % i, [128, NBLK, 129], BF16, st=p2) for i in range(2)]
        QT = [[sb("QT%d_%d" % (i, m), [128, TOWN], BF16, st=p2) for m in range(2)] for i in range(2)]
        for i in range(2):
            for m in range(2):
                op('dve', lambda i=i, m=m: V.memset(QT[i][m][:], 0.0), w=('QT%d' % i,))
        pT_ = [sb("pTb%d" % i, [128, 256], BF16, st=p2) for i in range(3)]
        o1 = sb("o1", [128, 128], st=p2); oo = sb("oo", [128, 128], st=p2)
        hdb = sb("hdb", [128, 128], BF16, st=p2); hdT = sb("hdT", [128, 128], BF16, st=p2)
        sd = sb("sd", [128, 8], st=p2); junk2 = sb("junk2", [128, 128], BF16, st=p2)
        psS = [ps("psS%d" % i, [128, 512], st=p2) for i in range(3)]
        psO1 = [ps("psO1_%d" % i, [128, 512], st=p2) for i in range(2)]
        psO2 = [ps("psO2_%d" % i, [128, 512], st=p2) for i in range(2)]
        psX = ps("psX", [128, 1024], BF16, st=p2)
        it = 0
        for h in range(4):
            hb_ = h % 2
            load(KT[hb_][:], KT_s[h], 'KT%d' % hb_, 'd_KT%d' % hb_)
            load(VV[hb_][:], V_s[h], 'VV%d' % hb_, 'd_VV%d' % hb_)
            for m in range(2):
                op('sp', lambda m=m: SP.dma_start(out=QT[hb_][m][m * 64:(m + 1) * 64, :], in_=QT_s[h, m * 64:(m + 1) * 64, :]),
                   r=(), w=('QT%d' % hb_,), dma='d_QT%d' % hb_)
            items = [(p, j) for p in range(NP) for j in range(2 * p + 2)]
            LOOK = 2
            bufof = {}

            def emit_S(ix):
                nonlocal it
                p, j = items[ix]
                u = 2 * p + 1 - j
                sbuf_i = it % 3; it += 1
                bufof[ix] = sbuf_i
                pS = psS[sbuf_i]; pSk = 'psS%d' % sbuf_i; pb = pT_[sbuf_i]; pbk = 'pTb%d' % sbuf_i
                for m in range(2):
                    op('pe', lambda m=m: T.matmul(pS[:, m * 128:(m + 1) * 128], lhsT=KT[hb_][:, j * 128:(j + 1) * 128],
                                                  rhs=QT[hb_][m][:, p * 128:(p + 1) * 128], start=True, stop=True),
                       r=('KT%d' % hb_, 'QT%d' % hb_), w=(pSk,))
                op('act', lambda: A.activation(out=pb[:], in_=pS[:, 0:256], func=AF.Exp, bias=alibi[:, h, u:u + 1]),
                   r=(pSk, 'alibi'), w=(pbk,))
                if u <= 1:
                    op('dve', lambda: V.tensor_tensor(out=pb[:].rearrange("p (m t) -> p m t", m=2),
                                                      in0=pb[:].rearrange("p (m t) -> p m t", m=2),
                                                      in1=masku[:, u:u + 1, :].to_broadcast([128, 2, 128]), op=ALU.mult),
                       r=(pbk, 'masku'), w=(pbk,))

            def emit_AV(ix):
                p, j = items[ix]
                ob = p % 2
                nj = 2 * p + 2
                sbuf_i = bufof.pop(ix)
                pb = pT_[sbuf_i]; pbk = 'pTb%d' % sbuf_i
                op('pe', lambda: T.matmul(psO1[ob][:, 0:129], lhsT=pb[:, 0:128], rhs=VV[hb_][:, j, :], start=(j == 0), stop=(j == nj - 1)),
                   r=(pbk, 'VV%d' % hb_), w=('psO1_%d' % ob,))
                op('pe', lambda: T.matmul(psO2[ob][:, 0:129], lhsT=pb[:, 128:256], rhs=VV[hb_][:, j, :], start=(j == 0), stop=(j == nj - 1)),
                   r=(pbk, 'VV%d' % hb_), w=('psO2_%d' % ob,))
                if j != nj - 1:
                    return
                op('dve', lambda: V.reciprocal(out=sd[:, 0:1], in_=psO1[ob][:, 128:129]), r=('psO1_%d' % ob,), w=('sd',))
                op('dve', lambda: V.reciprocal(out=sd[:, 1:2], in_=psO2[ob][:, 128:129]), r=('psO2_%d' % ob,), w=('sd',))
                op('dve', lambda: V.tensor_tensor(out=sd[:, 2:3], in0=sd[:, 1:2], in1=lam[:, 1:2], op=ALU.mult), r=('sd', 'lam'), w=('sd',))
                op('dve', lambda: V.tensor_scalar(out=o1[:], in0=psO1[ob][:, 0:128], scalar1=sd[:, 0:1], scalar2=None, op0=ALU.mult),
                   r=('psO1_%d' % ob, 'sd'), w=('o1',))
                op('dve', lambda: V.scalar_tensor_tensor(out=oo[:], in0=psO2[ob][:, 0:128], scalar=sd[:, 2:3], in1=o1[:],
                                                         op0=ALU.mult, op1=ALU.add), r=('psO2_%d' % ob, 'sd', 'o1'), w=('oo',))
                op('dve', lambda: V.tensor_tensor(out=o1[:], in0=oo[:], in1=oo[:], op=ALU.mult), r=('oo',), w=('o1',))
                op('dve', lambda: V.tensor_reduce(out=sd[:, 3:4], in_=o1[:], axis=AX.X, op=ALU.add), r=('o1',), w=('sd3',))
                op('dve', lambda: V.tensor_scalar(out=sd[:, 4:5], in0=sd[:, 3:4], scalar1=1.0 / 128, scalar2=EPS, op0=ALU.mult, op1=ALU.add),
                   r=('sd3',), w=('sd4',))
                op('act', lambda: A.activation(out=sd[:, 4:5], in_=sd[:, 4:5], func=AF.Ln), r=('sd4',), w=('sd4',))
                op('act', lambda: A.activation(out=sd[:, 4:5], in_=sd[:, 4:5], func=AF.Exp, scale=-0.5), r=('sd4',), w=('sd4',))
                op('dve', lambda: V.scalar_tensor_tensor(out=hdb[:], in0=oo[:], scalar=sd[:, 4:5], in1=dagb[:], op0=ALU.mult, op1=ALU.mult),
                   r=('oo', 'sd4', 'dagb'), w=('hdb',))
                op('pe', lambda: T.transpose(out=psX[:, 0:128], in_=hdb[:], identity=identb[:]), r=('hdb', 'identb'), w=('psX',))
                op('dve', lambda: V.tensor_copy(out=hdT[:], in_=psX[:, 0:128]), r=('psX',), w=('hdT',))
                op('sp', lambda: SP.dma_start(out=MIX_s[:, 4 + h, p * 128:(p + 1) * 128], in_=hdT[:]), r=('hdT',), w=('MIX_s',), dma='d_mixs2')

            for ix in range(len(items) + LOOK):
                if ix < len(items):
                    emit_S(ix)
                if ix - LOOK >= 0:
                    emit_AV(ix - LOOK)
        if dbg:
            op('sp', lambda: SP.dma_start(out=dbgs['mix'], in_=MIX_s), r=('MIX_s',), w=('dbgmix',), dma='d_dbg')
        P.barrier()

    if STOP >= 3:
        PHASES2(nc, P, L, locals())


def PHASES2(nc, P, L, L2):
    es = L['es']; op = P.op; sb = L['sb']; ps = L['ps']; load = L['load']
    V = nc.vector; A = nc.scalar; G = nc.gpsimd; T = nc.tensor; SP = nc.sync
    S = L['S']; NBLK = L['NBLK']; NP = L['NP']; TOWN = L['TOWN']; NB = L['NB']
    dbg = L['dbg']; dbgs = L['dbgs']
    identb, identf, tri, tristrict, onesf, onesb = L['identb'], L['identf'], L['tri'], L['tristrict'], L['onesf'], L['onesb']
    pidx, iota, thr, mod = L['pidx'], L['iota'], L['thr'], L['mod']
    xo, out = L['xo'], L['out']
    MIX_s, H2_s, ACC_s, XS_s, YS_s = L['MIX_s'], L['H2_s'], L['ACC_s'], L['XS_s'], L['YS_s']
    SHIFT_A, SCALE_A, GATE_A, SHIFT_F, SCALE_F, GATE_F = [mod[:, i * D:(i + 1) * D] for i in range(6)]
    rstd_from_ss = L2['rstd_from_ss']

    with ExitStack() as p3:
        DESTI = sb("DESTI", [128, NP, 8], I32, st=p3)
        GATE8 = sb("GATE8", [128, NP, 8], st=p3)
        WIDX = sb("WIDX", [128, NB], I32, st=p3)
        cntbc = sb("cntbc", [128, NE], st=p3)
        p34 = ExitStack()
        RANK = sb("RANK", [128, NP, NE], st=p34)
        GD = sb("GD", [128, NP, NE], st=p34)
        with ExitStack() as p3a:
            woutb = sb("woutb", [128, 8, D], BF16, st=p3a)
            wrt = sb("wrt", [128, 8, NE], st=p3a)
            ws1b = sb("ws1b", [128, 8, 256], BF16, st=p3a); ws3b = sb("ws3b", [128, 8, 256], BF16, st=p3a)
            ws2b = sb("ws2b", [128, 2, D], BF16, st=p3a)
            rb = sb("rb", [128, NE], st=p3a)
            load(rb[:], L['rbias'], 'rb', 'd_c3')
            load(wrt[:], L['w_router'].rearrange("(k p) n -> p k n", p=128), 'wrt', 'd_c3')
            with ExitStack() as p3w:
                stg = sb("stg", [128, 4, D], st=p3w)
                wov = L['w_out'].rearrange("(k p) n -> p k n", p=128)
                for hf in range(2):
                    load(stg[:], wov[:, hf * 4:(hf + 1) * 4, :], 'stg', 'd_stg')
                    op('dve', lambda hf=hf: V.tensor_copy(out=woutb[:, hf * 4:(hf + 1) * 4, :], in_=stg[:]), r=('stg',), w=('woutb',))
                stv = stg[:].rearrange("p a n -> p (a n)")[:, 0:2048].rearrange("p (k n) -> p k n", k=8)
                load(stv, L['ws1'].rearrange("(k p) n -> p k n", p=128), 'stg', 'd_stg')
                op('dve', lambda: V.tensor_copy(out=ws1b[:], in_=stv), r=('stg',), w=('ws1b',))
                load(stv, L['ws3'].rearrange("(k p) n -> p k n", p=128), 'stg', 'd_stg')
                op('dve', lambda: V.tensor_copy(out=ws3b[:], in_=stv), r=('stg',), w=('ws3b',))
                load(stg[:, 0:2, :], L['ws2'].rearrange("(k p) n -> p k n", p=128), 'stg', 'd_stg')
                op('dve', lambda: V.tensor_copy(out=ws2b[:], in_=stg[:, 0:2, :]), r=('stg',), w=('ws2b',))
                P.barrier()
            xob = sb("xob", [128, D], st=p3a)
            mixT = sb("mixT", [128, 8, 128], BF16, st=p3a)
            x1 = sb("x1", [128, D], st=p3a)
            junk3 = sb("junk3", [128, D], BF16, st=p3a)
            s3 = sb("s3", [128, 8], st=p3a)
            h2n = sb("h2n", [128, D], st=p3a)
            h2 = sb("h2", [128, D], st=p3a)
            h2b = sb("h2b", [128, D], BF16, st=p3a)
            h2T = sb("h2T", [128, 8, 128], st=p3a)
            h2Tb = sb("h2Tb", [128, 8, 128], BF16, st=p3a)
            sg = sb("sg", [128, NE], st=p3a); selv = sb("selv", [128, NE], st=p3a)
            m8 = sb("m8", [128, 8, 8], st=p3a); gs = sb("gs", [128, 8], st=p3a); gm8 = sb("gm8", [128, 8], st=p3a)
            gmask = sb("gmask", [128, 8], st=p3a); gneg = sb("gneg", [128, 8], st=p3a)
            msk = sb("msk", [128, NE], st=p3a); t8 = sb("t8", [128, 8], st=p3a)
            mask8 = sb("mask8", [128, NE], st=p3a); mask8b = sb("mask8b", [128, NE], BF16, st=p3a)
            sil = sb("sil", [128, 256], st=p3a); actT = sb("actT", [128, 2, 128], BF16, st=p3a)
            accb = sb("accb", [128, D], st=p3a)
            pbig = ps("pbig", [128, 1024], st=p3a)
            pTf = ps("pTf", [128, 1024], st=p3a)
            pr = ps("pr", [128, 512], st=p3a)
            pg = ps("pg", [128, 512], st=p3a)
            pcn = ps("pcn", [128, 512], st=p3a)
            op('dve', lambda: V.memset(cntbc[:], 0.0), w=('cntbc',))
            for p in range(NP):
                tsl = slice(p * 128, (p + 1) * 128)
                load(xob[:], xo[tsl, :], 'xob', 'd_xob')
                load(mixT[:], MIX_s[:, :, tsl], 'mixT', 'd_mixT')
                for nh in range(2):
                    for k in range(8):
                        op('pe', lambda nh=nh, k=k: T.matmul(pbig[:, nh * 512:(nh + 1) * 512], lhsT=mixT[:, k, :],
                                                             rhs=woutb[:, k, nh * 512:(nh + 1) * 512], start=(k == 0), stop=(k == 7)),
                           r=('mixT', 'woutb'), w=('pbig',))
                op('dve', lambda: V.tensor_tensor(out=x1[:], in0=pbig[:], in1=GATE_A, op=ALU.mult), r=('pbig', 'mod'), w=('x1',))
                op('pool', lambda: G.tensor_tensor(out=x1[:], in0=x1[:], in1=xob[:], op=ALU.add), r=('x1', 'xob'), w=('x1',))
                op('act', lambda: A.activation(out=junk3[:], in_=x1[:], func=AF.Square, accum_out=s3[:, 0:1]), r=('x1',), w=('junk3', 's3'))
                rstd_from_ss(s3[:, 0:1], D, s3[:, 1:2], 's3', 's31')
                op('dve', lambda: V.scalar_tensor_tensor(out=h2n[:], in0=x1[:], scalar=s3[:, 1:2], in1=SCALE_F, op0=ALU.mult, op1=ALU.mult),
                   r=('x1', 's31', 'mod'), w=('h2n',))
                op('pool', lambda: G.tensor_tensor(out=h2[:], in0=h2n[:], in1=SHIFT_F, op=ALU.add), r=('h2n', 'mod'), w=('h2',))
                op('act', lambda: A.activation(out=h2b[:], in_=h2[:], func=AF.Copy), r=('h2',), w=('h2b',))
                op('sp', lambda: SP.dma_start(out=H2_s[tsl, :], in_=h2b[:]), r=('h2b',), w=('H2_s',), dma='d_h2s')
                for k in range(8):
                    op('pe', lambda k=k: T.transpose(out=pTf[:, k * 128:(k + 1) * 128], in_=h2[:, k * 128:(k + 1) * 128], identity=identf[:]),
                       r=('h2', 'identf'), w=('pTf',))
                op('act', lambda: A.activation(out=h2T[:], in_=pTf[:].rearrange("p (k t) -> p k t", k=8), func=AF.Copy), r=('pTf',), w=('h2T',))
                op('dve', lambda: V.tensor_copy(out=h2Tb[:], in_=h2T[:]), r=('h2T',), w=('h2Tb',))
                for k in range(8):
                    op('pe', lambda k=k: T.matmul(pr[:, 0:NE], lhsT=h2T[:, k, :], rhs=wrt[:, k, :], start=(k == 0), stop=(k == 7)),
                       r=('h2T', 'wrt'), w=('pr',))
                op('act', lambda: A.activation(out=sg[:], in_=pr[:, 0:NE], func=AF.Sigmoid), r=('pr',), w=('sg',))
                op('dve', lambda: V.tensor_tensor(out=selv[:], in0=sg[:], in1=rb[:], op=ALU.add), r=('sg', 'rb'), w=('selv',))
                for g in range(8):
                    op('dve', lambda g=g: V.max(out=m8[:, g, :], in_=selv[:, g * 32:(g + 1) * 32]), r=('selv',), w=('m8',))
                op('dve', lambda: V.tensor_tensor(out=gs[:], in0=m8[:, :, 0], in1=m8[:, :, 1], op=ALU.add), r=('m8',), w=('gs',))
                op('dve', lambda: V.max(out=gm8[:], in_=gs[:]), r=('gs',), w=('gm8',))
                op('dve', lambda: V.tensor_scalar(out=gmask[:], in0=gs[:], scalar1=gm8[:, 3:4], scalar2=None, op0=ALU.is_ge),
                   r=('gs', 'gm8'), w=('gmask',))
                op('dve', lambda: V.tensor_scalar(out=gneg[:], in0=gmask[:], scalar1=1.0, scalar2=1e30, op0=ALU.subtract, op1=ALU.mult),
                   r=('gmask',), w=('gneg',))
                op('dve', lambda: V.tensor_tensor(out=msk[:].rearrange("p (g e) -> p g e", g=8), in0=selv[:].rearrange("p (g e) -> p g e", g=8),
                                                  in1=gmask[:].rearrange("p (g o) -> p g o", o=1).to_broadcast([128, 8, 32]), op=ALU.mult),
                   r=('selv', 'gmask'), w=('msk',))
                op('dve', lambda: V.tensor_tensor(out=msk[:].rearrange("p (g e) -> p g e", g=8), in0=msk[:].rearrange("p (g e) -> p g e", g=8),
                                                  in1=gneg[:].rearrange("p (g o) -> p g o", o=1).to_broadcast([128, 8, 32]), op=ALU.add),
                   r=('msk', 'gneg'), w=('msk',))
                op('dve', lambda: V.max(out=t8[:], in_=msk[:]), r=('msk',), w=('t8',))
                op('dve', lambda: V.tensor_scalar(out=mask8[:], in0=msk[:], scalar1=t8[:, 7:8], scalar2=None, op0=ALU.is_ge),
                   r=('msk', 't8'), w=('mask8',))
                op('pool', lambda: G.tensor_copy(out=mask8b[:], in_=mask8[:]), r=('mask8',), w=('mask8b',))
                op('dve', lambda: V.tensor_tensor(out=GD[:, p, :], in0=sg[:], in1=mask8[:], op=ALU.mult), r=('sg', 'mask8'), w=('GD',))
                op('dve', lambda: V.tensor_reduce(out=s3[:, 2:3], in_=GD[:, p, :], axis=AX.X, op=ALU.add), r=('GD',), w=('s32',))
                op('dve', lambda: V.reciprocal(out=s3[:, 3:4], in_=s3[:, 2:3]), r=('s32',), w=('s33',))
                op('dve', lambda: V.tensor_scalar(out=s3[:, 3:4], in0=s3[:, 3:4], scalar1=2.5, scalar2=None, op0=ALU.mult),
                   r=('s33',), w=('s33',))
                op('dve', lambda: V.tensor_scalar(out=GD[:, p, :], in0=GD[:, p, :], scalar1=s3[:, 3:4], scalar2=None, op0=ALU.mult),
                   r=('GD', 's33'), w=('GD',))
                op('pe', lambda: T.matmul(pcn[:, 0:NE], lhsT=tristrict[:], rhs=mask8b[:], start=True, stop=True),
                   r=('tristrict', 'mask8b'), w=('pcn',))
                op('pe', lambda: T.matmul(pcn[:, NE:2 * NE], lhsT=onesb[:], rhs=mask8b[:], start=True, stop=True),
                   r=('onesb', 'mask8b'), w=('pcn',))
                op('dve', lambda: V.tensor_tensor(out=RANK[:, p, :], in0=pcn[:, 0:NE], in1=cntbc[:], op=ALU.add), r=('pcn', 'cntbc'), w=('RANK',))
                op('dve', lambda: V.tensor_tensor(out=cntbc[:], in0=pcn[:, NE:2 * NE], in1=cntbc[:], op=ALU.add), r=('pcn', 'cntbc'), w=('cntbc',))
                for wi, wb_ in enumerate((ws1b, ws3b)):
                    for fc in range(2):
                        c0 = (wi * 2 + fc) * 128
                        for k in range(8):
                            op('pe', lambda k=k, fc=fc, wb_=wb_, c0=c0: T.matmul(pg[:, c0:c0 + 128], lhsT=wb_[:, k, fc * 128:(fc + 1) * 128],
                                                                                 rhs=h2Tb[:, k, :], start=(k == 0), stop=(k == 7)),
                               r=('h2Tb', 'ws1b', 'ws3b'), w=('pg',))
                op('act', lambda: A.activation(out=sil[:], in_=pg[:, 0:256], func=AF.Silu), r=('pg',), w=('sil',))
                op('dve', lambda: V.tensor_tensor(out=actT[:].rearrange("p a t -> p (a t)"), in0=sil[:], in1=pg[:, 256:512], op=ALU.mult),
                   r=('sil', 'pg'), w=('actT',))
                for nh in range(2):
                    for fc in range(2):
                        op('pe', lambda nh=nh, fc=fc: T.matmul(pbig[:, nh * 512:(nh + 1) * 512], lhsT=actT[:, fc, :],
                                                               rhs=ws2b[:, fc, nh * 512:(nh + 1) * 512], start=(fc == 0), stop=(fc == 1)),
                           r=('actT', 'ws2b'), w=('pbig',))
                op('dve', lambda: V.tensor_tensor(out=accb[:], in0=pbig[:], in1=GATE_F, op=ALU.mult), r=('pbig', 'mod'), w=('accb',))
                op('pool', lambda: G.tensor_tensor(out=accb[:], in0=accb[:], in1=x1[:], op=ALU.add), r=('accb', 'x1'), w=('accb',))
                op('sp', lambda: SP.dma_start(out=ACC_s[tsl, :], in_=accb[:]), r=('accb',), w=('ACC_s',), dma='d_accs')
            if dbg:
                op('sp', lambda: SP.dma_start(out=dbgs['acc'], in_=ACC_s), r=('ACC_s',), w=('dbgacc',), dma='d_dbg')
                op('sp', lambda: SP.dma_start(out=dbgs['h2'], in_=H2_s), r=('H2_s',), w=('dbgh2',), dma='d_dbg')
                op('sp', lambda: SP.dma_start(out=dbgs['gd'], in_=GD[:]), r=('GD',), w=('dbggd',), dma='d_dbg')
            P.barrier()

        if STOP < 4:
            p34.close()
            return
        with ExitStack() as p4:
            cntT = sb("cntT", [128, 2], st=p4)
            cmp_ = sb("cmp", [128, 32], st=p4)
            nblk = sb("nblk", [128, 2], st=p4)
            nbb = sb("nbb", [128, 2, 128], st=p4)
            trihi = sb("trihi", [128, 2, NE], st=p4)
            trilo = sb("trilo", [128, 2, NE], st=p4)
            pstart = sb("pstart", [128, NE], st=p4)
            pendc = sb("pendc", [128, 2], st=p4)
            Am = sb("Am", [128, 2, 512], st=p4)
            bef = sb("bef", [128, 512], st=p4)
            key = sb("key", [128, NE], st=p4); k8 = sb("k8", [128, 8], st=p4); oh = sb("oh", [128, NE], st=p4)
            d8 = sb("d8", [128, 8], st=p4)
            pq = ps("pq", [128, 512], st=p4)
            pq2 = ps("pq2", [128, 512], st=p4)
            pq3 = ps("pq3", [128, 512], st=p4)
            for c in range(2):
                op('pe', lambda c=c: T.transpose(out=pq[:, c * 128:(c + 1) * 128], in_=cntbc[:, c * 128:(c + 1) * 128], identity=identf[:]),
                   r=('cntbc', 'identf'), w=('pq',))
            op('dve', lambda: V.tensor_copy(out=cntT[:], in_=pq[:, 0:256].rearrange("p (c t) -> p c t", c=2)[:, :, 0]), r=('pq',), w=('cntT',))
            for c in range(2):
                op('dve', lambda c=c: V.tensor_scalar(out=cmp_[:], in0=thr[:], scalar1=cntT[:, c:c + 1], scalar2=None, op0=ALU.is_lt),
                   r=('thr', 'cntT'), w=('cmp',))
                op('dve', lambda c=c: V.tensor_reduce(out=nblk[:, c:c + 1], in_=cmp_[:], axis=AX.X, op=ALU.add), r=('cmp',), w=('nblk',))
                op('dve', lambda c=c: V.tensor_copy(out=nbb[:, c, :], in_=nblk[:, c:c + 1].to_broadcast([128, 128])), r=('nblk',), w=('nbb',))
            op('dve', lambda: V.memset(trihi[:], 0.0), w=('trihi',))
            op('dve', lambda: V.tensor_copy(out=trihi[:, 0, 0:128], in_=tri[:]), r=('tri',), w=('trihi',))
            op('dve', lambda: V.memset(trihi[:, 0, 128:256], 1.0), w=('trihi',))
            op('dve', lambda: V.tensor_copy(out=trihi[:, 1, 128:256], in_=tri[:]), r=('tri',), w=('trihi',))
            op('dve', lambda: V.tensor_copy(out=trilo[:], in_=trihi[:]), r=('trihi',), w=('trilo',))
            op('dve', lambda: V.tensor_tensor(out=trilo[:, 0, 0:128], in0=trilo[:, 0, 0:128], in1=identf[:], op=ALU.subtract),
               r=('trilo', 'identf'), w=('trilo',))
            op('dve', lambda: V.tensor_tensor(out=trilo[:, 1, 128:256], in0=trilo[:, 1, 128:256], in1=identf[:], op=ALU.subtract),
               r=('trilo', 'identf'), w=('trilo',))
            for c in range(2):
                op('pe', lambda c=c: T.matmul(pq2[:, 0:NE], lhsT=nbb[:, c, :], rhs=trihi[:, c, :], start=(c == 0), stop=(c == 1)),
                   r=('nbb', 'trihi'), w=('pq2',))
            for c in range(2):
                op('pe', lambda c=c: T.matmul(pq2[:, NE:2 * NE], lhsT=nbb[:, c, :], rhs=trilo[:, c, :], start=(c == 0), stop=(c == 1)),
                   r=('nbb', 'trilo'), w=('pq2',))
            op('dve', lambda: V.tensor_scalar(out=pstart[:], in0=pq2[:, NE:2 * NE], scalar1=128.0, scalar2=1.0, op0=ALU.mult, op1=ALU.add),
               r=('pq2',), w=('pstart',))
            op('dve', lambda: V.tensor_copy(out=bef[:, 0:NE], in_=pq2[:, 0:NE]), r=('pq2',), w=('bef',))
            for c in range(2):
                op('pe', lambda c=c: T.transpose(out=pq[:, 256 + c * 128:256 + (c + 1) * 128], in_=bef[:, c * 128:(c + 1) * 128], identity=identf[:]),
                   r=('bef', 'identf'), w=('pq',))
            op('dve', lambda: V.tensor_copy(out=pendc[:], in_=pq[:, 256:512].rearrange("p (c t) -> p c t", c=2)[:, :, 0]), r=('pq',), w=('pendc',))
            for c in range(2):
                op('dve', lambda c=c: V.tensor_scalar(out=Am[:, c, 0:NB], in0=iota[:, 0:NB], scalar1=pendc[:, c:c + 1], scalar2=None, op0=ALU.is_ge),
                   r=('iota', 'pendc'), w=('Am',))
            for c in range(2):
                op('pe', lambda c=c: T.matmul(pq3[:, 0:NB], lhsT=onesf[:], rhs=Am[:, c, 0:NB], start=(c == 0), stop=(c == 1)),
                   r=('onesf', 'Am'), w=('pq3',))
            op('dve', lambda: V.tensor_scalar(out=bef[:, 0:NB], in0=pq3[:, 0:NB], scalar1=255.0, scalar2=128.0, op0=ALU.min, op1=ALU.mult),
               r=('pq3',), w=('bef',))
            op('dve', lambda: V.tensor_scalar(out=bef[:, 0:NB], in0=bef[:, 0:NB], scalar1=pidx[:, 0:1], scalar2=None, op0=ALU.add),
               r=('bef', 'pidx'), w=('bef',))
            op('dve', lambda: V.tensor_copy(out=WIDX[:], in_=bef[:, 0:NB]), r=('bef',), w=('WIDX',))
            for p in range(NP):
                op('dve', lambda p=p: V.tensor_tensor(out=key[:], in0=RANK[:, p, :], in1=pstart[:], op=ALU.add), r=('RANK', 'pstart'), w=('key',))
                op('dve', lambda p=p: V.scalar_tensor_tensor(out=oh[:], in0=GD[:, p, :], scalar=0.0, in1=key[:], op0=ALU.is_gt, op1=ALU.mult),
                   r=('GD', 'key'), w=('oh',))
                op('dve', lambda: V.max(out=k8[:], in_=oh[:]), r=('oh',), w=('k8',))
                op('dve', lambda: V.tensor_scalar(out=d8[:], in0=k8[:], scalar1=-1.0, scalar2=None, op0=ALU.add), r=('k8',), w=('d8',))
                op('dve', lambda p=p: V.tensor_copy(out=DESTI[:, p, :], in_=d8[:]), r=('d8',), w=('DESTI',))
                for k in range(8):
                    op('dve', lambda p=p, k=k: V.scalar_tensor_tensor(out=key[:], in0=oh[:], scalar=k8[:, k:k + 1], in1=GD[:, p, :],
                                                                      op0=ALU.is_equal, op1=ALU.mult), r=('oh', 'k8', 'GD'), w=('key',))
                    op('dve', lambda p=p, k=k: V.tensor_reduce(out=GATE8[:, p, k:k + 1], in_=key[:], axis=AX.X, op=ALU.add),
                       r=('key',), w=('GATE8',))
            P.barrier()
        p34.close()

        if STOP < 5:
            return
        with ExitStack() as p5:
            hrow = [sb("hrow%d" % i, [128, D], BF16, st=p5) for i in range(2)]
            for p in range(NP):
                b = p % 2
                load(hrow[b][:], H2_s[p * 128:(p + 1) * 128, :], 'hrow%d' % b, 'd_hrow%d' % b)
                for k in range(8):
                    op('pool', lambda p=p, k=k, b=b: G.indirect_dma_start(
                        out=XS_s, out_offset=bass.IndirectOffsetOnAxis(DESTI[:, p, k:k + 1], 0), in_=hrow[b][:], in_offset=None),
                       r=('hrow%d' % b, 'DESTI'), w=('XS_s',), dma='d_scat%d' % b)
            P.barrier()

        if STOP < 6:
            return
        with ExitStack() as p6:
            xs = [sb("xs%d" % i, [128, D], BF16, st=p6) for i in range(3)]
            xT = sb("xT", [128, 8, 128], BF16, st=p6)
            wfa = [sb("wfa_%d" % i, [128, 6144], st=p6) for i in range(3)]
            wf = [[wfa[i][:, j * 2048:(j + 1) * 2048] for i in range(3)] for j in range(3)]
            wb = [[sb("wb%d_%d" % (j, i), [128, 2048], BF16, st=p6) for i in range(2)] for j in range(3)]
            sil6 = sb("sil6", [128, 256], st=p6); act6 = sb("act6", [128, 2, 128], BF16, st=p6)
            ysb = [sb("ysb%d" % i, [128, D], st=p6) for i in range(2)]
            pX = ps("pX6", [128, 1024], BF16, st=p6)
            pG = [ps("pG6_%d" % i, [128, 512], st=p6) for i in range(2)]
            pY = [ps("pY6_%d" % i, [128, 1024], st=p6) for i in range(2)]
            cast_eng = ('dve', 'act', 'act')

            def fetch(i):
                b = i % 3
                load(xs[b][:], XS_s[i * 128:(i + 1) * 128, :], 'xs%d' % b, 'd_xs%d' % b)
                op('pool', lambda b=b, i=i: G.indirect_dma_start(
                    out=wfa[b][:], out_offset=None, in_=L['wexp'], in_offset=bass.IndirectOffsetOnAxis(WIDX[:, i:i + 1], 0)),
                   r=('WIDX',), w=('wf0_%d' % b, 'wf1_%d' % b, 'wf2_%d' % b), dma='d_wf_%d' % b)
            fetch(0)
            fetch(1)
            for i in range(NB):
                b = i % 2
                bf = i % 3
                if i + 2 < NB:
                    fetch(i + 2)
                for j in range(3):
                    e_ = cast_eng[j]
                    if e_ == 'act':
                        op('act', lambda j=j, b=b: A.activation(out=wb[j][b][:], in_=wf[j][bf], func=AF.Copy),
                           r=('wf%d_%d' % (j, bf),), w=('wb%d_%d' % (j, b),))
                    else:
                        op(e_, lambda j=j, b=b, e_=e_: P.E[e_].tensor_copy(out=wb[j][b][:], in_=wf[j][bf]),
                           r=('wf%d_%d' % (j, bf),), w=('wb%d_%d' % (j, b),))
                xv = xs[bf][:].rearrange("p (q c) -> p c q", c=8)
                for c in range(8):
                    op('pe', lambda c=c: T.transpose(out=pX[:, c * 128:(c + 1) * 128], in_=xv[:, c, :], identity=identb[:]),
                       r=('xs%d' % bf, 'identb'), w=('pX6',))
                op('dve', lambda: V.tensor_copy(out=xT[:], in_=pX[:].rearrange("p (c t) -> p c t", c=8)), r=('pX6',), w=('xT',))
                pg_ = pG[b]; pgk = 'pG6_%d' % b
                for wi in range(2):
                    wv_ = wb[wi][b][:].rearrange("p (c f) -> p c f", c=8)
                    for fc in range(2):
                        c0 = (wi * 2 + fc) * 128
                        for c in range(8):
                            op('pe', lambda c=c, fc=fc, wv_=wv_, c0=c0: T.matmul(
                                pg_[:, c0:c0 + 128], lhsT=wv_[:, c, :].rearrange("p (m two) -> p two m", two=2)[:, fc, :],
                                rhs=xT[:, c, :], start=(c == 0), stop=(c == 7)),
                               r=('xT', 'wb%d_%d' % (wi, b)), w=(pgk,))
                op('act', lambda: A.activation(out=sil6[:], in_=pg_[:, 0:256], func=AF.Silu), r=(pgk,), w=('sil6',))
                op('dve', lambda: V.tensor_tensor(out=act6[:].rearrange("p a t -> p (a t)"), in0=sil6[:], in1=pg_[:, 256:512], op=ALU.mult),
                   r=('sil6', pgk), w=('act6',))
                w2v = wb[2][b][:].rearrange("p (c n) -> p c n", c=2)
                py_ = pY[b]; pyk = 'pY6_%d' % b
                for nh in range(2):
                    for fc in range(2):
                        op('pe', lambda nh=nh, fc=fc: T.matmul(py_[:, nh * 512:(nh + 1) * 512], lhsT=act6[:, fc, :],
                                                               rhs=w2v[:, fc, nh * 512:(nh + 1) * 512], start=(fc == 0), stop=(fc == 1)),
                           r=('act6', 'wb2_%d' % b), w=(pyk,))
                op('act', lambda: A.activation(out=ysb[b][:, 0:512], in_=py_[:, 0:512], func=AF.Copy), r=(pyk,), w=('ysb%d' % b,))
                op('dve', lambda: V.tensor_copy(out=ysb[b][:, 512:1024], in_=py_[:, 512:1024]), r=(pyk,), w=('ysb%d' % b,))
                op('sp', lambda i=i: SP.dma_start(out=YS_s[i * 128:(i + 1) * 128, :], in_=ysb[b][:]), r=('ysb%d' % b,), w=('YS_s',), dma='d_ys%d' % b)
            P.barrier()

        if STOP < 7:
            return
        with ExitStack() as p7:
            yg = [sb("yg%d" % i, [128, D], st=p7) for i in range(3)]
            acc = sb("acc7", [128, D], st=p7)
            base = [sb("base%d" % i, [128, D], st=p7) for i in range(2)]
            ob = [sb("ob%d" % i, [128, D], st=p7) for i in range(2)]
            ng = 0
            for p in range(NP):
                b = p % 2
                tsl = slice(p * 128, (p + 1) * 128)
                load(base[b][:], ACC_s[tsl, :], 'base%d' % b, 'd_base%d' % b)
                for k in range(8):
                    gi = ng % 3; ng += 1
                    op('pool', lambda p=p, k=k, gi=gi: G.indirect_dma_start(
                        out=yg[gi][:], out_offset=None, in_=YS_s, in_offset=bass.IndirectOffsetOnAxis(DESTI[:, p, k:k + 1], 0)),
                       r=('DESTI', 'YS_s'), w=('yg%d' % gi,), dma='d_yg%d' % gi)
                    if k == 0:
                        op('dve', lambda p=p, k=k, gi=gi: V.tensor_scalar(out=acc[:], in0=yg[gi][:], scalar1=GATE8[:, p, k:k + 1], scalar2=None,
                                                                          op0=ALU.mult), r=('yg%d' % gi, 'GATE8'), w=('acc7',))
                    else:
                        op('dve', lambda p=p, k=k, gi=gi: V.scalar_tensor_tensor(out=acc[:], in0=yg[gi][:], scalar=GATE8[:, p, k:k + 1], in1=acc[:],
                                                                                 op0=ALU.mult, op1=ALU.add), r=('yg%d' % gi, 'GATE8', 'acc7'), w=('acc7',))
                op('dve', lambda: V.tensor_tensor(out=ob[b][:], in0=acc[:], in1=GATE_F, op=ALU.mult), r=('acc7', 'mod'), w=('ob%d' % b,))
                op('pool', lambda: G.tensor_tensor(out=ob[b][:], in0=ob[b][:], in1=base[b][:], op=ALU.add), r=('ob%d' % b, 'base%d' % b), w=('ob%d' % b,))
                op('sp', lambda: SP.dma_start(out=out[tsl, :], in_=ob[b][:]), r=('ob%d' % b,), w=('out',), dma='d_out%d' % b)
            P.barrier()


def _consts(S, half):
    NBLK = S // 128
    TOWN = S // 2
    NB = TOWN * 8 // 128 + 256
    bf = ml_dtypes.bfloat16
    c = {}
    c['c_identb'] = np.eye(128, dtype=np.float32).astype(bf)
    c['c_identf'] = np.eye(128, dtype=np.float32)
    u = np.arange(128)
    c['c_tri'] = (u[:, None] <= u[None, :]).astype(np.float32)
    c['c_tristrict'] = (u[:, None] < u[None, :]).astype(np.float32).astype(bf)
    c['c_maskml'] = ((u[:, None] <= u[None, :]).astype(np.float32) * (128.0 ** -0.5)).astype(np.float32)
    causal = (u[:, None] <= u[None, :]).astype(np.float32)
    mk = np.zeros((128, 2, 128), np.float32)
    if half == 0:
        mk[:, 1, :] = causal; mk[:, 0, :] = 0.0
    else:
        mk[:, 1, :] = 1.0; mk[:, 0, :] = causal
    c['c_masku'] = mk.astype(bf)
    slopes = 2.0 ** (-8.0 * np.arange(1, 5) / 4.0)
    uu = np.arange(NBLK + 1)
    delta = (uu - 1) if half == 0 else uu
    tab = slopes[None, :, None] * u[:, None, None] - 128.0 * slopes[None, :, None] * delta[None, None, :]
    c['c_alibi'] = tab.astype(np.float32)
    s = np.zeros((128, 2), np.float32); s[:, half] = 1.0
    c['c_sel'] = s
    c['c_pidx'] = u.astype(np.float32)[:, None].copy()
    c['c_iota'] = np.tile(np.arange(512, dtype=np.float32)[None, :], (128, 1))
    c['c_thr'] = np.tile((128.0 * np.arange(32, dtype=np.float32))[None, :], (128, 1))
    return c


def _rep(v, n=128):
    return np.ascontiguousarray(np.broadcast_to(np.asarray(v, np.float32).reshape(1, -1), (n, np.asarray(v).size)))


def make_in_maps(inp, S, B):
    f = np.float32
    maps = []
    shared = {}
    shared['w_ada'] = np.ascontiguousarray(inp['w_ada'][0], f)
    shared['b_ada'] = _rep(inp['b_ada'][0])
    shared['w_in'] = np.ascontiguousarray(inp['w_in'][0], f)
    cw = np.asarray(inp['conv_w'][0], f)
    shared['convw'] = np.ascontiguousarray(cw.reshape(4, 8, 128).transpose(2, 1, 0))
    shared['convb'] = np.ascontiguousarray(np.asarray(inp['conv_b'][0], f).reshape(8, 128).T)
    shared['gate_b'] = _rep(inp['gate_b'][0])
    shared['mlg'] = _rep(np.tile(np.asarray(inp['ml_norm_g'][0], f), 4))
    shared['qg'] = _rep(inp['da_q_norm_g'][0])
    shared['kg'] = _rep(inp['da_k_norm_g'][0])
    lv = np.stack([inp['lambda_q1'][0], inp['lambda_k1'][0], inp['lambda_q2'][0], inp['lambda_k2'][0]]).astype(f)
    shared['lamv'] = np.ascontiguousarray(np.broadcast_to(lv[None], (128, 4, 64)))
    shared['dag'] = _rep(inp['da_norm_g'][0])
    shared['w_out'] = np.ascontiguousarray(inp['w_out'][0], f)
    shared['w_router'] = np.ascontiguousarray(inp['w_router'][0], f)
    shared['rbias'] = _rep(inp['router_bias'][0])
    shared['wexp'] = np.concatenate([np.asarray(inp['w1'][0], f).reshape(NE * 128, 2048),
                                     np.asarray(inp['w3'][0], f).reshape(NE * 128, 2048),
                                     np.asarray(inp['w2'][0], f).reshape(NE * 128, 2048)], axis=1)
    shared['ws1'] = np.ascontiguousarray(inp['ws1'][0], f)
    shared['ws3'] = np.ascontiguousarray(inp['ws3'][0], f)
    shared['ws2'] = np.ascontiguousarray(inp['ws2'][0], f)
    consts = [_consts(S, 0), _consts(S, 1)]
    xall = np.asarray(inp['x'], f)
    call = np.asarray(inp['c'], f)
    for b in range(B):
        for half in range(2):
            m = dict(shared)
            m.update(consts[half])
            m['x'] = np.ascontiguousarray(xall[b])
            m['xo'] = np.ascontiguousarray(xall[b].reshape(S // 256, 2, 128, D)[:, half].reshape(S // 2, D))
            m['ccol'] = np.ascontiguousarray(call[b].reshape(8, 128).T)
            maps.append(m)
    return maps


_NC_CACHE = {}


def run(inp, S, B, dbg=False):
    key = (S, dbg)
    if key not in _NC_CACHE:
        _NC_CACHE[key] = build_nc(S, dbg)
    nc = _NC_CACHE[key]
    maps = make_in_maps(inp, S, B)
    res = run_bass_kernel_spmd(nc, maps, core_ids=list(range(2 * B)))
    outf = np.zeros((B, S, D), np.float32)
    for b in range(B):
        for half in range(2):
            r = res.results[b * 2 + half]["out"]
            outf[b].reshape(S // 256, 2, 128, D)[:, half] = r.reshape(S // 256, 128, D)
    return outf, res


def kernel(**inputs):
    x = np.asarray(inputs['x'])
    B, S, _ = x.shape
    outf, _ = run(inputs, S, B, dbg=False)
    return outf
```

```python
import numpy as np
import ml_dtypes
from contextlib import ExitStack
import concourse.bass as bass
import concourse.mybir as mybir
from concourse.bass_utils import run_bass_kernel_spmd

F32 = mybir.dt.float32
BF16 = mybir.dt.bfloat16
I32 = mybir.dt.int32
ALU = mybir.AluOpType
AF = mybir.ActivationFunctionType
AX = mybir.AxisListType

D = 1024
DIN = 3592
NE = 256
EPS = 1e-6
SAME_ENGINE_RAW_WAIT = True
STOP = 99


class Prog:
    def __init__(self, nc, es):
        self.nc = nc
        self.es = es
        self.E = {'pe': nc.tensor, 'act': nc.scalar, 'dve': nc.vector, 'pool': nc.gpsimd, 'sp': nc.sync}
        self.sems = {}
        self.val = {}
        for e in self.E:
            self._sem(e)
        self.known = {e: {} for e in self.E}
        self.lastw = {}
        self.readers = {}
        self.n = 0

    def _sem(self, name):
        if name not in self.sems:
            self.sems[name] = self.es.enter_context(self.nc.semaphore("s_" + name))
            self.val[name] = 0
        return self.sems[name]

    def _wait(self, eng, ev):
        sn, v, snap = ev
        kn = self.known[eng]
        if kn.get(sn, 0) >= v:
            return
        self.E[eng].wait_ge(self.sems[sn], v)
        kn[sn] = v
        for k, vv in snap.items():
            if kn.get(k, 0) < vv:
                kn[k] = vv

    def op(self, eng, fn, r=(), w=(), dma=None):
        self.n += 1
        for res in r:
            ev = self.lastw.get(res)
            if ev is not None:
                if ev[0] == eng and not SAME_ENGINE_RAW_WAIT:
                    continue
                if ev[0] == 'pe' and eng == 'pe':
                    continue
                self._wait(eng, ev)
        for res in w:
            ev = self.lastw.get(res)
            if ev is not None and ev[0] != eng:
                self._wait(eng, ev)
            for sn, ev2 in self.readers.get(res, {}).items():
                if sn != eng:
                    self._wait(eng, ev2)
        ins = fn()
        kn = self.known[eng]
        if dma is None:
            self.val[eng] += 1
            ins.then_inc(self.sems[eng], 1)
            ev = (eng, self.val[eng], dict(kn))
        else:
            self._sem(dma)
            self.val[dma] += 16
            ins.then_inc(self.sems[dma], 16)
            ev = (dma, self.val[dma], dict(kn))
        for res in w:
            self.lastw[res] = ev
            self.readers[res] = {}
        for res in r:
            self.readers.setdefault(res, {})[ev[0]] = ev
        return ev

    def barrier(self):
        for e in self.E:
            for sn, v in self.val.items():
                if v > 0 and sn != e:
                    self._wait(e, (sn, v, {}))
            if self.val[e] > 0:
                self._wait(e, (e, self.val[e], {}))
        self.lastw = {}
        self.readers = {}

    def finish(self):
        self.barrier()


def build_nc(S, dbg=False):
    NBLK = S // 128
    NP = NBLK // 2
    TOWN = S // 2
    NSB = S // 512
    NB = TOWN * 8 // 128 + 256
    assert NB <= 512
    nc = bass.Bass("TRN2", target_bir_lowering=False)

    def din(name, shape, dt=F32):
        return nc.dram_tensor(name, list(shape), dt, kind="ExternalInput").ap()

    def dscr(name, shape, dt):
        return nc.dram_tensor(name, list(shape), dt).ap()

    x = din("x", [S, D])
    xo = din("xo", [TOWN, D])
    ccol = din("ccol", [128, 8])
    w_ada = din("w_ada", [D, 6 * D])
    b_ada = din("b_ada", [128, 6 * D])
    w_in = din("w_in", [D, DIN])
    convw = din("convw", [128, 8, 4])
    convb = din("convb", [128, 8])
    gate_b = din("gate_b", [128, 8])
    mlg = din("mlg", [128, 512])
    qg = din("qg", [128, 64])
    kg = din("kg", [128, 64])
    lamv = din("lamv", [128, 4, 64])
    dag = din("dag", [128, 128])
    w_out = din("w_out", [D, D])
    w_router = din("w_router", [D, NE])
    rbias = din("rbias", [128, NE])
    wexp = din("wexp", [NE * 128, 6144])
    ws1 = din("ws1", [D, 256])
    ws3 = din("ws3", [D, 256])
    ws2 = din("ws2", [256, D])
    c_identb = din("c_identb", [128, 128], BF16)
    c_identf = din("c_identf", [128, 128])
    c_tri = din("c_tri", [128, 128])
    c_tristrict = din("c_tristrict", [128, 128], BF16)
    c_maskml = din("c_maskml", [128, 128])
    c_masku = din("c_masku", [128, 2, 128], BF16)
    c_alibi = din("c_alibi", [128, 4, NBLK + 1])
    c_alibi2 = din("c_alibi2", [128, 4, NBLK + 1])
    c_sel = din("c_sel", [128, 2])
    c_pidx = din("c_pidx", [128, 1])
    c_iota = din("c_iota", [128, 512])
    c_thr = din("c_thr", [128, 32])
    out = nc.dram_tensor("out", [TOWN, D], F32, kind="ExternalOutput").ap()

    KT_s = dscr("KT_s", [4, 128, S], BF16)
    QT_s = dscr("QT_s", [4, 128, TOWN], BF16)
    V_s = dscr("V_s", [4, 128, NBLK, 129], BF16)
    MIX_s = dscr("MIX_s", [128, 8, TOWN], BF16)
    H2_s = dscr("H2_s", [TOWN, D], BF16)
    ACC_s = dscr("ACC_s", [TOWN, D], F32)
    XS_s = dscr("XS_s", [NB * 128, D], BF16)
    YS_s = dscr("YS_s", [NB * 128, D], F32)
    dbgs = {}
    if dbg:
        dbgs['mod'] = nc.dram_tensor("d_mod", [128, 6 * D], F32, kind="ExternalOutput").ap()
        dbgs['mix'] = nc.dram_tensor("d_mix", [128, 8, TOWN], BF16, kind="ExternalOutput").ap()
        dbgs['acc'] = nc.dram_tensor("d_acc", [TOWN, D], F32, kind="ExternalOutput").ap()
        dbgs['h2'] = nc.dram_tensor("d_h2", [TOWN, D], BF16, kind="ExternalOutput").ap()
        dbgs['gd'] = nc.dram_tensor("d_gd", [128, NP, NE], F32, kind="ExternalOutput").ap()

    with ExitStack() as es:
        P = Prog(nc, es)
        op = P.op

        def sb(name, shape, dt=F32, st=es):
            return st.enter_context(nc.sbuf_tensor(name, list(shape), dt))

        def ps(name, shape, dt=F32, st=es):
            return st.enter_context(nc.psum_tensor(name, list(shape), dt))

        V = nc.vector
        A = nc.scalar
        G = nc.gpsimd
        T = nc.tensor
        SP = nc.sync

        def load(dst, src, key, dsem, eng='sp'):
            return op(eng, lambda: P.E[eng].dma_start(out=dst, in_=src), r=(), w=(key,), dma=dsem)

        identb = sb("identb", [128, 128], BF16)
        identf = sb("identf", [128, 128])
        tri = sb("tri", [128, 128])
        tristrict = sb("tristrict", [128, 128], BF16)
        onesf = sb("onesf", [128, 128])
        onesb = sb("onesb", [128, 128], BF16)
        maskml = sb("maskml", [128, 128])
        masku = sb("masku", [128, 2, 128], BF16)
        alibi = sb("alibi", [128, 4, NBLK + 1])
        alibi2 = sb("alibi2", [128, 4, NBLK + 1])
        sel = sb("sel", [128, 2])
        pidx = sb("pidx", [128, 1])
        iota = sb("iota", [128, 512])
        thr = sb("thr", [128, 32])
        mod = sb("mod", [128, 6 * D])
        lam = sb("lam", [128, 4])
        dagb = sb("dagb", [128, 128])
        for t_, s_, k_ in ((identb, c_identb, 'identb'), (identf, c_identf, 'identf'), (tri, c_tri, 'tri'),
                           (tristrict, c_tristrict, 'tristrict'), (maskml, c_maskml, 'maskml'),
                           (masku, c_masku, 'masku'), (alibi, c_alibi, 'alibi'), (alibi2, c_alibi2, 'alibi'), (sel, c_sel, 'sel'),
                           (pidx, c_pidx, 'pidx'), (iota, c_iota, 'iota'), (thr, c_thr, 'thr'),
                           (dagb, dag, 'dagb')):
            load(t_[:], s_, k_, 'd_const')
        op('dve', lambda: V.memset(onesf[:], 1.0), w=('onesf',))
        op('dve', lambda: V.memset(onesb[:], 1.0), w=('onesb',))

        with ExitStack() as p0:
            cc = sb("cc", [128, 8], st=p0)
            sc = sb("sc", [128, 8], st=p0)
            scb = sb("scb", [128, 8, 128], st=p0)
            bada = sb("bada", [128, 6 * D], st=p0)
            wst = [sb("wst%d" % i, [128, 8, 512], st=p0) for i in range(2)]
            lv = sb("lv", [128, 4, 64], st=p0)
            lt = sb("lt", [128, 2, 64], st=p0)
            ls = sb("ls", [128, 2], st=p0)
            psm = [ps("psm%d" % i, [128, 512], st=p0) for i in range(2)]
            load(cc[:], ccol, 'cc', 'd_c0')
            load(bada[:], b_ada, 'bada', 'd_c0')
            load(lv[:], lamv, 'lv', 'd_c0')
            op('act', lambda: A.activation(out=sc[:], in_=cc[:], func=AF.Silu), r=('cc',), w=('sc',))
            for k in range(8):
                op('dve', lambda k=k: V.tensor_copy(out=scb[:, k, :], in_=sc[:, k:k + 1].to_broadcast([128, 128])),
                   r=('sc',), w=('scb',))
            wv = w_ada.rearrange("(k p) n -> p k n", p=128)
            for g in range(12):
                b = g % 2
                load(wst[b][:], wv[:, :, g * 512:(g + 1) * 512], 'wst%d' % b, 'd_wst%d' % b)
                for k in range(8):
                    op('pe', lambda k=k, b=b: T.matmul(psm[b][:], lhsT=scb[:, k, :], rhs=wst[b][:, k, :],
                                                       start=(k == 0), stop=(k == 7)),
                       r=('scb', 'wst%d' % b), w=('psm%d' % b,))
                op('dve', lambda g=g, b=b: V.tensor_tensor(out=mod[:, g * 512:(g + 1) * 512], in0=psm[b][:],
                                                           in1=bada[:, g * 512:(g + 1) * 512], op=ALU.add),
                   r=('psm%d' % b, 'bada'), w=('mod',))
            for c0 in (1024, 4096):
                op('dve', lambda c0=c0: V.tensor_scalar(out=mod[:, c0:c0 + 1024], in0=mod[:, c0:c0 + 1024],
                                                        scalar1=1.0, scalar2=None, op0=ALU.add),
                   r=('mod',), w=('mod',))
            op('dve', lambda: V.tensor_tensor(out=lt[:, 0, :], in0=lv[:, 0, :], in1=lv[:, 1, :], op=ALU.mult),
               r=('lv',), w=('lt',))
            op('dve', lambda: V.tensor_tensor(out=lt[:, 1, :], in0=lv[:, 2, :], in1=lv[:, 3, :], op=ALU.mult),
               r=('lv',), w=('lt',))
            op('dve', lambda: V.tensor_reduce(out=ls[:], in_=lt[:], axis=AX.X, op=ALU.add), r=('lt',), w=('ls',))
            op('act', lambda: A.activation(out=ls[:], in_=ls[:], func=AF.Exp), r=('ls',), w=('ls',))
            op('dve', lambda: V.tensor_tensor(out=lam[:, 0:1], in0=ls[:, 0:1], in1=ls[:, 1:2], op=ALU.subtract),
               r=('ls',), w=('lam',))
            op('dve', lambda: V.tensor_scalar(out=lam[:, 0:1], in0=lam[:, 0:1], scalar1=0.2, scalar2=None,
                                              op0=ALU.add), r=('lam',), w=('lam',))
            op('dve', lambda: V.tensor_scalar(out=lam[:, 1:2], in0=lam[:, 0:1], scalar1=-1.0, scalar2=None,
                                              op0=ALU.mult), r=('lam',), w=('lam',))
            op('dve', lambda: V.tensor_scalar(out=dagb[:], in0=dagb[:], scalar1=0.8, scalar2=None, op0=ALU.mult),
               r=('dagb',), w=('dagb',))
            if dbg:
                op('sp', lambda: SP.dma_start(out=dbgs['mod'], in_=mod[:]), r=('mod',), w=('dbgmod',), dma='d_dbg')
            P.barrier()

        if STOP >= 1:
            PHASES(nc, P, locals())
        P.finish()
    return nc


def PHASES(nc, P, L):
    es = L['es']; op = P.op; sb = L['sb']; ps = L['ps']; load = L['load']
    V = nc.vector; A = nc.scalar; G = nc.gpsimd; T = nc.tensor; SP = nc.sync
    S = L['S']; NBLK = L['NBLK']; NP = L['NP']; TOWN = L['TOWN']; NSB = L['NSB']; NB = L['NB']
    dbg = L['dbg']; dbgs = L['dbgs']
    identb, identf, tri, tristrict, onesf, onesb = L['identb'], L['identf'], L['tri'], L['tristrict'], L['onesf'], L['onesb']
    alibi2 = L['alibi2']
    maskml, masku, alibi, sel, pidx, iota, thr, mod, lam, dagb = (L['maskml'], L['masku'], L['alibi'], L['sel'],
                                                                  L['pidx'], L['iota'], L['thr'], L['mod'], L['lam'], L['dagb'])
    x, xo, w_in, out = L['x'], L['xo'], L['w_in'], L['out']
    KT_s, QT_s, V_s, MIX_s, H2_s, ACC_s, XS_s, YS_s = (L['KT_s'], L['QT_s'], L['V_s'], L['MIX_s'], L['H2_s'],
                                                       L['ACC_s'], L['XS_s'], L['YS_s'])
    SHIFT_A, SCALE_A, GATE_A, SHIFT_F, SCALE_F, GATE_F = [mod[:, i * D:(i + 1) * D] for i in range(6)]

    def rstd_from_ss(ssap, n, outap, rkey, wkey, eng='dve'):
        op(eng, lambda: V.tensor_scalar(out=outap, in0=ssap, scalar1=1.0 / n, scalar2=EPS, op0=ALU.mult, op1=ALU.add),
           r=(rkey,), w=(wkey,))
        op('act', lambda: A.activation(out=outap, in_=outap, func=AF.Sqrt), r=(wkey,), w=(wkey,))
        op(eng, lambda: V.reciprocal(out=outap, in_=outap), r=(wkey,), w=(wkey,))

    with ExitStack() as p1:
        winb = sb("winb", [128, 8, DIN], BF16, st=p1)
        with ExitStack() as p1w:
            wstg = [sb("wstg%d" % i, [128, DIN], st=p1w) for i in range(2)]
            for k in range(8):
                b = k % 2
                load(wstg[b][:], w_in[k * 128:(k + 1) * 128, :], 'wstg%d' % b, 'd_wstg%d' % b)
                e_ = 'dve' if b == 0 else 'pool'
                op(e_, lambda k=k, b=b, e_=e_: P.E[e_].tensor_copy(out=winb[:, k, :], in_=wstg[b][:]),
                   r=('wstg%d' % b,), w=('winb',))
            P.barrier()
        cw = sb("cw", [128, 8, 4], st=p1); cb = sb("cb", [128, 8], st=p1); gb = sb("gb", [128, 8], st=p1)
        mlgb = sb("mlgb", [128, 512], st=p1); qgb = sb("qgb", [128, 64], st=p1); kgb = sb("kgb", [128, 64], st=p1)
        load(cw[:], L['convw'], 'cw', 'd_c1'); load(cb[:], L['convb'], 'cb', 'd_c1'); load(gb[:], L['gate_b'], 'gb', 'd_c1')
        load(mlgb[:], L['mlg'], 'mlgb', 'd_c1'); load(qgb[:], L['qg'], 'qgb', 'd_c1'); load(kgb[:], L['kg'], 'kgb', 'd_c1')
        op('dve', lambda: V.tensor_scalar(out=qgb[:], in0=qgb[:], scalar1=0.125, scalar2=None, op0=ALU.mult),
           r=('qgb',), w=('qgb',))
        xb = [sb("xb%d" % i, [128, D], st=p1) for i in range(2)]
        junk = sb("junk", [128, D], BF16, st=p1)
        ss = sb("ss", [128, 8], st=p1)
        hn = sb("hn", [128, D], st=p1)
        hb = sb("hb", [128, D], BF16, st=p1)
        hT = sb("hT", [128, 8, 512], BF16, st=p1)
        raw = sb("raw", [128, 8, 516], st=p1)
        cacc = sb("cacc", [128, 512], st=p1)
        qkT_all = sb("qkT", [128, 2, 8, 512], BF16, st=p1)
        vaug_all = sb("vaug", [128, 2, 4, 4 * 129], BF16, st=p1)
        gsig_all = sb("gsig", [128, 2, 4, 512], st=p1)
        gcol_all = sb("gcol", [128, 2, 4, 8], st=p1)
        lf = sb("lf", [128, 4, 4], st=p1)
        gg = sb("gg", [128, 16], st=p1); eg = sb("eg", [128, 16], st=p1); eb = sb("eb", [128, 16], st=p1)
        wk = sb("wk", [128, 16], st=p1); carry = sb("carry", [128, 16], st=p1)
        tq = sb("tq", [128, 512], st=p1); tq2 = sb("tq2", [128, 512], st=p1)
        qn = sb("qn", [128, 512], BF16, st=p1); kn = sb("kn", [128, 512], BF16, st=p1)
        ss8 = sb("ss8", [128, 16], st=p1)
        qTp = sb("qTp", [128, 2, 4, 128], BF16, st=p1)
        qTo = sb("qTo", [128, 4, 128], BF16, st=p1)
        qTt = sb("qTt", [128, 4, 128], st=p1)
        kTs = sb("kTs", [128, 4, 128], BF16, st=p1)
        vda = sb("vda", [128, 4, 129], BF16, st=p1)
        Cst = sb("Cst", [128, 4, 129], st=p1)
        Cbf = sb("Cbf", [128, 4, 129], BF16, st=p1)
        sTb2 = sb("sTb", [128, 4, 128], BF16, st=p1)
        kwb2 = sb("kwb", [128, 4, 128], BF16, st=p1)
        sm2 = sb("sm", [128, 4, 8], st=p1)
        hbuf2 = sb("hbuf", [128, 4, 128], st=p1)
        junkB = sb("junkB", [128, 4, 128], BF16, st=p1)
        hm = sb("hm", [128, 2, 512], st=p1)
        hmo = sb("hmo", [128, 512], st=p1)
        hmb = sb("hmb", [128, 512], BF16, st=p1)
        hmT = sb("hmT", [128, 4, 128], BF16, st=p1)
        pT = ps("pT", [128, 1024], BF16, st=p1)
        pfm = [ps("pfm%d" % i, [128, 512], st=p1) for i in range(2)]
        ptm = [ps("ptm%d" % i, [128, 512], st=p1) for i in range(2)]
        pml = ps("pml", [128, 512], st=p1)
        pn = ps("pn", [128, 512], st=p1)
        pc = ps("pc", [128, 512], st=p1)
        op('dve', lambda: V.memset(raw[:], 0.0), w=tuple('raw%d' % g for g in range(8)))
        op('dve', lambda: V.memset(Cst[:], 0.0), w=tuple('Cst%d' % g for g in range(4)))
        op('dve', lambda: V.memset(Cbf[:], 0.0), w=tuple('Cbf%d' % g for g in range(4)))
        op('dve', lambda: V.memset(vaug_all[:], 1.0), w=tuple('vaug%d_%d' % (g, q) for g in range(4) for q in range(2)))
        op('dve', lambda: V.memset(vda[:], 1.0), w=('vda',))
        nxb = 0

        def stageA(sbi):
            nonlocal nxb
            bp = sbi % 2; kp = '_%d' % bp
            qkT = qkT_all[:, bp]; gsig = gsig_all[:, bp]; gcol = gcol_all[:, bp]
            vaug = vaug_all[:, bp].rearrange("p a (h e) -> p a h e", h=4)
            for bi in range(4):
                blk = sbi * 4 + bi
                xt = xb[nxb % 2]; xk = 'xb%d' % (nxb % 2); nxb += 1
                load(xt[:], x[blk * 128:(blk + 1) * 128, :], xk, 'd_' + xk)
                op('act', lambda xt=xt: A.activation(out=junk[:], in_=xt[:], func=AF.Square, accum_out=ss[:, 0:1]),
                   r=(xk,), w=('junk', 'ss'))
                rstd_from_ss(ss[:, 0:1], D, ss[:, 1:2], 'ss', 'ss1')
                op('dve', lambda xt=xt: V.scalar_tensor_tensor(out=hn[:], in0=xt[:], scalar=ss[:, 1:2], in1=SCALE_A,
                                                               op0=ALU.mult, op1=ALU.mult), r=(xk, 'ss1', 'mod'), w=('hn',))
                op('pool', lambda: G.tensor_tensor(out=hb[:], in0=hn[:], in1=SHIFT_A, op=ALU.add), r=('hn', 'mod'), w=('hb',))
                yield
                for hf in range(2):
                    for k in range(4):
                        op('pe', lambda k=k, hf=hf: T.transpose(out=pT[:, k * 128:(k + 1) * 128],
                                                                in_=hb[:, (hf * 4 + k) * 128:(hf * 4 + k + 1) * 128],
                                                                identity=identb[:]), r=('hb', 'identb'), w=('pT',))
                    op('act', lambda bi=bi, hf=hf: A.activation(out=hT[:, hf * 4:(hf + 1) * 4, bi * 128:(bi + 1) * 128],
                                                                in_=pT[:, 0:512].rearrange("p (k t) -> p k t", k=4), func=AF.Copy),
                       r=('pT',), w=('hT',))
                    yield
            for g in range(8):
                pf = pfm[0]; pk = 'pfm0'
                for k in range(8):
                    op('pe', lambda g=g, k=k, pf=pf: T.matmul(pf[:], lhsT=winb[:, k, g * 128:(g + 1) * 128], rhs=hT[:, k, :],
                                                              start=(k == 0), stop=(k == 7)), r=('winb', 'hT'), w=(pk,))
                op('act', lambda g=g, pf=pf: A.activation(out=raw[:, g, 3:515], in_=pf[:], func=AF.Copy), r=(pk,), w=('raw%d' % g,))
                yield
                op('dve', lambda g=g: V.tensor_scalar(out=cacc[:], in0=raw[:, g, 3:515], scalar1=cw[:, g, 3:4],
                                                      scalar2=cb[:, g:g + 1], op0=ALU.mult, op1=ALU.add),
                   r=('raw%d' % g, 'cw', 'cb'), w=('cacc',))
                for j in range(3):
                    op('dve', lambda g=g, j=j: V.scalar_tensor_tensor(out=cacc[:], in0=raw[:, g, j:j + 512],
                                                                      scalar=cw[:, g, j:j + 1], in1=cacc[:],
                                                                      op0=ALU.mult, op1=ALU.add),
                       r=('raw%d' % g, 'cw', 'cacc'), w=('cacc',))
                yield
                op('act', lambda g=g: A.activation(out=qkT[:, g, :], in_=cacc[:], func=AF.Silu), r=('cacc',), w=('qkT%d' % g + kp,))
                op('pool', lambda g=g: G.tensor_copy(out=raw[:, g, 0:3], in_=raw[:, g, 512:515]), r=('raw%d' % g,), w=('raw%d' % g,))
                yield
            groups = [(1024, 512), (1536, 512), (2048, 8), (2056, 512), (2568, 512), (3080, 512)]
            npt = 0
            for bi in range(4):
                blk = sbi * 4 + bi
                par = blk % 2
                pair = blk // 2

                def proj(gi):
                    nonlocal npt
                    c0, wd = groups[gi]
                    pt_ = ptm[npt % 2]; pk_ = 'ptm%d' % (npt % 2); npt += 1
                    for k in range(8):
                        op('pe', lambda k=k: T.matmul(pt_[:, 0:wd], lhsT=hT[:, k, bi * 128:(bi + 1) * 128],
                                                      rhs=winb[:, k, c0:c0 + wd], start=(k == 0), stop=(k == 7)),
                           r=('hT', 'winb'), w=(pk_,))
                    return pt_, pk_
                pt_, pk_ = proj(0)
                op('act', lambda pt_=pt_: A.activation(out=vaug[:, bi, :, 0:128], in_=pt_[:].rearrange("p (h e) -> p h e", h=4),
                                                       func=AF.Copy), r=(pk_,), w=('vaug%d' % bi + kp,))
                yield
                pt_, pk_ = proj(1)
                op('act', lambda pt_=pt_: A.activation(out=gsig[:, bi, :], in_=pt_[:], func=AF.Sigmoid), r=(pk_,), w=('gsig%d' % bi + kp,))
                op('pool', lambda: G.tensor_tensor(out=gsig[:, bi, :], in0=gsig[:, bi, :], in1=mlgb[:], op=ALU.mult),
                   r=('gsig%d' % bi + kp, 'mlgb'), w=('gsig%d' % bi + kp,))
                yield
                pt_, pk_ = proj(2)
                op('dve', lambda pt_=pt_: V.tensor_tensor(out=gcol[:, bi, :], in0=pt_[:, 0:8], in1=gb[:], op=ALU.add),
                   r=(pk_, 'gb'), w=('gcol' + kp,))
                for which, gi, gn, dst in (('q', 3, qgb, qn), ('k', 4, kgb, kn)):
                    yield
                    pt_, pk_ = proj(gi)
                    so = 0 if which == 'q' else 8
                    op('act', lambda pt_=pt_: A.activation(out=tq[:], in_=pt_[:], func=AF.Square), r=(pk_,), w=('tq',))
                    op('dve', lambda so=so: V.tensor_reduce(out=ss8[:, so:so + 8], in_=tq[:].rearrange("p (a d) -> p a d", d=64),
                                                            axis=AX.X, op=ALU.add), r=('tq',), w=('ss8',))
                    rstd_from_ss(ss8[:, so:so + 8], 64, ss8[:, so:so + 8], 'ss8', 'ss8')
                    yield
                    op('dve', lambda pt_=pt_, so=so: V.tensor_tensor(
                        out=tq2[:].rearrange("p (a d) -> p a d", d=64), in0=pt_[:].rearrange("p (a d) -> p a d", d=64),
                        in1=ss8[:, so:so + 8].rearrange("p (a o) -> p a o", o=1).to_broadcast([128, 8, 64]), op=ALU.mult),
                       r=(pk_, 'ss8'), w=('tq2',))
                    op('pool', lambda gn=gn, dst=dst: G.tensor_tensor(
                        out=dst[:].rearrange("p (a d) -> p a d", d=64), in0=tq2[:].rearrange("p (a d) -> p a d", d=64),
                        in1=gn[:].rearrange("p (o d) -> p o d", o=1).to_broadcast([128, 8, 64]), op=ALU.mult),
                       r=('tq2', 'qgb', 'kgb'), w=(which + 'n',))
                    yield
                    for h in range(4):
                        op('pe', lambda h=h, dst=dst: T.transpose(out=pT[:, h * 128:(h + 1) * 128], in_=dst[:, h * 128:(h + 1) * 128],
                                                                  identity=identb[:]), r=(which + 'n', 'identb'), w=('pT',))
                    if which == 'q':
                        op('act', lambda: A.activation(out=qTp[:, par, :, :], in_=pT[:, 0:512].rearrange("p (h t) -> p h t", h=4),
                                                       func=AF.Copy), r=('pT',), w=('qTp',))
                    else:
                        op('act', lambda: A.activation(out=kTs[:], in_=pT[:, 0:512].rearrange("p (h t) -> p h t", h=4),
                                                       func=AF.Copy), r=('pT',), w=('kTs',))
                        op('sp', lambda: SP.dma_start(out=KT_s[:, :, blk * 128:(blk + 1) * 128].rearrange("h p t -> p h t"),
                                                      in_=kTs[:]), r=('kTs',), w=('KT_s',), dma='d_kts')
                if par == 1:
                    op('dve', lambda: V.tensor_scalar(out=qTt[:], in0=qTp[:, 0, :, :], scalar1=sel[:, 0:1], scalar2=None,
                                                      op0=ALU.mult), r=('qTp', 'sel'), w=('qTt',))
                    op('dve', lambda: V.scalar_tensor_tensor(out=qTo[:], in0=qTp[:, 1, :, :], scalar=sel[:, 1:2], in1=qTt[:],
                                                             op0=ALU.mult, op1=ALU.add), r=('qTp', 'sel', 'qTt'), w=('qTo',))
                    op('sp', lambda: SP.dma_start(out=QT_s[:, :, pair * 128:(pair + 1) * 128].rearrange("h p t -> p h t"),
                                                  in_=qTo[:]), r=('qTo',), w=('QT_s',), dma='d_qts')
                yield
                pt_, pk_ = proj(5)
                op('act', lambda pt_=pt_: A.activation(out=vda[:, :, 0:128], in_=pt_[:].rearrange("p (h e) -> p h e", h=4),
                                                       func=AF.Copy), r=(pk_,), w=('vda',))
                op('sp', lambda: SP.dma_start(out=V_s[:, :, blk, :].rearrange("h p e -> p h e"), in_=vda[:]),
                   r=('vda',), w=('V_s',), dma='d_vs')
                yield
        def stageB(sbi):
            bp = sbi % 2; kp = '_%d' % bp
            qkT = qkT_all[:, bp]; gsig = gsig_all[:, bp]; gcol = gcol_all[:, bp]
            vaug = vaug_all[:, bp].rearrange("p a (h e) -> p a h e", h=4)
            fpre = gcol[:, :, 4:8]
            op('act', lambda: A.activation(out=lf[:], in_=fpre, func=AF.Exp, scale=-1.0), r=('gcol' + kp,), w=('lf',))
            op('act', lambda: A.activation(out=lf[:], in_=lf[:], func=AF.Ln, bias=1.0), r=('lf',), w=('lf',))
            op('dve', lambda: V.tensor_scalar(out=lf[:], in0=lf[:], scalar1=-1.0, scalar2=None, op0=ALU.mult), r=('lf',), w=('lf',))
            lf2 = lf[:].rearrange("p a h -> p (a h)")
            op('pe', lambda: T.matmul(pn[:, 400:416], lhsT=tri[:], rhs=lf2, start=True, stop=True), r=('tri', 'lf'), w=('bk1',))
            op('pe', lambda: T.matmul(pn[:, 416:432], lhsT=onesf[:], rhs=lf2, start=True, stop=True), r=('onesf', 'lf'), w=('bk1',))
            op('dve', lambda: V.tensor_tensor(out=gg[:].rearrange("p (a h) -> p a h", h=4), in0=gcol[:, :, 0:4],
                                              in1=pn[:, 400:416].rearrange("p (a h) -> p a h", h=4), op=ALU.subtract),
               r=('gcol' + kp, 'bk1'), w=('gg',))
            op('act', lambda: A.activation(out=eg[:], in_=gg[:], func=AF.Exp), r=('gg',), w=('eg',))
            op('act', lambda: A.activation(out=eb[:], in_=pn[:, 400:416], func=AF.Exp), r=('bk1',), w=('eb',))
            op('act', lambda: A.activation(out=carry[:], in_=pn[:, 416:432], func=AF.Exp), r=('bk1',), w=('carry',))
            op('dve', lambda: V.tensor_tensor(out=wk[:], in0=gg[:], in1=pn[:, 416:432], op=ALU.add), r=('gg', 'bk1'), w=('wk',))
            op('act', lambda: A.activation(out=wk[:], in_=wk[:], func=AF.Exp, bias=-2.4260151319598084), r=('wk',), w=('wk',))
            yield
            for bi in range(4):
                blk = sbi * 4 + bi
                par = blk % 2
                pair = blk // 2
                tsl = slice(bi * 128, (bi + 1) * 128)
                def headgen(h, sl):
                    col = bi * 4 + h
                    qT_ = qkT[:, h, tsl]
                    kT_ = qkT[:, 4 + h, tsl]
                    ks = '_s%d' % sl
                    sTb_ = sTb2[:, sl, :]; kwb_ = kwb2[:, sl, :]; sm_ = sm2[:, sl, :]; hbuf_ = hbuf2[:, sl, :]
                    bank_ = (pml, pn, pc, pfm[1])[sl]
                    bk = 'bk%d' % sl
                    pS_ = bank_[:, 0:128]
                    pK_ = pT[:, 512 + sl * 128:512 + (sl + 1) * 128]
                    pn_ = bank_[:, 128:257]
                    pc_ = bank_[:, 260:389]
                    op('pe', lambda: T.matmul(pS_, lhsT=kT_, rhs=qT_, start=True, stop=True),
                       r=('qkT%d' % h + kp, 'qkT%d' % (4 + h) + kp), w=(bk,))
                    op('pe', lambda: T.transpose(out=pK_, in_=kT_, identity=identb[:]), r=('qkT%d' % (4 + h) + kp, 'identb'), w=('pT',))
                    yield
                    op('dve', lambda: V.scalar_tensor_tensor(out=sTb_, in0=pS_, scalar=eg[:, col:col + 1], in1=maskml[:],
                                                             op0=ALU.mult, op1=ALU.mult), r=(bk, 'eg', 'maskml'), w=('sTb' + ks,))
                    op('act', lambda: A.activation(out=kwb_, in_=pK_, func=AF.Copy, scale=wk[:, col:col + 1]),
                       r=('pT', 'wk'), w=('kwb' + ks,))
                    yield
                    op('pe', lambda: T.matmul(pn_, lhsT=sTb_, rhs=vaug[:, bi, h, :], start=True, stop=False),
                       r=('sTb' + ks, 'vaug%d' % bi + kp), w=(bk,))
                    op('pe', lambda: T.matmul(pn_, lhsT=qT_, rhs=Cbf[:, h, :], start=False, stop=True),
                       r=('qkT%d' % h + kp, 'Cbf%d' % h), w=(bk,))
                    op('pe', lambda: T.matmul(pc_, lhsT=kwb_, rhs=vaug[:, bi, h, :], start=True, stop=True),
                       r=('kwb' + ks, 'vaug%d' % bi + kp), w=(bk,))
                    yield
                    op('dve', lambda: V.tensor_scalar(out=sm_[:, 0:1], in0=pn_[:, 128:129], scalar1=eb[:, col:col + 1], scalar2=None,
                                                      op0=ALU.mult), r=(bk, 'eb'), w=('sm' + ks,))
                    op('dve', lambda: V.scalar_tensor_tensor(out=Cst[:, h, :], in0=Cst[:, h, :], scalar=carry[:, col:col + 1],
                                                             in1=pc_, op0=ALU.mult, op1=ALU.add),
                       r=('Cst%d' % h, 'carry', bk), w=('Cst%d' % h,))
                    op('pool', lambda: G.tensor_copy(out=Cbf[:, h, :], in_=Cst[:, h, :]), r=('Cst%d' % h,), w=('Cbf%d' % h,))
                    yield
                    op('dve', lambda: V.scalar_tensor_tensor(out=sm_[:, 1:2], in0=sm_[:, 0:1], scalar=-1.0, in1=sm_[:, 0:1],
                                                             op0=ALU.mult, op1=ALU.max), r=('sm' + ks,), w=('sm' + ks,))
                    yield
                    op('dve', lambda: V.tensor_scalar(out=sm_[:, 1:2], in0=sm_[:, 1:2], scalar1=1.0, scalar2=None,
                                                      op0=ALU.max), r=('sm' + ks,), w=('sm' + ks,))
                    yield
                    op('dve', lambda: V.reciprocal(out=sm_[:, 1:2], in_=sm_[:, 1:2]), r=('sm' + ks,), w=('sm' + ks,))
                    yield
                    op('dve', lambda: V.tensor_tensor(out=sm_[:, 2:3], in0=sm_[:, 1:2], in1=eb[:, col:col + 1], op=ALU.mult),
                       r=('sm' + ks, 'eb'), w=('sm' + ks,))
                    yield
                    op('act', lambda: A.activation(out=hbuf_, in_=pn_[:, 0:128], func=AF.Copy, scale=sm_[:, 2:3]),
                       r=(bk, 'sm' + ks), w=('hbuf' + ks,))
                    yield
                    op('act', lambda: A.activation(out=junkB[:, sl, :], in_=hbuf_, func=AF.Square, accum_out=sm_[:, 3:4]),
                       r=('hbuf' + ks,), w=('junkB' + ks, 'sm3' + ks))
                    yield
                    op('dve', lambda: V.tensor_scalar(out=sm_[:, 4:5], in0=sm_[:, 3:4], scalar1=1.0 / 128, scalar2=EPS, op0=ALU.mult, op1=ALU.add),
                       r=('sm3' + ks,), w=('sm4' + ks,))
                    yield
                    op('act', lambda: A.activation(out=sm_[:, 4:5], in_=sm_[:, 4:5], func=AF.Sqrt), r=('sm4' + ks,), w=('sm4' + ks,))
                    yield
                    op('dve', lambda: V.reciprocal(out=sm_[:, 4:5], in_=sm_[:, 4:5]), r=('sm4' + ks,), w=('sm4' + ks,))
                    yield
                    op('dve', lambda: V.scalar_tensor_tensor(out=hm[:, par, h * 128:(h + 1) * 128], in0=hbuf_, scalar=sm_[:, 4:5],
                                                             in1=gsig[:, bi, h * 128:(h + 1) * 128], op0=ALU.mult, op1=ALU.mult),
                       r=('hbuf' + ks, 'sm4' + ks, 'gsig%d' % bi + kp), w=('hm',))

                gens = [headgen(h_, h_) for h_ in range(4)]
                while gens:
                    for g_ in list(gens):
                        try:
                            next(g_)
                        except StopIteration:
                            gens.remove(g_)
                    yield
                if par == 1:
                    op('dve', lambda: V.tensor_scalar(out=hmo[:], in0=hm[:, 0, :], scalar1=sel[:, 0:1], scalar2=None, op0=ALU.mult),
                       r=('hm', 'sel'), w=('hmo',))
                    op('dve', lambda: V.scalar_tensor_tensor(out=hmb[:], in0=hm[:, 1, :], scalar=sel[:, 1:2], in1=hmo[:],
                                                             op0=ALU.mult, op1=ALU.add), r=('hm', 'sel', 'hmo'), w=('hmb',))
                    for h in range(4):
                        op('pe', lambda h=h: T.transpose(out=pT[:, h * 128:(h + 1) * 128], in_=hmb[:, h * 128:(h + 1) * 128],
                                                         identity=identb[:]), r=('hmb', 'identb'), w=('pT',))
                    op('act', lambda: A.activation(out=hmT[:], in_=pT[:, 0:512].rearrange("p (h t) -> p h t", h=4), func=AF.Copy),
                       r=('pT',), w=('hmT',))
                    op('sp', lambda: SP.dma_start(out=MIX_s[:, 0:4, pair * 128:(pair + 1) * 128], in_=hmT[:]),
                       r=('hmT',), w=('MIX_s',), dma='d_mixs')
                yield

        def run_il(ga, gb_):
            alive = [g_ for g_ in (ga, gb_) if g_ is not None]
            while alive:
                for g_ in list(alive):
                    try:
                        next(g_)
                    except StopIteration:
                        alive.remove(g_)
        run_il(stageA(0), None)
        for sbi in range(NSB):
            run_il(stageA(sbi + 1) if sbi + 1 < NSB else None, stageB(sbi))
        P.barrier()

    if STOP < 2:
        return
    with ExitStack() as p2:
        KT = [sb("KT%d" % i, [128, S], BF16, st=p2) for i in range(2)]
        VV = [sb("VV%d" % i, [128, NBLK, 129], BF16, st=p2) for i in range(2)]
        QT = [[sb("QT%d_%d" % (i, m), [128, TOWN], BF16, st=p2) for m in range(2)] for i in range(2)]
        for i in range(2):
            for m in range(2):
                op('dve', lambda i=i, m=m: V.memset(QT[i][m][:], 0.0), w=('QT%d' % i,))
        pT_ = [sb("pTb%d" % i, [128, 512], BF16, st=p2) for i in range(3)]
        o1 = sb("o1", [128, 128], st=p2); oo = sb("oo", [128, 128], st=p2)
        hdb = sb("hdb", [128, 128], BF16, st=p2); hdT = sb("hdT", [128, 128], BF16, st=p2)
        sd = sb("sd", [128, 8], st=p2); junk2 = sb("junk2", [128, 128], BF16, st=p2)
        psS = [ps("psS%d" % i, [128, 512], st=p2) for i in range(3)]
        psO1 = [ps("psO1_%d" % i, [128, 512], st=p2) for i in range(2)]
        psO2 = [ps("psO2_%d" % i, [128, 512], st=p2) for i in range(2)]
        psX = ps("psX", [128, 1024], BF16, st=p2)
        it = 0
        for h in range(4):
            hb_ = h % 2
            load(KT[hb_][:], KT_s[h], 'KT%d' % hb_, 'd_KT%d' % hb_)
            load(VV[hb_][:], V_s[h], 'VV%d' % hb_, 'd_VV%d' % hb_)
            for m in range(2):
                op('sp', lambda m=m: SP.dma_start(out=QT[hb_][m][m * 64:(m + 1) * 64, :], in_=QT_s[h, m * 64:(m + 1) * 64, :]),
                   r=(), w=('QT%d' % hb_,), dma='d_QT%d' % hb_)
            if h == 0:
                items = [('S', p, j, alibi) for p in range(NP) for j in range(2 * p + 2)]
            else:
                items = []
                for q in range(NP // 2):
                    p0, p1 = 2 * q, 2 * q + 1
                    for j in range(2 * p0 + 2):
                        items.append(('P', p0, p1, j))
                    for j in (2 * p0 + 2, 2 * p0 + 3):
                        items.append(('S', p1, j, alibi2))
            LOOK = 2
            bufof = {}

            def emit_S(ix):
                nonlocal it
                ent = items[ix]
                sbuf_i = it % 3; it += 1
                bufof[ix] = sbuf_i
                pS = psS[sbuf_i]; pSk = 'psS%d' % sbuf_i; pb = pT_[sbuf_i]; pbk = 'pTb%d' % sbuf_i
                if ent[0] == 'P':
                    _, p0, p1, j = ent
                    slots = ((p0, 0), (p1, 256)); p = p0; tab = alibi; wd = 512
                else:
                    _, p, j, tab = ent
                    slots = ((p, 0),); wd = 256
                u = 2 * p + 1 - j
                for (sp_, coff) in slots:
                    for m in range(2):
                        op('pe', lambda m=m, sp_=sp_, coff=coff: T.matmul(
                            pS[:, coff + m * 128:coff + (m + 1) * 128], lhsT=KT[hb_][:, j * 128:(j + 1) * 128],
                            rhs=QT[hb_][m][:, sp_ * 128:(sp_ + 1) * 128], start=True, stop=True),
                           r=('KT%d' % hb_, 'QT%d' % hb_), w=(pSk,))
                op('act', lambda: A.activation(out=pb[:, 0:wd], in_=pS[:, 0:wd], func=AF.Exp, bias=tab[:, h, u:u + 1]),
                   r=(pSk, 'alibi'), w=(pbk,))
                if u <= 1:
                    op('dve', lambda: V.tensor_tensor(out=pb[:, 0:256].rearrange("p (m t) -> p m t", m=2),
                                                      in0=pb[:, 0:256].rearrange("p (m t) -> p m t", m=2),
                                                      in1=masku[:, u:u + 1, :].to_broadcast([128, 2, 128]), op=ALU.mult),
                       r=(pbk, 'masku'), w=(pbk,))

            def emit_AV(ix):
                ent = items[ix]
                sbuf_i = bufof.pop(ix)
                pb = pT_[sbuf_i]; pbk = 'pTb%d' % sbuf_i
                if ent[0] == 'P':
                    _, p0, p1, j = ent
                    slots = ((p0, 0), (p1, 256))
                else:
                    _, p0, j, _t = ent
                    slots = ((p0, 0),)
                done = []
                for (sp_, coff) in slots:
                    ob = sp_ % 2
                    last = (j == 2 * sp_ + 1)
                    op('pe', lambda: T.matmul(psO1[ob][:, 0:129], lhsT=pb[:, coff:coff + 128], rhs=VV[hb_][:, j, :], start=(j == 0), stop=last),
                       r=(pbk, 'VV%d' % hb_), w=('psO1_%d' % ob,))
                    op('pe', lambda: T.matmul(psO2[ob][:, 0:129], lhsT=pb[:, coff + 128:coff + 256], rhs=VV[hb_][:, j, :], start=(j == 0), stop=last),
                       r=(pbk, 'VV%d' % hb_), w=('psO2_%d' % ob,))
                    if last:
                        done.append(sp_)
                for sp_ in done:
                    epilogue(sp_)

            def epilogue(p):
                ob = p % 2
                op('dve', lambda: V.reciprocal(out=sd[:, 0:1], in_=psO1[ob][:, 128:129]), r=('psO1_%d' % ob,), w=('sd',))
                op('dve', lambda: V.reciprocal(out=sd[:, 1:2], in_=psO2[ob][:, 128:129]), r=('psO2_%d' % ob,), w=('sd',))
                op('dve', lambda: V.tensor_tensor(out=sd[:, 2:3], in0=sd[:, 1:2], in1=lam[:, 1:2], op=ALU.mult), r=('sd', 'lam'), w=('sd',))
                op('dve', lambda: V.tensor_scalar(out=o1[:], in0=psO1[ob][:, 0:128], scalar1=sd[:, 0:1], scalar2=None, op0=ALU.mult),
                   r=('psO1_%d' % ob, 'sd'), w=('o1',))
                op('dve', lambda: V.scalar_tensor_tensor(out=oo[:], in0=psO2[ob][:, 0:128], scalar=sd[:, 2:3], in1=o1[:],
                                                         op0=ALU.mult, op1=ALU.add), r=('psO2_%d' % ob, 'sd', 'o1'), w=('oo',))
                op('dve', lambda: V.tensor_tensor(out=o1[:], in0=oo[:], in1=oo[:], op=ALU.mult), r=('oo',), w=('o1',))
                op('dve', lambda: V.tensor_reduce(out=sd[:, 3:4], in_=o1[:], axis=AX.X, op=ALU.add), r=('o1',), w=('sd3',))
                op('dve', lambda: V.tensor_scalar(out=sd[:, 4:5], in0=sd[:, 3:4], scalar1=1.0 / 128, scalar2=EPS, op0=ALU.mult, op1=ALU.add),
                   r=('sd3',), w=('sd4',))
                op('act', lambda: A.activation(out=sd[:, 4:5], in_=sd[:, 4:5], func=AF.Ln), r=('sd4',), w=('sd4',))
                op('act', lambda: A.activation(out=sd[:, 4:5], in_=sd[:, 4:5], func=AF.Exp, scale=-0.5), r=('sd4',), w=('sd4',))
                op('dve', lambda: V.scalar_tensor_tensor(out=hdb[:], in0=oo[:], scalar=sd[:, 4:5], in1=dagb[:], op0=ALU.mult, op1=ALU.mult),
                   r=('oo', 'sd4', 'dagb'), w=('hdb',))
                op('pe', lambda: T.transpose(out=psX[:, 0:128], in_=hdb[:], identity=identb[:]), r=('hdb', 'identb'), w=('psX',))
                op('dve', lambda: V.tensor_copy(out=hdT[:], in_=psX[:, 0:128]), r=('psX',), w=('hdT',))
                op('sp', lambda: SP.dma_start(out=MIX_s[:, 4 + h, p * 128:(p + 1) * 128], in_=hdT[:]), r=('hdT',), w=('MIX_s',), dma='d_mixs2')

            for ix in range(len(items) + LOOK):
                if ix < len(items):
                    emit_S(ix)
                if ix - LOOK >= 0:
                    emit_AV(ix - LOOK)
        if dbg:
            op('sp', lambda: SP.dma_start(out=dbgs['mix'], in_=MIX_s), r=('MIX_s',), w=('dbgmix',), dma='d_dbg')
        P.barrier()

    if STOP >= 3:
        PHASES2(nc, P, L, locals())


def PHASES2(nc, P, L, L2):
    es = L['es']; op = P.op; sb = L['sb']; ps = L['ps']; load = L['load']
    V = nc.vector; A = nc.scalar; G = nc.gpsimd; T = nc.tensor; SP = nc.sync
    S = L['S']; NBLK = L['NBLK']; NP = L['NP']; TOWN = L['TOWN']; NB = L['NB']
    dbg = L['dbg']; dbgs = L['dbgs']
    identb, identf, tri, tristrict, onesf, onesb = L['identb'], L['identf'], L['tri'], L['tristrict'], L['onesf'], L['onesb']
    pidx, iota, thr, mod = L['pidx'], L['iota'], L['thr'], L['mod']
    xo, out = L['xo'], L['out']
    MIX_s, H2_s, ACC_s, XS_s, YS_s = L['MIX_s'], L['H2_s'], L['ACC_s'], L['XS_s'], L['YS_s']
    SHIFT_A, SCALE_A, GATE_A, SHIFT_F, SCALE_F, GATE_F = [mod[:, i * D:(i + 1) * D] for i in range(6)]
    rstd_from_ss = L2['rstd_from_ss']

    with ExitStack() as p3:
        DESTI = sb("DESTI", [128, NP, 8], I32, st=p3)
        GATE8 = sb("GATE8", [128, NP, 8], st=p3)
        WIDX = sb("WIDX", [128, NB], I32, st=p3)
        cntbc = sb("cntbc", [128, NE], st=p3)
        p34 = ExitStack()
        RANK = sb("RANK", [128, NP, NE], st=p34)
        GD = sb("GD", [128, NP, NE], st=p34)
        with ExitStack() as p3a:
            woutb = sb("woutb", [128, 8, D], BF16, st=p3a)
            wrt = sb("wrt", [128, 8, NE], st=p3a)
            ws1b = sb("ws1b", [128, 8, 256], BF16, st=p3a); ws3b = sb("ws3b", [128, 8, 256], BF16, st=p3a)
            ws2b = sb("ws2b", [128, 2, D], BF16, st=p3a)
            rb = sb("rb", [128, NE], st=p3a)
            load(rb[:], L['rbias'], 'rb', 'd_c3')
            load(wrt[:], L['w_router'].rearrange("(k p) n -> p k n", p=128), 'wrt', 'd_c3')
            with ExitStack() as p3w:
                stg = sb("stg", [128, 4, D], st=p3w)
                wov = L['w_out'].rearrange("(k p) n -> p k n", p=128)
                for hf in range(2):
                    load(stg[:], wov[:, hf * 4:(hf + 1) * 4, :], 'stg', 'd_stg')
                    op('dve', lambda hf=hf: V.tensor_copy(out=woutb[:, hf * 4:(hf + 1) * 4, :], in_=stg[:]), r=('stg',), w=('woutb',))
                stv = stg[:].rearrange("p a n -> p (a n)")[:, 0:2048].rearrange("p (k n) -> p k n", k=8)
                load(stv, L['ws1'].rearrange("(k p) n -> p k n", p=128), 'stg', 'd_stg')
                op('dve', lambda: V.tensor_copy(out=ws1b[:], in_=stv), r=('stg',), w=('ws1b',))
                load(stv, L['ws3'].rearrange("(k p) n -> p k n", p=128), 'stg', 'd_stg')
                op('dve', lambda: V.tensor_copy(out=ws3b[:], in_=stv), r=('stg',), w=('ws3b',))
                load(stg[:, 0:2, :], L['ws2'].rearrange("(k p) n -> p k n", p=128), 'stg', 'd_stg')
                op('dve', lambda: V.tensor_copy(out=ws2b[:], in_=stg[:, 0:2, :]), r=('stg',), w=('ws2b',))
                P.barrier()
            xob = sb("xob", [128, D], st=p3a)
            mixT = sb("mixT", [128, 8, 128], BF16, st=p3a)
            x1 = sb("x1", [128, D], st=p3a)
            junk3 = sb("junk3", [128, D], BF16, st=p3a)
            s3 = sb("s3", [128, 8], st=p3a)
            h2n = sb("h2n", [128, D], st=p3a)
            h2 = sb("h2", [128, D], st=p3a)
            h2b = sb("h2b", [128, D], BF16, st=p3a)
            h2T = sb("h2T", [128, 8, 128], st=p3a)
            h2Tb = sb("h2Tb", [128, 8, 128], BF16, st=p3a)
            sg = sb("sg", [128, NE], st=p3a); selv = sb("selv", [128, NE], st=p3a)
            m8 = sb("m8", [128, 8, 8], st=p3a); gs = sb("gs", [128, 8], st=p3a); gm8 = sb("gm8", [128, 8], st=p3a)
            gmask = sb("gmask", [128, 8], st=p3a); gneg = sb("gneg", [128, 8], st=p3a)
            msk = sb("msk", [128, NE], st=p3a); t8 = sb("t8", [128, 8], st=p3a)
            mask8 = sb("mask8", [128, NE], st=p3a); mask8b = sb("mask8b", [128, NE], BF16, st=p3a)
            sil = sb("sil", [128, 256], st=p3a); actT = sb("actT", [128, 2, 128], BF16, st=p3a)
            accb = sb("accb", [128, D], st=p3a)
            pbig = ps("pbig", [128, 1024], st=p3a)
            pTf = ps("pTf", [128, 1024], st=p3a)
            pr = ps("pr", [128, 512], st=p3a)
            pg = ps("pg", [128, 512], st=p3a)
            pcn = ps("pcn", [128, 512], st=p3a)
            op('dve', lambda: V.memset(cntbc[:], 0.0), w=('cntbc',))
            for p in range(NP):
                tsl = slice(p * 128, (p + 1) * 128)
                load(xob[:], xo[tsl, :], 'xob', 'd_xob')
                load(mixT[:], MIX_s[:, :, tsl], 'mixT', 'd_mixT')
                for nh in range(2):
                    for k in range(8):
                        op('pe', lambda nh=nh, k=k: T.matmul(pbig[:, nh * 512:(nh + 1) * 512], lhsT=mixT[:, k, :],
                                                             rhs=woutb[:, k, nh * 512:(nh + 1) * 512], start=(k == 0), stop=(k == 7)),
                           r=('mixT', 'woutb'), w=('pbig',))
                op('dve', lambda: V.tensor_tensor(out=x1[:], in0=pbig[:], in1=GATE_A, op=ALU.mult), r=('pbig', 'mod'), w=('x1',))
                op('pool', lambda: G.tensor_tensor(out=x1[:], in0=x1[:], in1=xob[:], op=ALU.add), r=('x1', 'xob'), w=('x1',))
                op('act', lambda: A.activation(out=junk3[:], in_=x1[:], func=AF.Square, accum_out=s3[:, 0:1]), r=('x1',), w=('junk3', 's3'))
                rstd_from_ss(s3[:, 0:1], D, s3[:, 1:2], 's3', 's31')
                op('dve', lambda: V.scalar_tensor_tensor(out=h2n[:], in0=x1[:], scalar=s3[:, 1:2], in1=SCALE_F, op0=ALU.mult, op1=ALU.mult),
                   r=('x1', 's31', 'mod'), w=('h2n',))
                op('pool', lambda: G.tensor_tensor(out=h2[:], in0=h2n[:], in1=SHIFT_F, op=ALU.add), r=('h2n', 'mod'), w=('h2',))
                op('act', lambda: A.activation(out=h2b[:], in_=h2[:], func=AF.Copy), r=('h2',), w=('h2b',))
                op('sp', lambda: SP.dma_start(out=H2_s[tsl, :], in_=h2b[:]), r=('h2b',), w=('H2_s',), dma='d_h2s')
                for k in range(8):
                    op('pe', lambda k=k: T.transpose(out=pTf[:, k * 128:(k + 1) * 128], in_=h2[:, k * 128:(k + 1) * 128], identity=identf[:]),
                       r=('h2', 'identf'), w=('pTf',))
                op('act', lambda: A.activation(out=h2T[:], in_=pTf[:].rearrange("p (k t) -> p k t", k=8), func=AF.Copy), r=('pTf',), w=('h2T',))
                op('dve', lambda: V.tensor_copy(out=h2Tb[:], in_=h2T[:]), r=('h2T',), w=('h2Tb',))
                for k in range(8):
                    op('pe', lambda k=k: T.matmul(pr[:, 0:NE], lhsT=h2T[:, k, :], rhs=wrt[:, k, :], start=(k == 0), stop=(k == 7)),
                       r=('h2T', 'wrt'), w=('pr',))
                op('act', lambda: A.activation(out=sg[:], in_=pr[:, 0:NE], func=AF.Sigmoid), r=('pr',), w=('sg',))
                op('dve', lambda: V.tensor_tensor(out=selv[:], in0=sg[:], in1=rb[:], op=ALU.add), r=('sg', 'rb'), w=('selv',))
                for g in range(8):
                    op('dve', lambda g=g: V.max(out=m8[:, g, :], in_=selv[:, g * 32:(g + 1) * 32]), r=('selv',), w=('m8',))
                op('dve', lambda: V.tensor_tensor(out=gs[:], in0=m8[:, :, 0], in1=m8[:, :, 1], op=ALU.add), r=('m8',), w=('gs',))
                op('dve', lambda: V.max(out=gm8[:], in_=gs[:]), r=('gs',), w=('gm8',))
                op('dve', lambda: V.tensor_scalar(out=gmask[:], in0=gs[:], scalar1=gm8[:, 3:4], scalar2=None, op0=ALU.is_ge),
                   r=('gs', 'gm8'), w=('gmask',))
                op('dve', lambda: V.tensor_scalar(out=gneg[:], in0=gmask[:], scalar1=1.0, scalar2=1e30, op0=ALU.subtract, op1=ALU.mult),
                   r=('gmask',), w=('gneg',))
                op('dve', lambda: V.tensor_tensor(out=msk[:].rearrange("p (g e) -> p g e", g=8), in0=selv[:].rearrange("p (g e) -> p g e", g=8),
                                                  in1=gmask[:].rearrange("p (g o) -> p g o", o=1).to_broadcast([128, 8, 32]), op=ALU.mult),
                   r=('selv', 'gmask'), w=('msk',))
                op('dve', lambda: V.tensor_tensor(out=msk[:].rearrange("p (g e) -> p g e", g=8), in0=msk[:].rearrange("p (g e) -> p g e", g=8),
                                                  in1=gneg[:].rearrange("p (g o) -> p g o", o=1).to_broadcast([128, 8, 32]), op=ALU.add),
                   r=('msk', 'gneg'), w=('msk',))
                op('dve', lambda: V.max(out=t8[:], in_=msk[:]), r=('msk',), w=('t8',))
                op('dve', lambda: V.tensor_scalar(out=mask8[:], in0=msk[:], scalar1=t8[:, 7:8], scalar2=None, op0=ALU.is_ge),
                   r=('msk', 't8'), w=('mask8',))
                op('pool', lambda: G.tensor_copy(out=mask8b[:], in_=mask8[:]), r=('mask8',), w=('mask8b',))
                op('dve', lambda: V.tensor_tensor(out=GD[:, p, :], in0=sg[:], in1=mask8[:], op=ALU.mult), r=('sg', 'mask8'), w=('GD',))
                op('dve', lambda: V.tensor_reduce(out=s3[:, 2:3], in_=GD[:, p, :], axis=AX.X, op=ALU.add), r=('GD',), w=('s32',))
                op('dve', lambda: V.reciprocal(out=s3[:, 3:4], in_=s3[:, 2:3]), r=('s32',), w=('s33',))
                op('dve', lambda: V.tensor_scalar(out=s3[:, 3:4], in0=s3[:, 3:4], scalar1=2.5, scalar2=None, op0=ALU.mult),
                   r=('s33',), w=('s33',))
                op('dve', lambda: V.tensor_scalar(out=GD[:, p, :], in0=GD[:, p, :], scalar1=s3[:, 3:4], scalar2=None, op0=ALU.mult),
                   r=('GD', 's33'), w=('GD',))
                op('pe', lambda: T.matmul(pcn[:, 0:NE], lhsT=tristrict[:], rhs=mask8b[:], start=True, stop=True),
                   r=('tristrict', 'mask8b'), w=('pcn',))
                op('pe', lambda: T.matmul(pcn[:, NE:2 * NE], lhsT=onesb[:], rhs=mask8b[:], start=True, stop=True),
                   r=('onesb', 'mask8b'), w=('pcn',))
                op('dve', lambda: V.tensor_tensor(out=RANK[:, p, :], in0=pcn[:, 0:NE], in1=cntbc[:], op=ALU.add), r=('pcn', 'cntbc'), w=('RANK',))
                op('dve', lambda: V.tensor_tensor(out=cntbc[:], in0=pcn[:, NE:2 * NE], in1=cntbc[:], op=ALU.add), r=('pcn', 'cntbc'), w=('cntbc',))
                for wi, wb_ in enumerate((ws1b, ws3b)):
                    for fc in range(2):
                        c0 = (wi * 2 + fc) * 128
                        for k in range(8):
                            op('pe', lambda k=k, fc=fc, wb_=wb_, c0=c0: T.matmul(pg[:, c0:c0 + 128], lhsT=wb_[:, k, fc * 128:(fc + 1) * 128],
                                                                                 rhs=h2Tb[:, k, :], start=(k == 0), stop=(k == 7)),
                               r=('h2Tb', 'ws1b', 'ws3b'), w=('pg',))
                op('act', lambda: A.activation(out=sil[:], in_=pg[:, 0:256], func=AF.Silu), r=('pg',), w=('sil',))
                op('dve', lambda: V.tensor_tensor(out=actT[:].rearrange("p a t -> p (a t)"), in0=sil[:], in1=pg[:, 256:512], op=ALU.mult),
                   r=('sil', 'pg'), w=('actT',))
                for nh in range(2):
                    for fc in range(2):
                        op('pe', lambda nh=nh, fc=fc: T.matmul(pbig[:, nh * 512:(nh + 1) * 512], lhsT=actT[:, fc, :],
                                                               rhs=ws2b[:, fc, nh * 512:(nh + 1) * 512], start=(fc == 0), stop=(fc == 1)),
                           r=('actT', 'ws2b'), w=('pbig',))
                op('dve', lambda: V.tensor_tensor(out=accb[:], in0=pbig[:], in1=GATE_F, op=ALU.mult), r=('pbig', 'mod'), w=('accb',))
                op('pool', lambda: G.tensor_tensor(out=accb[:], in0=accb[:], in1=x1[:], op=ALU.add), r=('accb', 'x1'), w=('accb',))
                op('sp', lambda: SP.dma_start(out=ACC_s[tsl, :], in_=accb[:]), r=('accb',), w=('ACC_s',), dma='d_accs')
            if dbg:
                op('sp', lambda: SP.dma_start(out=dbgs['acc'], in_=ACC_s), r=('ACC_s',), w=('dbgacc',), dma='d_dbg')
                op('sp', lambda: SP.dma_start(out=dbgs['h2'], in_=H2_s), r=('H2_s',), w=('dbgh2',), dma='d_dbg')
                op('sp', lambda: SP.dma_start(out=dbgs['gd'], in_=GD[:]), r=('GD',), w=('dbggd',), dma='d_dbg')
            P.barrier()

        if STOP < 4:
            p34.close()
            return
        with ExitStack() as p4:
            cntT = sb("cntT", [128, 2], st=p4)
            cmp_ = sb("cmp", [128, 32], st=p4)
            nblk = sb("nblk", [128, 2], st=p4)
            nbb = sb("nbb", [128, 2, 128], st=p4)
            trihi = sb("trihi", [128, 2, NE], st=p4)
            trilo = sb("trilo", [128, 2, NE], st=p4)
            pstart = sb("pstart", [128, NE], st=p4)
            pendc = sb("pendc", [128, 2], st=p4)
            Am = sb("Am", [128, 2, 512], st=p4)
            bef = sb("bef", [128, 512], st=p4)
            key = sb("key", [128, NE], st=p4); k8 = sb("k8", [128, 8], st=p4); oh = sb("oh", [128, NE], st=p4)
            d8 = sb("d8", [128, 8], st=p4)
            pq = ps("pq", [128, 512], st=p4)
            pq2 = ps("pq2", [128, 512], st=p4)
            pq3 = ps("pq3", [128, 512], st=p4)
            for c in range(2):
                op('pe', lambda c=c: T.transpose(out=pq[:, c * 128:(c + 1) * 128], in_=cntbc[:, c * 128:(c + 1) * 128], identity=identf[:]),
                   r=('cntbc', 'identf'), w=('pq',))
            op('dve', lambda: V.tensor_copy(out=cntT[:], in_=pq[:, 0:256].rearrange("p (c t) -> p c t", c=2)[:, :, 0]), r=('pq',), w=('cntT',))
            for c in range(2):
                op('dve', lambda c=c: V.tensor_scalar(out=cmp_[:], in0=thr[:], scalar1=cntT[:, c:c + 1], scalar2=None, op0=ALU.is_lt),
                   r=('thr', 'cntT'), w=('cmp',))
                op('dve', lambda c=c: V.tensor_reduce(out=nblk[:, c:c + 1], in_=cmp_[:], axis=AX.X, op=ALU.add), r=('cmp',), w=('nblk',))
                op('dve', lambda c=c: V.tensor_copy(out=nbb[:, c, :], in_=nblk[:, c:c + 1].to_broadcast([128, 128])), r=('nblk',), w=('nbb',))
            op('dve', lambda: V.memset(trihi[:], 0.0), w=('trihi',))
            op('dve', lambda: V.tensor_copy(out=trihi[:, 0, 0:128], in_=tri[:]), r=('tri',), w=('trihi',))
            op('dve', lambda: V.memset(trihi[:, 0, 128:256], 1.0), w=('trihi',))
            op('dve', lambda: V.tensor_copy(out=trihi[:, 1, 128:256], in_=tri[:]), r=('tri',), w=('trihi',))
            op('dve', lambda: V.tensor_copy(out=trilo[:], in_=trihi[:]), r=('trihi',), w=('trilo',))
            op('dve', lambda: V.tensor_tensor(out=trilo[:, 0, 0:128], in0=trilo[:, 0, 0:128], in1=identf[:], op=ALU.subtract),
               r=('trilo', 'identf'), w=('trilo',))
            op('dve', lambda: V.tensor_tensor(out=trilo[:, 1, 128:256], in0=trilo[:, 1, 128:256], in1=identf[:], op=ALU.subtract),
               r=('trilo', 'identf'), w=('trilo',))
            for c in range(2):
                op('pe', lambda c=c: T.matmul(pq2[:, 0:NE], lhsT=nbb[:, c, :], rhs=trihi[:, c, :], start=(c == 0), stop=(c == 1)),
                   r=('nbb', 'trihi'), w=('pq2',))
            for c in range(2):
                op('pe', lambda c=c: T.matmul(pq2[:, NE:2 * NE], lhsT=nbb[:, c, :], rhs=trilo[:, c, :], start=(c == 0), stop=(c == 1)),
                   r=('nbb', 'trilo'), w=('pq2',))
            op('dve', lambda: V.tensor_scalar(out=pstart[:], in0=pq2[:, NE:2 * NE], scalar1=128.0, scalar2=1.0, op0=ALU.mult, op1=ALU.add),
               r=('pq2',), w=('pstart',))
            op('dve', lambda: V.tensor_copy(out=bef[:, 0:NE], in_=pq2[:, 0:NE]), r=('pq2',), w=('bef',))
            for c in range(2):
                op('pe', lambda c=c: T.transpose(out=pq[:, 256 + c * 128:256 + (c + 1) * 128], in_=bef[:, c * 128:(c + 1) * 128], identity=identf[:]),
                   r=('bef', 'identf'), w=('pq',))
            op('dve', lambda: V.tensor_copy(out=pendc[:], in_=pq[:, 256:512].rearrange("p (c t) -> p c t", c=2)[:, :, 0]), r=('pq',), w=('pendc',))
            for c in range(2):
                op('dve', lambda c=c: V.tensor_scalar(out=Am[:, c, 0:NB], in0=iota[:, 0:NB], scalar1=pendc[:, c:c + 1], scalar2=None, op0=ALU.is_ge),
                   r=('iota', 'pendc'), w=('Am',))
            for c in range(2):
                op('pe', lambda c=c: T.matmul(pq3[:, 0:NB], lhsT=onesf[:], rhs=Am[:, c, 0:NB], start=(c == 0), stop=(c == 1)),
                   r=('onesf', 'Am'), w=('pq3',))
            op('dve', lambda: V.tensor_scalar(out=bef[:, 0:NB], in0=pq3[:, 0:NB], scalar1=255.0, scalar2=128.0, op0=ALU.min, op1=ALU.mult),
               r=('pq3',), w=('bef',))
            op('dve', lambda: V.tensor_scalar(out=bef[:, 0:NB], in0=bef[:, 0:NB], scalar1=pidx[:, 0:1], scalar2=None, op0=ALU.add),
               r=('bef', 'pidx'), w=('bef',))
            op('dve', lambda: V.tensor_copy(out=WIDX[:], in_=bef[:, 0:NB]), r=('bef',), w=('WIDX',))
            for p in range(NP):
                op('dve', lambda p=p: V.tensor_tensor(out=key[:], in0=RANK[:, p, :], in1=pstart[:], op=ALU.add), r=('RANK', 'pstart'), w=('key',))
                op('dve', lambda p=p: V.scalar_tensor_tensor(out=oh[:], in0=GD[:, p, :], scalar=0.0, in1=key[:], op0=ALU.is_gt, op1=ALU.mult),
                   r=('GD', 'key'), w=('oh',))
                op('dve', lambda: V.max(out=k8[:], in_=oh[:]), r=('oh',), w=('k8',))
                op('dve', lambda: V.tensor_scalar(out=d8[:], in0=k8[:], scalar1=-1.0, scalar2=None, op0=ALU.add), r=('k8',), w=('d8',))
                op('dve', lambda p=p: V.tensor_copy(out=DESTI[:, p, :], in_=d8[:]), r=('d8',), w=('DESTI',))
                for k in range(8):
                    op('dve', lambda p=p, k=k: V.scalar_tensor_tensor(out=key[:], in0=oh[:], scalar=k8[:, k:k + 1], in1=GD[:, p, :],
                                                                      op0=ALU.is_equal, op1=ALU.mult), r=('oh', 'k8', 'GD'), w=('key',))
                    op('dve', lambda p=p, k=k: V.tensor_reduce(out=GATE8[:, p, k:k + 1], in_=key[:], axis=AX.X, op=ALU.add),
                       r=('key',), w=('GATE8',))
            P.barrier()
        p34.close()

        if STOP < 5:
            return
        with ExitStack() as p5:
            hrow = [sb("hrow%d" % i, [128, D], BF16, st=p5) for i in range(2)]
            for p in range(NP):
                b = p % 2
                load(hrow[b][:], H2_s[p * 128:(p + 1) * 128, :], 'hrow%d' % b, 'd_hrow%d' % b)
                for k in range(8):
                    op('pool', lambda p=p, k=k, b=b: G.indirect_dma_start(
                        out=XS_s, out_offset=bass.IndirectOffsetOnAxis(DESTI[:, p, k:k + 1], 0), in_=hrow[b][:], in_offset=None),
                       r=('hrow%d' % b, 'DESTI'), w=('XS_s',), dma='d_scat%d' % b)
            P.barrier()

        if STOP < 6:
            return
        with ExitStack() as p6:
            xs = [sb("xs%d" % i, [128, D], BF16, st=p6) for i in range(3)]
            xT = sb("xT", [128, 8, 128], BF16, st=p6)
            wfa = [sb("wfa_%d" % i, [128, 6144], st=p6) for i in range(3)]
            wf = [[wfa[i][:, j * 2048:(j + 1) * 2048] for i in range(3)] for j in range(3)]
            wb = [[sb("wb%d_%d" % (j, i), [128, 2048], BF16, st=p6) for i in range(2)] for j in range(3)]
            sil6 = sb("sil6", [128, 256], st=p6); act6 = sb("act6", [128, 2, 128], BF16, st=p6)
            ysb = [sb("ysb%d" % i, [128, D], st=p6) for i in range(2)]
            pX = ps("pX6", [128, 1024], BF16, st=p6)
            pG = [ps("pG6_%d" % i, [128, 512], st=p6) for i in range(2)]
            pY = [ps("pY6_%d" % i, [128, 1024], st=p6) for i in range(2)]
            cast_eng = ('dve', 'act', 'act')

            def fetch(i):
                b = i % 3
                load(xs[b][:], XS_s[i * 128:(i + 1) * 128, :], 'xs%d' % b, 'd_xs%d' % b)
                op('pool', lambda b=b, i=i: G.indirect_dma_start(
                    out=wfa[b][:], out_offset=None, in_=L['wexp'], in_offset=bass.IndirectOffsetOnAxis(WIDX[:, i:i + 1], 0)),
                   r=('WIDX',), w=('wf0_%d' % b, 'wf1_%d' % b, 'wf2_%d' % b), dma='d_wf_%d' % b)
            fetch(0)
            fetch(1)
            for i in range(NB):
                b = i % 2
                bf = i % 3
                if i + 2 < NB:
                    fetch(i + 2)
                for j in range(3):
                    e_ = cast_eng[j]
                    if e_ == 'act':
                        op('act', lambda j=j, b=b: A.activation(out=wb[j][b][:], in_=wf[j][bf], func=AF.Copy),
                           r=('wf%d_%d' % (j, bf),), w=('wb%d_%d' % (j, b),))
                    else:
                        op(e_, lambda j=j, b=b, e_=e_: P.E[e_].tensor_copy(out=wb[j][b][:], in_=wf[j][bf]),
                           r=('wf%d_%d' % (j, bf),), w=('wb%d_%d' % (j, b),))
                xv = xs[bf][:].rearrange("p (q c) -> p c q", c=8)
                for c in range(8):
                    op('pe', lambda c=c: T.transpose(out=pX[:, c * 128:(c + 1) * 128], in_=xv[:, c, :], identity=identb[:]),
                       r=('xs%d' % bf, 'identb'), w=('pX6',))
                op('dve', lambda: V.tensor_copy(out=xT[:], in_=pX[:].rearrange("p (c t) -> p c t", c=8)), r=('pX6',), w=('xT',))
                pg_ = pG[b]; pgk = 'pG6_%d' % b
                for wi in range(2):
                    wv_ = wb[wi][b][:].rearrange("p (c f) -> p c f", c=8)
                    for fc in range(2):
                        c0 = (wi * 2 + fc) * 128
                        for c in range(8):
                            op('pe', lambda c=c, fc=fc, wv_=wv_, c0=c0: T.matmul(
                                pg_[:, c0:c0 + 128], lhsT=wv_[:, c, :].rearrange("p (m two) -> p two m", two=2)[:, fc, :],
                                rhs=xT[:, c, :], start=(c == 0), stop=(c == 7)),
                               r=('xT', 'wb%d_%d' % (wi, b)), w=(pgk,))
                op('act', lambda: A.activation(out=sil6[:], in_=pg_[:, 0:256], func=AF.Silu), r=(pgk,), w=('sil6',))
                op('dve', lambda: V.tensor_tensor(out=act6[:].rearrange("p a t -> p (a t)"), in0=sil6[:], in1=pg_[:, 256:512], op=ALU.mult),
                   r=('sil6', pgk), w=('act6',))
                w2v = wb[2][b][:].rearrange("p (c n) -> p c n", c=2)
                py_ = pY[b]; pyk = 'pY6_%d' % b
                for nh in range(2):
                    for fc in range(2):
                        op('pe', lambda nh=nh, fc=fc: T.matmul(py_[:, nh * 512:(nh + 1) * 512], lhsT=act6[:, fc, :],
                                                               rhs=w2v[:, fc, nh * 512:(nh + 1) * 512], start=(fc == 0), stop=(fc == 1)),
                           r=('act6', 'wb2_%d' % b), w=(pyk,))
                op('act', lambda: A.activation(out=ysb[b][:, 0:512], in_=py_[:, 0:512], func=AF.Copy), r=(pyk,), w=('ysb%d' % b,))
                op('dve', lambda: V.tensor_copy(out=ysb[b][:, 512:1024], in_=py_[:, 512:1024]), r=(pyk,), w=('ysb%d' % b,))
                op('sp', lambda i=i: SP.dma_start(out=YS_s[i * 128:(i + 1) * 128, :], in_=ysb[b][:]), r=('ysb%d' % b,), w=('YS_s',), dma='d_ys%d' % b)
            P.barrier()

        if STOP < 7:
            return
        with ExitStack() as p7:
            yg = [sb("yg%d" % i, [128, D], st=p7) for i in range(3)]
            acc = sb("acc7", [128, D], st=p7)
            base = [sb("base%d" % i, [128, D], st=p7) for i in range(2)]
            ob = [sb("ob%d" % i, [128, D], st=p7) for i in range(2)]
            ng = 0
            for p in range(NP):
                b = p % 2
                tsl = slice(p * 128, (p + 1) * 128)
                load(base[b][:], ACC_s[tsl, :], 'base%d' % b, 'd_base%d' % b)
                for k in range(8):
                    gi = ng % 3; ng += 1
                    op('pool', lambda p=p, k=k, gi=gi: G.indirect_dma_start(
                        out=yg[gi][:], out_offset=None, in_=YS_s, in_offset=bass.IndirectOffsetOnAxis(DESTI[:, p, k:k + 1], 0)),
                       r=('DESTI', 'YS_s'), w=('yg%d' % gi,), dma='d_yg%d' % gi)
                    if k == 0:
                        op('dve', lambda p=p, k=k, gi=gi: V.tensor_scalar(out=acc[:], in0=yg[gi][:], scalar1=GATE8[:, p, k:k + 1], scalar2=None,
                                                                          op0=ALU.mult), r=('yg%d' % gi, 'GATE8'), w=('acc7',))
                    else:
                        op('dve', lambda p=p, k=k, gi=gi: V.scalar_tensor_tensor(out=acc[:], in0=yg[gi][:], scalar=GATE8[:, p, k:k + 1], in1=acc[:],
                                                                                 op0=ALU.mult, op1=ALU.add), r=('yg%d' % gi, 'GATE8', 'acc7'), w=('acc7',))
                op('dve', lambda: V.tensor_tensor(out=ob[b][:], in0=acc[:], in1=GATE_F, op=ALU.mult), r=('acc7', 'mod'), w=('ob%d' % b,))
                op('pool', lambda: G.tensor_tensor(out=ob[b][:], in0=ob[b][:], in1=base[b][:], op=ALU.add), r=('ob%d' % b, 'base%d' % b), w=('ob%d' % b,))
                op('sp', lambda: SP.dma_start(out=out[tsl, :], in_=ob[b][:]), r=('ob%d' % b,), w=('out',), dma='d_out%d' % b)
            P.barrier()


def _consts(S, half):
    NBLK = S // 128
    TOWN = S // 2
    NB = TOWN * 8 // 128 + 256
    bf = ml_dtypes.bfloat16
    c = {}
    c['c_identb'] = np.eye(128, dtype=np.float32).astype(bf)
    c['c_identf'] = np.eye(128, dtype=np.float32)
    u = np.arange(128)
    c['c_tri'] = (u[:, None] <= u[None, :]).astype(np.float32)
    c['c_tristrict'] = (u[:, None] < u[None, :]).astype(np.float32).astype(bf)
    c['c_maskml'] = ((u[:, None] <= u[None, :]).astype(np.float32) * (128.0 ** -0.5)).astype(np.float32)
    causal = (u[:, None] <= u[None, :]).astype(np.float32)
    mk = np.zeros((128, 2, 128), np.float32)
    if half == 0:
        mk[:, 1, :] = causal; mk[:, 0, :] = 0.0
    else:
        mk[:, 1, :] = 1.0; mk[:, 0, :] = causal
    c['c_masku'] = mk.astype(bf)
    slopes = 2.0 ** (-8.0 * np.arange(1, 5) / 4.0)
    uu = np.arange(NBLK + 1)
    delta = (uu - 1) if half == 0 else uu
    tab = slopes[None, :, None] * u[:, None, None] - 128.0 * slopes[None, :, None] * delta[None, None, :]
    c['c_alibi'] = tab.astype(np.float32)
    c['c_alibi2'] = (tab + 256.0 * slopes[None, :, None]).astype(np.float32)
    s = np.zeros((128, 2), np.float32); s[:, half] = 1.0
    c['c_sel'] = s
    c['c_pidx'] = u.astype(np.float32)[:, None].copy()
    c['c_iota'] = np.tile(np.arange(512, dtype=np.float32)[None, :], (128, 1))
    c['c_thr'] = np.tile((128.0 * np.arange(32, dtype=np.float32))[None, :], (128, 1))
    return c


def _rep(v, n=128):
    return np.ascontiguousarray(np.broadcast_to(np.asarray(v, np.float32).reshape(1, -1), (n, np.asarray(v).size)))


def make_in_maps(inp, S, B):
    f = np.float32
    maps = []
    shared = {}
    shared['w_ada'] = np.ascontiguousarray(inp['w_ada'][0], f)
    shared['b_ada'] = _rep(inp['b_ada'][0])
    shared['w_in'] = np.ascontiguousarray(inp['w_in'][0], f)
    cw = np.asarray(inp['conv_w'][0], f)
    shared['convw'] = np.ascontiguousarray(cw.reshape(4, 8, 128).transpose(2, 1, 0))
    shared['convb'] = np.ascontiguousarray(np.asarray(inp['conv_b'][0], f).reshape(8, 128).T)
    shared['gate_b'] = _rep(inp['gate_b'][0])
    shared['mlg'] = _rep(np.tile(np.asarray(inp['ml_norm_g'][0], f), 4))
    shared['qg'] = _rep(inp['da_q_norm_g'][0])
    shared['kg'] = _rep(inp['da_k_norm_g'][0])
    lv = np.stack([inp['lambda_q1'][0], inp['lambda_k1'][0], inp['lambda_q2'][0], inp['lambda_k2'][0]]).astype(f)
    shared['lamv'] = np.ascontiguousarray(np.broadcast_to(lv[None], (128, 4, 64)))
    shared['dag'] = _rep(inp['da_norm_g'][0])
    shared['w_out'] = np.ascontiguousarray(inp['w_out'][0], f)
    shared['w_router'] = np.ascontiguousarray(inp['w_router'][0], f)
    shared['rbias'] = _rep(inp['router_bias'][0])
    shared['wexp'] = np.concatenate([np.asarray(inp['w1'][0], f).reshape(NE * 128, 2048),
                                     np.asarray(inp['w3'][0], f).reshape(NE * 128, 2048),
                                     np.asarray(inp['w2'][0], f).reshape(NE * 128, 2048)], axis=1)
    shared['ws1'] = np.ascontiguousarray(inp['ws1'][0], f)
    shared['ws3'] = np.ascontiguousarray(inp['ws3'][0], f)
    shared['ws2'] = np.ascontiguousarray(inp['ws2'][0], f)
    consts = [_consts(S, 0), _consts(S, 1)]
    xall = np.asarray(inp['x'], f)
    call = np.asarray(inp['c'], f)
    for b in range(B):
        for half in range(2):
            m = dict(shared)
            m.update(consts[half])
            m['x'] = np.ascontiguousarray(xall[b])
            m['xo'] = np.ascontiguousarray(xall[b].reshape(S // 256, 2, 128, D)[:, half].reshape(S // 2, D))
            m['ccol'] = np.ascontiguousarray(call[b].reshape(8, 128).T)
            maps.append(m)
    return maps


_NC_CACHE = {}


def run(inp, S, B, dbg=False):
    key = (S, dbg)
    if key not in _NC_CACHE:
        _NC_CACHE[key] = build_nc(S, dbg)
    nc = _NC_CACHE[key]
    maps = make_in_maps(inp, S, B)
    res = run_bass_kernel_spmd(nc, maps, core_ids=list(range(2 * B)))
    outf = np.zeros((B, S, D), np.float32)
    for b in range(B):
        for half in range(2):
            r = res.results[b * 2 + half]["out"]
            outf[b].reshape(S // 256, 2, 128, D)[:, half] = r.reshape(S // 256, 128, D)
    return outf, res


def kernel(**inputs):
    x = np.asarray(inputs['x'])
    B, S, _ = x.shape
    outf, _ = run(inputs, S, B, dbg=False)
    return outf
```

```python
import numpy as np
import ml_dtypes
from contextlib import ExitStack
import concourse.bass as bass
import concourse.mybir as mybir
from concourse.bass_utils import run_bass_kernel_spmd

F32 = mybir.dt.float32
BF16 = mybir.dt.bfloat16
I32 = mybir.dt.int32
ALU = mybir.AluOpType
AF = mybir.ActivationFunctionType
AX = mybir.AxisListType

D = 1024
DIN = 3592
NE = 256
EPS = 1e-6
SAME_ENGINE_RAW_WAIT = True
STOP = 99


class Prog:
    def __init__(self, nc, es):
        self.nc = nc
        self.es = es
        self.E = {'pe': nc.tensor, 'act': nc.scalar, 'dve': nc.vector, 'pool': nc.gpsimd, 'sp': nc.sync}
        self.sems = {}
        self.val = {}
        for e in self.E:
            self._sem(e)
        self.known = {e: {} for e in self.E}
        self.lastw = {}
        self.readers = {}
        self.n = 0

    def _sem(self, name):
        if name not in self.sems:
            self.sems[name] = self.es.enter_context(self.nc.semaphore("s_" + name))
            self.val[name] = 0
        return self.sems[name]

    def _wait(self, eng, ev):
        sn, v, snap = ev
        kn = self.known[eng]
        if kn.get(sn, 0) >= v:
            return
        self.E[eng].wait_ge(self.sems[sn], v)
        kn[sn] = v
        for k, vv in snap.items():
            if kn.get(k, 0) < vv:
                kn[k] = vv

    def op(self, eng, fn, r=(), w=(), dma=None):
        self.n += 1
        for res in r:
            ev = self.lastw.get(res)
            if ev is not None:
                if ev[0] == eng and not SAME_ENGINE_RAW_WAIT:
                    continue
                if ev[0] == 'pe' and eng == 'pe':
                    continue
                self._wait(eng, ev)
        for res in w:
            ev = self.lastw.get(res)
            if ev is not None and ev[0] != eng:
                self._wait(eng, ev)
            for sn, ev2 in self.readers.get(res, {}).items():
                if sn != eng:
                    self._wait(eng, ev2)
        ins = fn()
        kn = self.known[eng]
        if dma is None:
            self.val[eng] += 1
            ins.then_inc(self.sems[eng], 1)
            ev = (eng, self.val[eng], dict(kn))
        else:
            self._sem(dma)
            self.val[dma] += 16
            ins.then_inc(self.sems[dma], 16)
            ev = (dma, self.val[dma], dict(kn))
        for res in w:
            self.lastw[res] = ev
            self.readers[res] = {}
        for res in r:
            self.readers.setdefault(res, {})[ev[0]] = ev
        return ev

    def barrier(self):
        for e in self.E:
            for sn, v in self.val.items():
                if v > 0 and sn != e:
                    self._wait(e, (sn, v, {}))
            if self.val[e] > 0:
                self._wait(e, (e, self.val[e], {}))
        self.lastw = {}
        self.readers = {}

    def finish(self):
        self.barrier()


def build_nc(S, dbg=False):
    NBLK = S // 128
    NP = NBLK // 2
    TOWN = S // 2
    NSB = S // 512
    NB = TOWN * 8 // 128 + 256
    assert NB <= 512
    nc = bass.Bass("TRN2", target_bir_lowering=False)

    def din(name, shape, dt=F32):
        return nc.dram_tensor(name, list(shape), dt, kind="ExternalInput").ap()

    def dscr(name, shape, dt):
        return nc.dram_tensor(name, list(shape), dt).ap()

    x = din("x", [S, D])
    xo = din("xo", [TOWN, D])
    ccol = din("ccol", [128, 8])
    w_ada = din("w_ada", [D, 6 * D])
    b_ada = din("b_ada", [128, 6 * D])
    w_in = din("w_in", [D, DIN])
    convw = din("convw", [128, 8, 4])
    convb = din("convb", [128, 8])
    gate_b = din("gate_b", [128, 8])
    mlg = din("mlg", [128, 512])
    qg = din("qg", [128, 64])
    kg = din("kg", [128, 64])
    lamv = din("lamv", [128, 4, 64])
    dag = din("dag", [128, 128])
    w_out = din("w_out", [D, D])
    w_router = din("w_router", [D, NE])
    rbias = din("rbias", [128, NE])
    wexp = din("wexp", [NE * 128, 6144])
    ws1 = din("ws1", [D, 256])
    ws3 = din("ws3", [D, 256])
    ws2 = din("ws2", [256, D])
    c_identb = din("c_identb", [128, 128], BF16)
    c_identf = din("c_identf", [128, 128])
    c_tri = din("c_tri", [128, 128])
    c_tristrict = din("c_tristrict", [128, 128], BF16)
    c_maskml = din("c_maskml", [128, 128])
    c_masku = din("c_masku", [128, 2, 128], BF16)
    c_alibi = din("c_alibi", [128, 4, NBLK + 1])
    c_alibi2 = din("c_alibi2", [128, 4, NBLK + 1])
    c_sel = din("c_sel", [128, 2])
    c_pidx = din("c_pidx", [128, 1])
    c_iota = din("c_iota", [128, 512])
    c_thr = din("c_thr", [128, 32])
    out = nc.dram_tensor("out", [TOWN, D], F32, kind="ExternalOutput").ap()

    KT_s = dscr("KT_s", [4, 128, S], BF16)
    QT_s = dscr("QT_s", [4, 128, TOWN], BF16)
    V_s = dscr("V_s", [4, 128, NBLK, 129], BF16)
    MIX_s = dscr("MIX_s", [128, 8, TOWN], BF16)
    H2_s = dscr("H2_s", [TOWN, D], BF16)
    ACC_s = dscr("ACC_s", [TOWN, D], F32)
    XS_s = dscr("XS_s", [NB * 128, D], BF16)
    YS_s = dscr("YS_s", [NB * 128, D], F32)
    dbgs = {}
    if dbg:
        dbgs['mod'] = nc.dram_tensor("d_mod", [128, 6 * D], F32, kind="ExternalOutput").ap()
        dbgs['mix'] = nc.dram_tensor("d_mix", [128, 8, TOWN], BF16, kind="ExternalOutput").ap()
        dbgs['acc'] = nc.dram_tensor("d_acc", [TOWN, D], F32, kind="ExternalOutput").ap()
        dbgs['h2'] = nc.dram_tensor("d_h2", [TOWN, D], BF16, kind="ExternalOutput").ap()
        dbgs['gd'] = nc.dram_tensor("d_gd", [128, NP, NE], F32, kind="ExternalOutput").ap()

    with ExitStack() as es:
        P = Prog(nc, es)
        op = P.op

        def sb(name, shape, dt=F32, st=es):
            return st.enter_context(nc.sbuf_tensor(name, list(shape), dt))

        def ps(name, shape, dt=F32, st=es):
            return st.enter_context(nc.psum_tensor(name, list(shape), dt))

        V = nc.vector
        A = nc.scalar
        G = nc.gpsimd
        T = nc.tensor
        SP = nc.sync

        def load(dst, src, key, dsem, eng='sp'):
            return op(eng, lambda: P.E[eng].dma_start(out=dst, in_=src), r=(), w=(key,), dma=dsem)

        identb = sb("identb", [128, 128], BF16)
        identf = sb("identf", [128, 128])
        tri = sb("tri", [128, 128])
        tristrict = sb("tristrict", [128, 128], BF16)
        onesf = sb("onesf", [128, 128])
        onesb = sb("onesb", [128, 128], BF16)
        maskml = sb("maskml", [128, 128])
        masku = sb("masku", [128, 2, 128], BF16)
        alibi = sb("alibi", [128, 4, NBLK + 1])
        alibi2 = sb("alibi2", [128, 4, NBLK + 1])
        sel = sb("sel", [128, 2])
        pidx = sb("pidx", [128, 1])
        iota = sb("iota", [128, 512])
        thr = sb("thr", [128, 32])
        mod = sb("mod", [128, 6 * D])
        lam = sb("lam", [128, 4])
        dagb = sb("dagb", [128, 128])
        for t_, s_, k_ in ((identb, c_identb, 'identb'), (identf, c_identf, 'identf'), (tri, c_tri, 'tri'),
                           (tristrict, c_tristrict, 'tristrict'), (maskml, c_maskml, 'maskml'),
                           (masku, c_masku, 'masku'), (alibi, c_alibi, 'alibi'), (alibi2, c_alibi2, 'alibi'), (sel, c_sel, 'sel'),
                           (pidx, c_pidx, 'pidx'), (iota, c_iota, 'iota'), (thr, c_thr, 'thr'),
                           (dagb, dag, 'dagb')):
            load(t_[:], s_, k_, 'd_const')
        op('dve', lambda: V.memset(onesf[:], 1.0), w=('onesf',))
        op('dve', lambda: V.memset(onesb[:], 1.0), w=('onesb',))

        with ExitStack() as p0:
            cc = sb("cc", [128, 8], st=p0)
            sc = sb("sc", [128, 8], st=p0)
            scb = sb("scb", [128, 8, 128], st=p0)
            bada = sb("bada", [128, 6 * D], st=p0)
            wst = [sb("wst%d" % i, [128, 8, 512], st=p0) for i in range(2)]
            lv = sb("lv", [128, 4, 64], st=p0)
            lt = sb("lt", [128, 2, 64], st=p0)
            ls = sb("ls", [128, 2], st=p0)
            psm = [ps("psm%d" % i, [128, 512], st=p0) for i in range(2)]
            load(cc[:], ccol, 'cc', 'd_c0')
            load(bada[:], b_ada, 'bada', 'd_c0')
            load(lv[:], lamv, 'lv', 'd_c0')
            op('act', lambda: A.activation(out=sc[:], in_=cc[:], func=AF.Silu), r=('cc',), w=('sc',))
            for k in range(8):
                op('dve', lambda k=k: V.tensor_copy(out=scb[:, k, :], in_=sc[:, k:k + 1].to_broadcast([128, 128])),
                   r=('sc',), w=('scb',))
            wv = w_ada.rearrange("(k p) n -> p k n", p=128)
            for g in range(12):
                b = g % 2
                load(wst[b][:], wv[:, :, g * 512:(g + 1) * 512], 'wst%d' % b, 'd_wst%d' % b)
                for k in range(8):
                    op('pe', lambda k=k, b=b: T.matmul(psm[b][:], lhsT=scb[:, k, :], rhs=wst[b][:, k, :],
                                                       start=(k == 0), stop=(k == 7)),
                       r=('scb', 'wst%d' % b), w=('psm%d' % b,))
                op('dve', lambda g=g, b=b: V.tensor_tensor(out=mod[:, g * 512:(g + 1) * 512], in0=psm[b][:],
                                                           in1=bada[:, g * 512:(g + 1) * 512], op=ALU.add),
                   r=('psm%d' % b, 'bada'), w=('mod',))
            for c0 in (1024, 4096):
                op('dve', lambda c0=c0: V.tensor_scalar(out=mod[:, c0:c0 + 1024], in0=mod[:, c0:c0 + 1024],
                                                        scalar1=1.0, scalar2=None, op0=ALU.add),
                   r=('mod',), w=('mod',))
            op('dve', lambda: V.tensor_tensor(out=lt[:, 0, :], in0=lv[:, 0, :], in1=lv[:, 1, :], op=ALU.mult),
               r=('lv',), w=('lt',))
            op('dve', lambda: V.tensor_tensor(out=lt[:, 1, :], in0=lv[:, 2, :], in1=lv[:, 3, :], op=ALU.mult),
               r=('lv',), w=('lt',))
            op('dve', lambda: V.tensor_reduce(out=ls[:], in_=lt[:], axis=AX.X, op=ALU.add), r=('lt',), w=('ls',))
            op('act', lambda: A.activation(out=ls[:], in_=ls[:], func=AF.Exp), r=('ls',), w=('ls',))
            op('dve', lambda: V.tensor_tensor(out=lam[:, 0:1], in0=ls[:, 0:1], in1=ls[:, 1:2], op=ALU.subtract),
               r=('ls',), w=('lam',))
            op('dve', lambda: V.tensor_scalar(out=lam[:, 0:1], in0=lam[:, 0:1], scalar1=0.2, scalar2=None,
                                              op0=ALU.add), r=('lam',), w=('lam',))
            op('dve', lambda: V.tensor_scalar(out=lam[:, 1:2], in0=lam[:, 0:1], scalar1=-1.0, scalar2=None,
                                              op0=ALU.mult), r=('lam',), w=('lam',))
            op('dve', lambda: V.tensor_scalar(out=dagb[:], in0=dagb[:], scalar1=0.8, scalar2=None, op0=ALU.mult),
               r=('dagb',), w=('dagb',))
            if dbg:
                op('sp', lambda: SP.dma_start(out=dbgs['mod'], in_=mod[:]), r=('mod',), w=('dbgmod',), dma='d_dbg')
            P.barrier()

        if STOP >= 1:
            PHASES(nc, P, locals())
        P.finish()
    return nc


def PHASES(nc, P, L):
    es = L['es']; op = P.op; sb = L['sb']; ps = L['ps']; load = L['load']
    V = nc.vector; A = nc.scalar; G = nc.gpsimd; T = nc.tensor; SP = nc.sync
    S = L['S']; NBLK = L['NBLK']; NP = L['NP']; TOWN = L['TOWN']; NSB = L['NSB']; NB = L['NB']
    dbg = L['dbg']; dbgs = L['dbgs']
    identb, identf, tri, tristrict, onesf, onesb = L['identb'], L['identf'], L['tri'], L['tristrict'], L['onesf'], L['onesb']
    alibi2 = L['alibi2']
    maskml, masku, alibi, sel, pidx, iota, thr, mod, lam, dagb = (L['maskml'], L['masku'], L['alibi'], L['sel'],
                                                                  L['pidx'], L['iota'], L['thr'], L['mod'], L['lam'], L['dagb'])
    x, xo, w_in, out = L['x'], L['xo'], L['w_in'], L['out']
    KT_s, QT_s, V_s, MIX_s, H2_s, ACC_s, XS_s, YS_s = (L['KT_s'], L['QT_s'], L['V_s'], L['MIX_s'], L['H2_s'],
                                                       L['ACC_s'], L['XS_s'], L['YS_s'])
    SHIFT_A, SCALE_A, GATE_A, SHIFT_F, SCALE_F, GATE_F = [mod[:, i * D:(i + 1) * D] for i in range(6)]

    def rstd_from_ss(ssap, n, outap, rkey, wkey, eng='dve'):
        op(eng, lambda: V.tensor_scalar(out=outap, in0=ssap, scalar1=1.0 / n, scalar2=EPS, op0=ALU.mult, op1=ALU.add),
           r=(rkey,), w=(wkey,))
        op('act', lambda: A.activation(out=outap, in_=outap, func=AF.Sqrt), r=(wkey,), w=(wkey,))
        op(eng, lambda: V.reciprocal(out=outap, in_=outap), r=(wkey,), w=(wkey,))

    with ExitStack() as p1:
        winb = sb("winb", [128, 8, DIN], BF16, st=p1)
        with ExitStack() as p1w:
            wstg = [sb("wstg%d" % i, [128, DIN], st=p1w) for i in range(2)]
            for k in range(8):
                b = k % 2
                load(wstg[b][:], w_in[k * 128:(k + 1) * 128, :], 'wstg%d' % b, 'd_wstg%d' % b)
                e_ = 'dve' if b == 0 else 'pool'
                op(e_, lambda k=k, b=b, e_=e_: P.E[e_].tensor_copy(out=winb[:, k, :], in_=wstg[b][:]),
                   r=('wstg%d' % b,), w=('winb',))
            P.barrier()
        cw = sb("cw", [128, 8, 4], st=p1); cb = sb("cb", [128, 8], st=p1); gb = sb("gb", [128, 8], st=p1)
        mlgb = sb("mlgb", [128, 512], st=p1); qgb = sb("qgb", [128, 64], st=p1); kgb = sb("kgb", [128, 64], st=p1)
        load(cw[:], L['convw'], 'cw', 'd_c1'); load(cb[:], L['convb'], 'cb', 'd_c1'); load(gb[:], L['gate_b'], 'gb', 'd_c1')
        load(mlgb[:], L['mlg'], 'mlgb', 'd_c1'); load(qgb[:], L['qg'], 'qgb', 'd_c1'); load(kgb[:], L['kg'], 'kgb', 'd_c1')
        op('dve', lambda: V.tensor_scalar(out=qgb[:], in0=qgb[:], scalar1=0.125, scalar2=None, op0=ALU.mult),
           r=('qgb',), w=('qgb',))
        xb = [sb("xb%d" % i, [128, D], st=p1) for i in range(2)]
        junk = sb("junk", [128, D], BF16, st=p1)
        ss = sb("ss", [128, 8], st=p1)
        hn = sb("hn", [128, D], st=p1)
        hb = sb("hb", [128, D], BF16, st=p1)
        hT = sb("hT", [128, 8, 512], BF16, st=p1)
        raw = sb("raw", [128, 8, 516], st=p1)
        cacc = sb("cacc", [128, 512], st=p1)
        qkT_all = sb("qkT", [128, 2, 8, 512], BF16, st=p1)
        vaug_all = sb("vaug", [128, 2, 4, 4 * 129], BF16, st=p1)
        gsig_all = sb("gsig", [128, 2, 4, 512], st=p1)
        gcol_all = sb("gcol", [128, 2, 4, 8], st=p1)
        lf = sb("lf", [128, 4, 4], st=p1)
        gg = sb("gg", [128, 16], st=p1); eg = sb("eg", [128, 16], st=p1); eb = sb("eb", [128, 16], st=p1)
        wk = sb("wk", [128, 16], st=p1); carry = sb("carry", [128, 16], st=p1)
        tq = sb("tq", [128, 512], st=p1); tq2 = sb("tq2", [128, 512], st=p1)
        qn = sb("qn", [128, 512], BF16, st=p1); kn = sb("kn", [128, 512], BF16, st=p1)
        ss8 = sb("ss8", [128, 16], st=p1)
        qTp = sb("qTp", [128, 2, 4, 128], BF16, st=p1)
        qTo = sb("qTo", [128, 4, 128], BF16, st=p1)
        qTt = sb("qTt", [128, 4, 128], st=p1)
        kTs = sb("kTs", [128, 4, 128], BF16, st=p1)
        vda = sb("vda", [128, 4, 129], BF16, st=p1)
        Cst = sb("Cst", [128, 4, 129], st=p1)
        Cbf = sb("Cbf", [128, 4, 129], BF16, st=p1)
        sTb2 = sb("sTb", [128, 4, 128], BF16, st=p1)
        kwb2 = sb("kwb", [128, 4, 128], BF16, st=p1)
        sm2 = sb("sm", [128, 4, 8], st=p1)
        hbuf2 = sb("hbuf", [128, 4, 128], st=p1)
        junkB = sb("junkB", [128, 4, 128], BF16, st=p1)
        hm = sb("hm", [128, 2, 512], st=p1)
        hmo = sb("hmo", [128, 512], st=p1)
        hmb = sb("hmb", [128, 512], BF16, st=p1)
        hmT = sb("hmT", [128, 4, 128], BF16, st=p1)
        pT = ps("pT", [128, 1024], BF16, st=p1)
        pfm = [ps("pfm%d" % i, [128, 512], st=p1) for i in range(2)]
        ptm = [ps("ptm%d" % i, [128, 512], st=p1) for i in range(2)]
        pml = ps("pml", [128, 512], st=p1)
        pn = ps("pn", [128, 512], st=p1)
        pc = ps("pc", [128, 512], st=p1)
        op('dve', lambda: V.memset(raw[:], 0.0), w=tuple('raw%d' % g for g in range(8)))
        op('dve', lambda: V.memset(Cst[:], 0.0), w=tuple('Cst%d' % g for g in range(4)))
        op('dve', lambda: V.memset(Cbf[:], 0.0), w=tuple('Cbf%d' % g for g in range(4)))
        op('dve', lambda: V.memset(vaug_all[:], 1.0), w=tuple('vaug%d_%d' % (g, q) for g in range(4) for q in range(2)))
        op('dve', lambda: V.memset(vda[:], 1.0), w=('vda',))
        nxb = 0

        def stageA(sbi):
            nonlocal nxb
            bp = sbi % 2; kp = '_%d' % bp
            qkT = qkT_all[:, bp]; gsig = gsig_all[:, bp]; gcol = gcol_all[:, bp]
            vaug = vaug_all[:, bp].rearrange("p a (h e) -> p a h e", h=4)
            for bi in range(4):
                blk = sbi * 4 + bi
                xt = xb[nxb % 2]; xk = 'xb%d' % (nxb % 2); nxb += 1
                load(xt[:], x[blk * 128:(blk + 1) * 128, :], xk, 'd_' + xk)
                op('act', lambda xt=xt: A.activation(out=junk[:], in_=xt[:], func=AF.Square, accum_out=ss[:, 0:1]),
                   r=(xk,), w=('junk', 'ss'))
                rstd_from_ss(ss[:, 0:1], D, ss[:, 1:2], 'ss', 'ss1')
                op('dve', lambda xt=xt: V.scalar_tensor_tensor(out=hn[:], in0=xt[:], scalar=ss[:, 1:2], in1=SCALE_A,
                                                               op0=ALU.mult, op1=ALU.mult), r=(xk, 'ss1', 'mod'), w=('hn',))
                op('pool', lambda: G.tensor_tensor(out=hb[:], in0=hn[:], in1=SHIFT_A, op=ALU.add), r=('hn', 'mod'), w=('hb',))
                yield
                for hf in range(2):
                    for k in range(4):
                        op('pe', lambda k=k, hf=hf: T.transpose(out=pT[:, k * 128:(k + 1) * 128],
                                                                in_=hb[:, (hf * 4 + k) * 128:(hf * 4 + k + 1) * 128],
                                                                identity=identb[:]), r=('hb', 'identb'), w=('pT',))
                    op('act', lambda bi=bi, hf=hf: A.activation(out=hT[:, hf * 4:(hf + 1) * 4, bi * 128:(bi + 1) * 128],
                                                                in_=pT[:, 0:512].rearrange("p (k t) -> p k t", k=4), func=AF.Copy),
                       r=('pT',), w=('hT',))
                    yield
            for g in range(8):
                pf = pfm[0]; pk = 'pfm0'
                for k in range(8):
                    op('pe', lambda g=g, k=k, pf=pf: T.matmul(pf[:], lhsT=winb[:, k, g * 128:(g + 1) * 128], rhs=hT[:, k, :],
                                                              start=(k == 0), stop=(k == 7)), r=('winb', 'hT'), w=(pk,))
                op('act', lambda g=g, pf=pf: A.activation(out=raw[:, g, 3:515], in_=pf[:], func=AF.Copy), r=(pk,), w=('raw%d' % g,))
                yield
                op('dve', lambda g=g: V.tensor_scalar(out=cacc[:], in0=raw[:, g, 3:515], scalar1=cw[:, g, 3:4],
                                                      scalar2=cb[:, g:g + 1], op0=ALU.mult, op1=ALU.add),
                   r=('raw%d' % g, 'cw', 'cb'), w=('cacc',))
                for j in range(3):
                    op('dve', lambda g=g, j=j: V.scalar_tensor_tensor(out=cacc[:], in0=raw[:, g, j:j + 512],
                                                                      scalar=cw[:, g, j:j + 1], in1=cacc[:],
                                                                      op0=ALU.mult, op1=ALU.add),
                       r=('raw%d' % g, 'cw', 'cacc'), w=('cacc',))
                yield
                op('act', lambda g=g: A.activation(out=qkT[:, g, :], in_=cacc[:], func=AF.Silu), r=('cacc',), w=('qkT%d' % g + kp,))
                op('pool', lambda g=g: G.tensor_copy(out=raw[:, g, 0:3], in_=raw[:, g, 512:515]), r=('raw%d' % g,), w=('raw%d' % g,))
                yield
            groups = [(1024, 512), (1536, 512), (2048, 8), (2056, 512), (2568, 512), (3080, 512)]
            npt = 0
            for bi in range(4):
                blk = sbi * 4 + bi
                par = blk % 2
                pair = blk // 2

                def proj(gi):
                    nonlocal npt
                    c0, wd = groups[gi]
                    pt_ = ptm[npt % 2]; pk_ = 'ptm%d' % (npt % 2); npt += 1
                    for k in range(8):
                        op('pe', lambda k=k: T.matmul(pt_[:, 0:wd], lhsT=hT[:, k, bi * 128:(bi + 1) * 128],
                                                      rhs=winb[:, k, c0:c0 + wd], start=(k == 0), stop=(k == 7)),
                           r=('hT', 'winb'), w=(pk_,))
                    return pt_, pk_
                pt_, pk_ = proj(0)
                op('act', lambda pt_=pt_: A.activation(out=vaug[:, bi, :, 0:128], in_=pt_[:].rearrange("p (h e) -> p h e", h=4),
                                                       func=AF.Copy), r=(pk_,), w=('vaug%d' % bi + kp,))
                yield
                pt_, pk_ = proj(1)
                op('act', lambda pt_=pt_: A.activation(out=gsig[:, bi, :], in_=pt_[:], func=AF.Sigmoid), r=(pk_,), w=('gsig%d' % bi + kp,))
                op('pool', lambda: G.tensor_tensor(out=gsig[:, bi, :], in0=gsig[:, bi, :], in1=mlgb[:], op=ALU.mult),
                   r=('gsig%d' % bi + kp, 'mlgb'), w=('gsig%d' % bi + kp,))
                yield
                pt_, pk_ = proj(2)
                op('dve', lambda pt_=pt_: V.tensor_tensor(out=gcol[:, bi, :], in0=pt_[:, 0:8], in1=gb[:], op=ALU.add),
                   r=(pk_, 'gb'), w=('gcol' + kp,))
                for which, gi, gn, dst in (('q', 3, qgb, qn), ('k', 4, kgb, kn)):
                    yield
                    pt_, pk_ = proj(gi)
                    so = 0 if which == 'q' else 8
                    op('act', lambda pt_=pt_: A.activation(out=tq[:], in_=pt_[:], func=AF.Square), r=(pk_,), w=('tq',))
                    op('dve', lambda so=so: V.tensor_reduce(out=ss8[:, so:so + 8], in_=tq[:].rearrange("p (a d) -> p a d", d=64),
                                                            axis=AX.X, op=ALU.add), r=('tq',), w=('ss8',))
                    rstd_from_ss(ss8[:, so:so + 8], 64, ss8[:, so:so + 8], 'ss8', 'ss8')
                    yield
                    op('dve', lambda pt_=pt_, so=so: V.tensor_tensor(
                        out=tq2[:].rearrange("p (a d) -> p a d", d=64), in0=pt_[:].rearrange("p (a d) -> p a d", d=64),
                        in1=ss8[:, so:so + 8].rearrange("p (a o) -> p a o", o=1).to_broadcast([128, 8, 64]), op=ALU.mult),
                       r=(pk_, 'ss8'), w=('tq2',))
                    op('pool', lambda gn=gn, dst=dst: G.tensor_tensor(
                        out=dst[:].rearrange("p (a d) -> p a d", d=64), in0=tq2[:].rearrange("p (a d) -> p a d", d=64),
                        in1=gn[:].rearrange("p (o d) -> p o d", o=1).to_broadcast([128, 8, 64]), op=ALU.mult),
                       r=('tq2', 'qgb', 'kgb'), w=(which + 'n',))
                    yield
                    for h in range(4):
                        op('pe', lambda h=h, dst=dst: T.transpose(out=pT[:, h * 128:(h + 1) * 128], in_=dst[:, h * 128:(h + 1) * 128],
                                                                  identity=identb[:]), r=(which + 'n', 'identb'), w=('pT',))
                    if which == 'q':
                        op('act', lambda: A.activation(out=qTp[:, par, :, :], in_=pT[:, 0:512].rearrange("p (h t) -> p h t", h=4),
                                                       func=AF.Copy), r=('pT',), w=('qTp',))
                    else:
                        op('act', lambda: A.activation(out=kTs[:], in_=pT[:, 0:512].rearrange("p (h t) -> p h t", h=4),
                                                       func=AF.Copy), r=('pT',), w=('kTs',))
                        op('sp', lambda: SP.dma_start(out=KT_s[:, :, blk * 128:(blk + 1) * 128].rearrange("h p t -> p h t"),
                                                      in_=kTs[:]), r=('kTs',), w=('KT_s',), dma='d_kts')
                if par == 1:
                    op('dve', lambda: V.tensor_scalar(out=qTt[:], in0=qTp[:, 0, :, :], scalar1=sel[:, 0:1], scalar2=None,
                                                      op0=ALU.mult), r=('qTp', 'sel'), w=('qTt',))
                    op('dve', lambda: V.scalar_tensor_tensor(out=qTo[:], in0=qTp[:, 1, :, :], scalar=sel[:, 1:2], in1=qTt[:],
                                                             op0=ALU.mult, op1=ALU.add), r=('qTp', 'sel', 'qTt'), w=('qTo',))
                    op('sp', lambda: SP.dma_start(out=QT_s[:, :, pair * 128:(pair + 1) * 128].rearrange("h p t -> p h t"),
                                                  in_=qTo[:]), r=('qTo',), w=('QT_s',), dma='d_qts')
                yield
                pt_, pk_ = proj(5)
                op('act', lambda pt_=pt_: A.activation(out=vda[:, :, 0:128], in_=pt_[:].rearrange("p (h e) -> p h e", h=4),
                                                       func=AF.Copy), r=(pk_,), w=('vda',))
                op('sp', lambda: SP.dma_start(out=V_s[:, :, blk, :].rearrange("h p e -> p h e"), in_=vda[:]),
                   r=('vda',), w=('V_s',), dma='d_vs')
                yield
        def stageB(sbi):
            bp = sbi % 2; kp = '_%d' % bp
            qkT = qkT_all[:, bp]; gsig = gsig_all[:, bp]; gcol = gcol_all[:, bp]
            vaug = vaug_all[:, bp].rearrange("p a (h e) -> p a h e", h=4)
            fpre = gcol[:, :, 4:8]
            op('act', lambda: A.activation(out=lf[:], in_=fpre, func=AF.Exp, scale=-1.0), r=('gcol' + kp,), w=('lf',))
            op('act', lambda: A.activation(out=lf[:], in_=lf[:], func=AF.Ln, bias=1.0), r=('lf',), w=('lf',))
            op('dve', lambda: V.tensor_scalar(out=lf[:], in0=lf[:], scalar1=-1.0, scalar2=None, op0=ALU.mult), r=('lf',), w=('lf',))
            lf2 = lf[:].rearrange("p a h -> p (a h)")
            op('pe', lambda: T.matmul(pn[:, 400:416], lhsT=tri[:], rhs=lf2, start=True, stop=True), r=('tri', 'lf'), w=('bk1',))
            op('pe', lambda: T.matmul(pn[:, 416:432], lhsT=onesf[:], rhs=lf2, start=True, stop=True), r=('onesf', 'lf'), w=('bk1',))
            op('dve', lambda: V.tensor_tensor(out=gg[:].rearrange("p (a h) -> p a h", h=4), in0=gcol[:, :, 0:4],
                                              in1=pn[:, 400:416].rearrange("p (a h) -> p a h", h=4), op=ALU.subtract),
               r=('gcol' + kp, 'bk1'), w=('gg',))
            op('act', lambda: A.activation(out=eg[:], in_=gg[:], func=AF.Exp), r=('gg',), w=('eg',))
            op('act', lambda: A.activation(out=eb[:], in_=pn[:, 400:416], func=AF.Exp), r=('bk1',), w=('eb',))
            op('act', lambda: A.activation(out=carry[:], in_=pn[:, 416:432], func=AF.Exp), r=('bk1',), w=('carry',))
            op('dve', lambda: V.tensor_tensor(out=wk[:], in0=gg[:], in1=pn[:, 416:432], op=ALU.add), r=('gg', 'bk1'), w=('wk',))
            op('act', lambda: A.activation(out=wk[:], in_=wk[:], func=AF.Exp, bias=-2.4260151319598084), r=('wk',), w=('wk',))
            yield
            for bi in range(4):
                blk = sbi * 4 + bi
                par = blk % 2
                pair = blk // 2
                tsl = slice(bi * 128, (bi + 1) * 128)
                def headgen(h, sl):
                    col = bi * 4 + h
                    qT_ = qkT[:, h, tsl]
                    kT_ = qkT[:, 4 + h, tsl]
                    ks = '_s%d' % sl
                    sTb_ = sTb2[:, sl, :]; kwb_ = kwb2[:, sl, :]; sm_ = sm2[:, sl, :]; hbuf_ = hbuf2[:, sl, :]
                    bank_ = (pml, pn, pc, pfm[1])[sl]
                    bk = 'bk%d' % sl
                    pS_ = bank_[:, 0:128]
                    pK_ = pT[:, 512 + sl * 128:512 + (sl + 1) * 128]
                    pn_ = bank_[:, 128:257]
                    pc_ = bank_[:, 260:389]
                    op('pe', lambda: T.matmul(pS_, lhsT=kT_, rhs=qT_, start=True, stop=True),
                       r=('qkT%d' % h + kp, 'qkT%d' % (4 + h) + kp), w=(bk,))
                    op('pe', lambda: T.transpose(out=pK_, in_=kT_, identity=identb[:]), r=('qkT%d' % (4 + h) + kp, 'identb'), w=('pT',))
                    yield
                    op('dve', lambda: V.scalar_tensor_tensor(out=sTb_, in0=pS_, scalar=eg[:, col:col + 1], in1=maskml[:],
                                                             op0=ALU.mult, op1=ALU.mult), r=(bk, 'eg', 'maskml'), w=('sTb' + ks,))
                    op('act', lambda: A.activation(out=kwb_, in_=pK_, func=AF.Copy, scale=wk[:, col:col + 1]),
                       r=('pT', 'wk'), w=('kwb' + ks,))
                    yield
                    op('pe', lambda: T.matmul(pn_, lhsT=sTb_, rhs=vaug[:, bi, h, :], start=True, stop=False),
                       r=('sTb' + ks, 'vaug%d' % bi + kp), w=(bk,))
                    op('pe', lambda: T.matmul(pn_, lhsT=qT_, rhs=Cbf[:, h, :], start=False, stop=True),
                       r=('qkT%d' % h + kp, 'Cbf%d' % h), w=(bk,))
                    op('pe', lambda: T.matmul(pc_, lhsT=kwb_, rhs=vaug[:, bi, h, :], start=True, stop=True),
                       r=('kwb' + ks, 'vaug%d' % bi + kp), w=(bk,))
                    yield
                    op('dve', lambda: V.tensor_scalar(out=sm_[:, 0:1], in0=pn_[:, 128:129], scalar1=eb[:, col:col + 1], scalar2=None,
                                                      op0=ALU.mult), r=(bk, 'eb'), w=('sm' + ks,))
                    op('dve', lambda: V.scalar_tensor_tensor(out=Cst[:, h, :], in0=Cst[:, h, :], scalar=carry[:, col:col + 1],
                                                             in1=pc_, op0=ALU.mult, op1=ALU.add),
                       r=('Cst%d' % h, 'carry', bk), w=('Cst%d' % h,))
                    op('pool', lambda: G.tensor_copy(out=Cbf[:, h, :], in_=Cst[:, h, :]), r=('Cst%d' % h,), w=('Cbf%d' % h,))
                    yield
                    op('dve', lambda: V.scalar_tensor_tensor(out=sm_[:, 1:2], in0=sm_[:, 0:1], scalar=-1.0, in1=sm_[:, 0:1],
                                                             op0=ALU.mult, op1=ALU.max), r=('sm' + ks,), w=('sm' + ks,))
                    yield
                    op('dve', lambda: V.tensor_scalar(out=sm_[:, 1:2], in0=sm_[:, 1:2], scalar1=1.0, scalar2=None,
                                                      op0=ALU.max), r=('sm' + ks,), w=('sm' + ks,))
                    yield
                    op('dve', lambda: V.reciprocal(out=sm_[:, 1:2], in_=sm_[:, 1:2]), r=('sm' + ks,), w=('sm' + ks,))
                    yield
                    op('dve', lambda: V.tensor_tensor(out=sm_[:, 2:3], in0=sm_[:, 1:2], in1=eb[:, col:col + 1], op=ALU.mult),
                       r=('sm' + ks, 'eb'), w=('sm' + ks,))
                    yield
                    op('act', lambda: A.activation(out=hbuf_, in_=pn_[:, 0:128], func=AF.Copy, scale=sm_[:, 2:3]),
                       r=(bk, 'sm' + ks), w=('hbuf' + ks,))
                    yield
                    op('act', lambda: A.activation(out=junkB[:, sl, :], in_=hbuf_, func=AF.Square, accum_out=sm_[:, 3:4]),
                       r=('hbuf' + ks,), w=('junkB' + ks, 'sm3' + ks))
                    yield
                    op('dve', lambda: V.tensor_scalar(out=sm_[:, 4:5], in0=sm_[:, 3:4], scalar1=1.0 / 128, scalar2=EPS, op0=ALU.mult, op1=ALU.add),
                       r=('sm3' + ks,), w=('sm4' + ks,))
                    yield
                    op('act', lambda: A.activation(out=sm_[:, 4:5], in_=sm_[:, 4:5], func=AF.Sqrt), r=('sm4' + ks,), w=('sm4' + ks,))
                    yield
                    op('dve', lambda: V.reciprocal(out=sm_[:, 4:5], in_=sm_[:, 4:5]), r=('sm4' + ks,), w=('sm4' + ks,))
                    yield
                    op('dve', lambda: V.scalar_tensor_tensor(out=hm[:, par, h * 128:(h + 1) * 128], in0=hbuf_, scalar=sm_[:, 4:5],
                                                             in1=gsig[:, bi, h * 128:(h + 1) * 128], op0=ALU.mult, op1=ALU.mult),
                       r=('hbuf' + ks, 'sm4' + ks, 'gsig%d' % bi + kp), w=('hm',))

                gens = [headgen(h_, h_) for h_ in range(4)]
                while gens:
                    for g_ in list(gens):
                        try:
                            next(g_)
                        except StopIteration:
                            gens.remove(g_)
                    yield
                if par == 1:
                    op('dve', lambda: V.tensor_scalar(out=hmo[:], in0=hm[:, 0, :], scalar1=sel[:, 0:1], scalar2=None, op0=ALU.mult),
                       r=('hm', 'sel'), w=('hmo',))
                    op('dve', lambda: V.scalar_tensor_tensor(out=hmb[:], in0=hm[:, 1, :], scalar=sel[:, 1:2], in1=hmo[:],
                                                             op0=ALU.mult, op1=ALU.add), r=('hm', 'sel', 'hmo'), w=('hmb',))
                    for h in range(4):
                        op('pe', lambda h=h: T.transpose(out=pT[:, h * 128:(h + 1) * 128], in_=hmb[:, h * 128:(h + 1) * 128],
                                                         identity=identb[:]), r=('hmb', 'identb'), w=('pT',))
                    op('act', lambda: A.activation(out=hmT[:], in_=pT[:, 0:512].rearrange("p (h t) -> p h t", h=4), func=AF.Copy),
                       r=('pT',), w=('hmT',))
                    op('sp', lambda: SP.dma_start(out=MIX_s[:, 0:4, pair * 128:(pair + 1) * 128], in_=hmT[:]),
                       r=('hmT',), w=('MIX_s',), dma='d_mixs')
                yield

        def run_il(ga, gb_):
            alive = [g_ for g_ in (ga, gb_) if g_ is not None]
            while alive:
                for g_ in list(alive):
                    try:
                        next(g_)
                    except StopIteration:
                        alive.remove(g_)
        run_il(stageA(0), None)
        for sbi in range(NSB):
            run_il(stageA(sbi + 1) if sbi + 1 < NSB else None, stageB(sbi))
        P.barrier()

    if STOP < 2:
        return
    with ExitStack() as p2:
        KT = [sb("KT%d" % i, [128, S], BF16, st=p2) for i in range(2)]
        VV = [sb("VV%d" % i, [128, NBLK, 129], BF16, st=p2) for i in range(2)]
        QT = [[sb("QT%d_%d" % (i, m), [128, TOWN], BF16, st=p2) for m in range(2)] for i in range(2)]
        for i in range(2):
            for m in range(2):
                op('dve', lambda i=i, m=m: V.memset(QT[i][m][:], 0.0), w=('QT%d' % i,))
        pT_ = [sb("pTb%d" % i, [128, 512], BF16, st=p2) for i in range(3)]
        o1 = sb("o1", [128, 128], st=p2); oo = sb("oo", [128, 128], st=p2)
        hdb = sb("hdb", [128, 128], BF16, st=p2); hdT = sb("hdT", [128, 128], BF16, st=p2)
        sd = sb("sd", [128, 8], st=p2); junk2 = sb("junk2", [128, 128], BF16, st=p2)
        psS = [ps("psS%d" % i, [128, 512], st=p2) for i in range(3)]
        psO1 = [ps("psO1_%d" % i, [128, 512], st=p2) for i in range(2)]
        psO2 = [ps("psO2_%d" % i, [128, 512], st=p2) for i in range(2)]
        psX = ps("psX", [128, 1024], BF16, st=p2)
        it = 0
        for h in range(4):
            hb_ = h % 2
            load(KT[hb_][:], KT_s[h], 'KT%d' % hb_, 'd_KT%d' % hb_)
            load(VV[hb_][:], V_s[h], 'VV%d' % hb_, 'd_VV%d' % hb_)
            for m in range(2):
                op('sp', lambda m=m: SP.dma_start(out=QT[hb_][m][m * 64:(m + 1) * 64, :], in_=QT_s[h, m * 64:(m + 1) * 64, :]),
                   r=(), w=('QT%d' % hb_,), dma='d_QT%d' % hb_)
            if h == 0:
                items = [('S', p, j, alibi) for p in range(NP) for j in range(2 * p + 2)]
            else:
                items = []
                for q in range(NP // 2):
                    p0, p1 = 2 * q, 2 * q + 1
                    for j in range(2 * p0 + 2):
                        items.append(('P', p0, p1, j))
                    for j in (2 * p0 + 2, 2 * p0 + 3):
                        items.append(('S', p1, j, alibi2))
            LOOK = 2
            bufof = {}

            def emit_S(ix):
                nonlocal it
                ent = items[ix]
                sbuf_i = it % 3; it += 1
                bufof[ix] = sbuf_i
                pS = psS[sbuf_i]; pSk = 'psS%d' % sbuf_i; pb = pT_[sbuf_i]; pbk = 'pTb%d' % sbuf_i
                if ent[0] == 'P':
                    _, p0, p1, j = ent
                    slots = ((p0, 0), (p1, 256)); p = p0; tab = alibi; wd = 512
                else:
                    _, p, j, tab = ent
                    slots = ((p, 0),); wd = 256
                u = 2 * p + 1 - j
                for (sp_, coff) in slots:
                    for m in range(2):
                        op('pe', lambda m=m, sp_=sp_, coff=coff: T.matmul(
                            pS[:, coff + m * 128:coff + (m + 1) * 128], lhsT=KT[hb_][:, j * 128:(j + 1) * 128],
                            rhs=QT[hb_][m][:, sp_ * 128:(sp_ + 1) * 128], start=True, stop=True),
                           r=('KT%d' % hb_, 'QT%d' % hb_), w=(pSk,))
                op('act', lambda: A.activation(out=pb[:, 0:wd], in_=pS[:, 0:wd], func=AF.Exp, bias=tab[:, h, u:u + 1]),
                   r=(pSk, 'alibi'), w=(pbk,))
                if u <= 1:
                    op('dve', lambda: V.tensor_tensor(out=pb[:, 0:256].rearrange("p (m t) -> p m t", m=2),
                                                      in0=pb[:, 0:256].rearrange("p (m t) -> p m t", m=2),
                                                      in1=masku[:, u:u + 1, :].to_broadcast([128, 2, 128]), op=ALU.mult),
                       r=(pbk, 'masku'), w=(pbk,))

            def emit_AV(ix):
                ent = items[ix]
                sbuf_i = bufof.pop(ix)
                pb = pT_[sbuf_i]; pbk = 'pTb%d' % sbuf_i
                if ent[0] == 'P':
                    _, p0, p1, j = ent
                    slots = ((p0, 0), (p1, 256))
                else:
                    _, p0, j, _t = ent
                    slots = ((p0, 0),)
                done = []
                for (sp_, coff) in slots:
                    ob = sp_ % 2
                    last = (j == 2 * sp_ + 1)
                    op('pe', lambda: T.matmul(psO1[ob][:, 0:129], lhsT=pb[:, coff:coff + 128], rhs=VV[hb_][:, j, :], start=(j == 0), stop=last),
                       r=(pbk, 'VV%d' % hb_), w=('psO1_%d' % ob,))
                    op('pe', lambda: T.matmul(psO2[ob][:, 0:129], lhsT=pb[:, coff + 128:coff + 256], rhs=VV[hb_][:, j, :], start=(j == 0), stop=last),
                       r=(pbk, 'VV%d' % hb_), w=('psO2_%d' % ob,))
                    if last:
                        done.append(sp_)
                for sp_ in done:
                    epilogue(sp_)

            def epilogue(p):
                ob = p % 2
                op('dve', lambda: V.reciprocal(out=sd[:, 0:1], in_=psO1[ob][:, 128:129]), r=('psO1_%d' % ob,), w=('sd',))
                op('dve', lambda: V.reciprocal(out=sd[:, 1:2], in_=psO2[ob][:, 128:129]), r=('psO2_%d' % ob,), w=('sd',))
                op('dve', lambda: V.tensor_tensor(out=sd[:, 2:3], in0=sd[:, 1:2], in1=lam[:, 1:2], op=ALU.mult), r=('sd', 'lam'), w=('sd',))
                op('dve', lambda: V.tensor_scalar(out=o1[:], in0=psO1[ob][:, 0:128], scalar1=sd[:, 0:1], scalar2=None, op0=ALU.mult),
                   r=('psO1_%d' % ob, 'sd'), w=('o1',))
                op('dve', lambda: V.scalar_tensor_tensor(out=oo[:], in0=psO2[ob][:, 0:128], scalar=sd[:, 2:3], in1=o1[:],
                                                         op0=ALU.mult, op1=ALU.add), r=('psO2_%d' % ob, 'sd', 'o1'), w=('oo',))
                op('dve', lambda: V.tensor_tensor(out=o1[:], in0=oo[:], in1=oo[:], op=ALU.mult), r=('oo',), w=('o1',))
                op('dve', lambda: V.tensor_reduce(out=sd[:, 3:4], in_=o1[:], axis=AX.X, op=ALU.add), r=('o1',), w=('sd3',))
                op('dve', lambda: V.tensor_scalar(out=sd[:, 4:5], in0=sd[:, 3:4], scalar1=1.0 / 128, scalar2=EPS, op0=ALU.mult, op1=ALU.add),
                   r=('sd3',), w=('sd4',))
                op('act', lambda: A.activation(out=sd[:, 4:5], in_=sd[:, 4:5], func=AF.Ln), r=('sd4',), w=('sd4',))
                op('act', lambda: A.activation(out=sd[:, 4:5], in_=sd[:, 4:5], func=AF.Exp, scale=-0.5), r=('sd4',), w=('sd4',))
                op('dve', lambda: V.scalar_tensor_tensor(out=hdb[:], in0=oo[:], scalar=sd[:, 4:5], in1=dagb[:], op0=ALU.mult, op1=ALU.mult),
                   r=('oo', 'sd4', 'dagb'), w=('hdb',))
                op('pe', lambda: T.transpose(out=psX[:, 0:128], in_=hdb[:], identity=identb[:]), r=('hdb', 'identb'), w=('psX',))
                op('dve', lambda: V.tensor_copy(out=hdT[:], in_=psX[:, 0:128]), r=('psX',), w=('hdT',))
                op('sp', lambda: SP.dma_start(out=MIX_s[:, 4 + h, p * 128:(p + 1) * 128], in_=hdT[:]), r=('hdT',), w=('MIX_s',), dma='d_mixs2')

            for ix in range(len(items) + LOOK):
                if ix < len(items):
                    emit_S(ix)
                if ix - LOOK >= 0:
                    emit_AV(ix - LOOK)
        if dbg:
            op('sp', lambda: SP.dma_start(out=dbgs['mix'], in_=MIX_s), r=('MIX_s',), w=('dbgmix',), dma='d_dbg')
        P.barrier()

    if STOP >= 3:
        PHASES2(nc, P, L, locals())


def PHASES2(nc, P, L, L2):
    es = L['es']; op = P.op; sb = L['sb']; ps = L['ps']; load = L['load']
    V = nc.vector; A = nc.scalar; G = nc.gpsimd; T = nc.tensor; SP = nc.sync
    S = L['S']; NBLK = L['NBLK']; NP = L['NP']; TOWN = L['TOWN']; NB = L['NB']
    dbg = L['dbg']; dbgs = L['dbgs']
    identb, identf, tri, tristrict, onesf, onesb = L['identb'], L['identf'], L['tri'], L['tristrict'], L['onesf'], L['onesb']
    pidx, iota, thr, mod = L['pidx'], L['iota'], L['thr'], L['mod']
    xo, out = L['xo'], L['out']
    MIX_s, H2_s, ACC_s, XS_s, YS_s = L['MIX_s'], L['H2_s'], L['ACC_s'], L['XS_s'], L['YS_s']
    SHIFT_A, SCALE_A, GATE_A, SHIFT_F, SCALE_F, GATE_F = [mod[:, i * D:(i + 1) * D] for i in range(6)]
    rstd_from_ss = L2['rstd_from_ss']

    with ExitStack() as p3:
        DESTI = sb("DESTI", [128, NP, 8], I32, st=p3)
        GATE8 = sb("GATE8", [128, NP, 8], st=p3)
        WIDX = sb("WIDX", [128, NB], I32, st=p3)
        cntbc = sb("cntbc", [128, NE], st=p3)
        p34 = ExitStack()
        RANK = sb("RANK", [128, NP, NE], st=p34)
        GD = sb("GD", [128, NP, NE], st=p34)
        with ExitStack() as p3a:
            woutb = sb("woutb", [128, 8, D], BF16, st=p3a)
            wrt = sb("wrt", [128, 8, NE], st=p3a)
            ws1b = sb("ws1b", [128, 8, 256], BF16, st=p3a); ws3b = sb("ws3b", [128, 8, 256], BF16, st=p3a)
            ws2b = sb("ws2b", [128, 2, D], BF16, st=p3a)
            rb = sb("rb", [128, NE], st=p3a)
            load(rb[:], L['rbias'], 'rb', 'd_c3')
            load(wrt[:], L['w_router'].rearrange("(k p) n -> p k n", p=128), 'wrt', 'd_c3')
            with ExitStack() as p3w:
                stg = sb("stg", [128, 4, D], st=p3w)
                wov = L['w_out'].rearrange("(k p) n -> p k n", p=128)
                for hf in range(2):
                    load(stg[:], wov[:, hf * 4:(hf + 1) * 4, :], 'stg', 'd_stg')
                    op('dve', lambda hf=hf: V.tensor_copy(out=woutb[:, hf * 4:(hf + 1) * 4, :], in_=stg[:]), r=('stg',), w=('woutb',))
                stv = stg[:].rearrange("p a n -> p (a n)")[:, 0:2048].rearrange("p (k n) -> p k n", k=8)
                load(stv, L['ws1'].rearrange("(k p) n -> p k n", p=128), 'stg', 'd_stg')
                op('dve', lambda: V.tensor_copy(out=ws1b[:], in_=stv), r=('stg',), w=('ws1b',))
                load(stv, L['ws3'].rearrange("(k p) n -> p k n", p=128), 'stg', 'd_stg')
                op('dve', lambda: V.tensor_copy(out=ws3b[:], in_=stv), r=('stg',), w=('ws3b',))
                load(stg[:, 0:2, :], L['ws2'].rearrange("(k p) n -> p k n", p=128), 'stg', 'd_stg')
                op('dve', lambda: V.tensor_copy(out=ws2b[:], in_=stg[:, 0:2, :]), r=('stg',), w=('ws2b',))
                P.barrier()
            xob = sb("xob", [128, D], st=p3a)
            mixT = sb("mixT", [128, 8, 128], BF16, st=p3a)
            x1 = sb("x1", [128, D], st=p3a)
            junk3 = sb("junk3", [128, D], BF16, st=p3a)
            s3 = sb("s3", [128, 8], st=p3a)
            h2n = sb("h2n", [128, D], st=p3a)
            h2 = sb("h2", [128, D], st=p3a)
            h2b = sb("h2b", [128, D], BF16, st=p3a)
            h2T = sb("h2T", [128, 8, 128], st=p3a)
            h2Tb = sb("h2Tb", [128, 8, 128], BF16, st=p3a)
            sg = sb("sg", [128, NE], st=p3a); selv = sb("selv", [128, NE], st=p3a)
            m8 = sb("m8", [128, 8, 8], st=p3a); gs = sb("gs", [128, 8], st=p3a); gm8 = sb("gm8", [128, 8], st=p3a)
            gmask = sb("gmask", [128, 8], st=p3a); gneg = sb("gneg", [128, 8], st=p3a)
            msk = sb("msk", [128, NE], st=p3a); t8 = sb("t8", [128, 8], st=p3a)
            mask8 = sb("mask8", [128, NE], st=p3a); mask8b = sb("mask8b", [128, NE], BF16, st=p3a)
            sil = sb("sil", [128, 256], st=p3a); actT = sb("actT", [128, 2, 128], BF16, st=p3a)
            accb = sb("accb", [128, D], st=p3a)
            pbig = ps("pbig", [128, 1024], st=p3a)
            pTf = ps("pTf", [128, 1024], st=p3a)
            pr = ps("pr", [128, 512], st=p3a)
            pg = ps("pg", [128, 512], st=p3a)
            pcn = ps("pcn", [128, 512], st=p3a)
            op('dve', lambda: V.memset(cntbc[:], 0.0), w=('cntbc',))
            for p in range(NP):
                tsl = slice(p * 128, (p + 1) * 128)
                load(xob[:], xo[tsl, :], 'xob', 'd_xob')
                load(mixT[:], MIX_s[:, :, tsl], 'mixT', 'd_mixT')
                for nh in range(2):
                    for k in range(8):
                        op('pe', lambda nh=nh, k=k: T.matmul(pbig[:, nh * 512:(nh + 1) * 512], lhsT=mixT[:, k, :],
                                                             rhs=woutb[:, k, nh * 512:(nh + 1) * 512], start=(k == 0), stop=(k == 7)),
                           r=('mixT', 'woutb'), w=('pbig',))
                op('dve', lambda: V.tensor_tensor(out=x1[:], in0=pbig[:], in1=GATE_A, op=ALU.mult), r=('pbig', 'mod'), w=('x1',))
                op('pool', lambda: G.tensor_tensor(out=x1[:], in0=x1[:], in1=xob[:], op=ALU.add), r=('x1', 'xob'), w=('x1',))
                op('act', lambda: A.activation(out=junk3[:], in_=x1[:], func=AF.Square, accum_out=s3[:, 0:1]), r=('x1',), w=('junk3', 's3'))
                rstd_from_ss(s3[:, 0:1], D, s3[:, 1:2], 's3', 's31')
                op('dve', lambda: V.scalar_tensor_tensor(out=h2n[:], in0=x1[:], scalar=s3[:, 1:2], in1=SCALE_F, op0=ALU.mult, op1=ALU.mult),
                   r=('x1', 's31', 'mod'), w=('h2n',))
                op('pool', lambda: G.tensor_tensor(out=h2[:], in0=h2n[:], in1=SHIFT_F, op=ALU.add), r=('h2n', 'mod'), w=('h2',))
                op('act', lambda: A.activation(out=h2b[:], in_=h2[:], func=AF.Copy), r=('h2',), w=('h2b',))
                op('sp', lambda: SP.dma_start(out=H2_s[tsl, :], in_=h2b[:]), r=('h2b',), w=('H2_s',), dma='d_h2s')
                for k in range(8):
                    op('pe', lambda k=k: T.transpose(out=pTf[:, k * 128:(k + 1) * 128], in_=h2[:, k * 128:(k + 1) * 128], identity=identf[:]),
                       r=('h2', 'identf'), w=('pTf',))
                op('act', lambda: A.activation(out=h2T[:], in_=pTf[:].rearrange("p (k t) -> p k t", k=8), func=AF.Copy), r=('pTf',), w=('h2T',))
                op('dve', lambda: V.tensor_copy(out=h2Tb[:], in_=h2T[:]), r=('h2T',), w=('h2Tb',))
                for k in range(8):
                    op('pe', lambda k=k: T.matmul(pr[:, 0:NE], lhsT=h2T[:, k, :], rhs=wrt[:, k, :], start=(k == 0), stop=(k == 7)),
                       r=('h2T', 'wrt'), w=('pr',))
                op('act', lambda: A.activation(out=sg[:], in_=pr[:, 0:NE], func=AF.Sigmoid), r=('pr',), w=('sg',))
                op('dve', lambda: V.tensor_tensor(out=selv[:], in0=sg[:], in1=rb[:], op=ALU.add), r=('sg', 'rb'), w=('selv',))
                for g in range(8):
                    op('dve', lambda g=g: V.max(out=m8[:, g, :], in_=selv[:, g * 32:(g + 1) * 32]), r=('selv',), w=('m8',))
                op('dve', lambda: V.tensor_tensor(out=gs[:], in0=m8[:, :, 0], in1=m8[:, :, 1], op=ALU.add), r=('m8',), w=('gs',))
                op('dve', lambda: V.max(out=gm8[:], in_=gs[:]), r=('gs',), w=('gm8',))
                op('dve', lambda: V.tensor_scalar(out=gmask[:], in0=gs[:], scalar1=gm8[:, 3:4], scalar2=None, op0=ALU.is_ge),
                   r=('gs', 'gm8'), w=('gmask',))
                op('dve', lambda: V.tensor_scalar(out=gneg[:], in0=gmask[:], scalar1=1.0, scalar2=1e30, op0=ALU.subtract, op1=ALU.mult),
                   r=('gmask',), w=('gneg',))
                op('dve', lambda: V.tensor_tensor(out=msk[:].rearrange("p (g e) -> p g e", g=8), in0=selv[:].rearrange("p (g e) -> p g e", g=8),
                                                  in1=gmask[:].rearrange("p (g o) -> p g o", o=1).to_broadcast([128, 8, 32]), op=ALU.mult),
                   r=('selv', 'gmask'), w=('msk',))
                op('dve', lambda: V.tensor_tensor(out=msk[:].rearrange("p (g e) -> p g e", g=8), in0=msk[:].rearrange("p (g e) -> p g e", g=8),
                                                  in1=gneg[:].rearrange("p (g o) -> p g o", o=1).to_broadcast([128, 8, 32]), op=ALU.add),
                   r=('msk', 'gneg'), w=('msk',))
                op('dve', lambda: V.max(out=t8[:], in_=msk[:]), r=('msk',), w=('t8',))
                op('dve', lambda: V.tensor_scalar(out=mask8[:], in0=msk[:], scalar1=t8[:, 7:8], scalar2=None, op0=ALU.is_ge),
                   r=('msk', 't8'), w=('mask8',))
                op('pool', lambda: G.tensor_copy(out=mask8b[:], in_=mask8[:]), r=('mask8',), w=('mask8b',))
                op('dve', lambda: V.tensor_tensor(out=GD[:, p, :], in0=sg[:], in1=mask8[:], op=ALU.mult), r=('sg', 'mask8'), w=('GD',))
                op('dve', lambda: V.tensor_reduce(out=s3[:, 2:3], in_=GD[:, p, :], axis=AX.X, op=ALU.add), r=('GD',), w=('s32',))
                op('dve', lambda: V.reciprocal(out=s3[:, 3:4], in_=s3[:, 2:3]), r=('s32',), w=('s33',))
                op('dve', lambda: V.tensor_scalar(out=s3[:, 3:4], in0=s3[:, 3:4], scalar1=2.5, scalar2=None, op0=ALU.mult),
                   r=('s33',), w=('s33',))
                op('dve', lambda: V.tensor_scalar(out=GD[:, p, :], in0=GD[:, p, :], scalar1=s3[:, 3:4], scalar2=None, op0=ALU.mult),
                   r=('GD', 's33'), w=('GD',))
                op('pe', lambda: T.matmul(pcn[:, 0:NE], lhsT=tristrict[:], rhs=mask8b[:], start=True, stop=True),
                   r=('tristrict', 'mask8b'), w=('pcn',))
                op('pe', lambda: T.matmul(pcn[:, NE:2 * NE], lhsT=onesb[:], rhs=mask8b[:], start=True, stop=True),
                   r=('onesb', 'mask8b'), w=('pcn',))
                op('dve', lambda: V.tensor_tensor(out=RANK[:, p, :], in0=pcn[:, 0:NE], in1=cntbc[:], op=ALU.add), r=('pcn', 'cntbc'), w=('RANK',))
                op('dve', lambda: V.tensor_tensor(out=cntbc[:], in0=pcn[:, NE:2 * NE], in1=cntbc[:], op=ALU.add), r=('pcn', 'cntbc'), w=('cntbc',))
                for wi, wb_ in enumerate((ws1b, ws3b)):
                    for fc in range(2):
                        c0 = (wi * 2 + fc) * 128
                        for k in range(8):
                            op('pe', lambda k=k, fc=fc, wb_=wb_, c0=c0: T.matmul(pg[:, c0:c0 + 128], lhsT=wb_[:, k, fc * 128:(fc + 1) * 128],
                                                                                 rhs=h2Tb[:, k, :], start=(k == 0), stop=(k == 7)),
                               r=('h2Tb', 'ws1b', 'ws3b'), w=('pg',))
                op('act', lambda: A.activation(out=sil[:], in_=pg[:, 0:256], func=AF.Silu), r=('pg',), w=('sil',))
                op('dve', lambda: V.tensor_tensor(out=actT[:].rearrange("p a t -> p (a t)"), in0=sil[:], in1=pg[:, 256:512], op=ALU.mult),
                   r=('sil', 'pg'), w=('actT',))
                for nh in range(2):
                    for fc in range(2):
                        op('pe', lambda nh=nh, fc=fc: T.matmul(pbig[:, nh * 512:(nh + 1) * 512], lhsT=actT[:, fc, :],
                                                               rhs=ws2b[:, fc, nh * 512:(nh + 1) * 512], start=(fc == 0), stop=(fc == 1)),
                           r=('actT', 'ws2b'), w=('pbig',))
                op('dve', lambda: V.tensor_tensor(out=accb[:], in0=pbig[:], in1=GATE_F, op=ALU.mult), r=('pbig', 'mod'), w=('accb',))
                op('pool', lambda: G.tensor_tensor(out=accb[:], in0=accb[:], in1=x1[:], op=ALU.add), r=('accb', 'x1'), w=('accb',))
                op('sp', lambda: SP.dma_start(out=ACC_s[tsl, :], in_=accb[:]), r=('accb',), w=('ACC_s',), dma='d_accs')
            if dbg:
                op('sp', lambda: SP.dma_start(out=dbgs['acc'], in_=ACC_s), r=('ACC_s',), w=('dbgacc',), dma='d_dbg')
                op('sp', lambda: SP.dma_start(out=dbgs['h2'], in_=H2_s), r=('H2_s',), w=('dbgh2',), dma='d_dbg')
                op('sp', lambda: SP.dma_start(out=dbgs['gd'], in_=GD[:]), r=('GD',), w=('dbggd',), dma='d_dbg')
            P.barrier()

        if STOP < 4:
            p34.close()
            return
        with ExitStack() as p4:
            cntT = sb("cntT", [128, 2], st=p4)
            cmp_ = sb("cmp", [128, 32], st=p4)
            nblk = sb("nblk", [128, 2], st=p4)
            nbb = sb("nbb", [128, 2, 128], st=p4)
            trihi = sb("trihi", [128, 2, NE], st=p4)
            trilo = sb("trilo", [128, 2, NE], st=p4)
            pstart = sb("pstart", [128, NE], st=p4)
            pendc = sb("pendc", [128, 2], st=p4)
            Am = sb("Am", [128, 2, 512], st=p4)
            bef = sb("bef", [128, 512], st=p4)
            key = sb("key", [128, NE], st=p4); k8 = sb("k8", [128, 8], st=p4); oh = sb("oh", [128, NE], st=p4)
            d8 = sb("d8", [128, 8], st=p4)
            pq = ps("pq", [128, 512], st=p4)
            pq2 = ps("pq2", [128, 512], st=p4)
            pq3 = ps("pq3", [128, 512], st=p4)
            for c in range(2):
                op('pe', lambda c=c: T.transpose(out=pq[:, c * 128:(c + 1) * 128], in_=cntbc[:, c * 128:(c + 1) * 128], identity=identf[:]),
                   r=('cntbc', 'identf'), w=('pq',))
            op('dve', lambda: V.tensor_copy(out=cntT[:], in_=pq[:, 0:256].rearrange("p (c t) -> p c t", c=2)[:, :, 0]), r=('pq',), w=('cntT',))
            for c in range(2):
                op('dve', lambda c=c: V.tensor_scalar(out=cmp_[:], in0=thr[:], scalar1=cntT[:, c:c + 1], scalar2=None, op0=ALU.is_lt),
                   r=('thr', 'cntT'), w=('cmp',))
                op('dve', lambda c=c: V.tensor_reduce(out=nblk[:, c:c + 1], in_=cmp_[:], axis=AX.X, op=ALU.add), r=('cmp',), w=('nblk',))
                op('dve', lambda c=c: V.tensor_copy(out=nbb[:, c, :], in_=nblk[:, c:c + 1].to_broadcast([128, 128])), r=('nblk',), w=('nbb',))
            op('dve', lambda: V.memset(trihi[:], 0.0), w=('trihi',))
            op('dve', lambda: V.tensor_copy(out=trihi[:, 0, 0:128], in_=tri[:]), r=('tri',), w=('trihi',))
            op('dve', lambda: V.memset(trihi[:, 0, 128:256], 1.0), w=('trihi',))
            op('dve', lambda: V.tensor_copy(out=trihi[:, 1, 128:256], in_=tri[:]), r=('tri',), w=('trihi',))
            op('dve', lambda: V.tensor_copy(out=trilo[:], in_=trihi[:]), r=('trihi',), w=('trilo',))
            op('dve', lambda: V.tensor_tensor(out=trilo[:, 0, 0:128], in0=trilo[:, 0, 0:128], in1=identf[:], op=ALU.subtract),
               r=('trilo', 'identf'), w=('trilo',))
            op('dve', lambda: V.tensor_tensor(out=trilo[:, 1, 128:256], in0=trilo[:, 1, 128:256], in1=identf[:], op=ALU.subtract),
               r=('trilo', 'identf'), w=('trilo',))
            for c in range(2):
                op('pe', lambda c=c: T.matmul(pq2[:, 0:NE], lhsT=nbb[:, c, :], rhs=trihi[:, c, :], start=(c == 0), stop=(c == 1)),
                   r=('nbb', 'trihi'), w=('pq2',))
            for c in range(2):
                op('pe', lambda c=c: T.matmul(pq2[:, NE:2 * NE], lhsT=nbb[:, c, :], rhs=trilo[:, c, :], start=(c == 0), stop=(c == 1)),
                   r=('nbb', 'trilo'), w=('pq2',))
            op('dve', lambda: V.tensor_scalar(out=pstart[:], in0=pq2[:, NE:2 * NE], scalar1=128.0, scalar2=1.0, op0=ALU.mult, op1=ALU.add),
               r=('pq2',), w=('pstart',))
            op('dve', lambda: V.tensor_copy(out=bef[:, 0:NE], in_=pq2[:, 0:NE]), r=('pq2',), w=('bef',))
            for c in range(2):
                op('pe', lambda c=c: T.transpose(out=pq[:, 256 + c * 128:256 + (c + 1) * 128], in_=bef[:, c * 128:(c + 1) * 128], identity=identf[:]),
                   r=('bef', 'identf'), w=('pq',))
            op('dve', lambda: V.tensor_copy(out=pendc[:], in_=pq[:, 256:512].rearrange("p (c t) -> p c t", c=2)[:, :, 0]), r=('pq',), w=('pendc',))
            for c in range(2):
                op('dve', lambda c=c: V.tensor_scalar(out=Am[:, c, 0:NB], in0=iota[:, 0:NB], scalar1=pendc[:, c:c + 1], scalar2=None, op0=ALU.is_ge),
                   r=('iota', 'pendc'), w=('Am',))
            for c in range(2):
                op('pe', lambda c=c: T.matmul(pq3[:, 0:NB], lhsT=onesf[:], rhs=Am[:, c, 0:NB], start=(c == 0), stop=(c == 1)),
                   r=('onesf', 'Am'), w=('pq3',))
            op('dve', lambda: V.tensor_scalar(out=bef[:, 0:NB], in0=pq3[:, 0:NB], scalar1=255.0, scalar2=128.0, op0=ALU.min, op1=ALU.mult),
               r=('pq3',), w=('bef',))
            op('dve', lambda: V.tensor_scalar(out=bef[:, 0:NB], in0=bef[:, 0:NB], scalar1=pidx[:, 0:1], scalar2=None, op0=ALU.add),
               r=('bef', 'pidx'), w=('bef',))
            op('dve', lambda: V.tensor_copy(out=WIDX[:], in_=bef[:, 0:NB]), r=('bef',), w=('WIDX',))
            for p in range(NP):
                op('dve', lambda p=p: V.tensor_tensor(out=key[:], in0=RANK[:, p, :], in1=pstart[:], op=ALU.add), r=('RANK', 'pstart'), w=('key',))
                op('dve', lambda p=p: V.scalar_tensor_tensor(out=oh[:], in0=GD[:, p, :], scalar=0.0, in1=key[:], op0=ALU.is_gt, op1=ALU.mult),
                   r=('GD', 'key'), w=('oh',))
                op('dve', lambda: V.max(out=k8[:], in_=oh[:]), r=('oh',), w=('k8',))
                op('dve', lambda: V.tensor_scalar(out=d8[:], in0=k8[:], scalar1=-1.0, scalar2=None, op0=ALU.add), r=('k8',), w=('d8',))
                op('dve', lambda p=p: V.tensor_copy(out=DESTI[:, p, :], in_=d8[:]), r=('d8',), w=('DESTI',))
                for k in range(8):
                    op('dve', lambda p=p, k=k: V.scalar_tensor_tensor(out=key[:], in0=oh[:], scalar=k8[:, k:k + 1], in1=GD[:, p, :],
                                                                      op0=ALU.is_equal, op1=ALU.mult), r=('oh', 'k8', 'GD'), w=('key',))
                    op('dve', lambda p=p, k=k: V.tensor_reduce(out=GATE8[:, p, k:k + 1], in_=key[:], axis=AX.X, op=ALU.add),
                       r=('key',), w=('GATE8',))
            P.barrier()
        p34.close()

        if STOP < 5:
            return
        with ExitStack() as p5:
            hrow = [sb("hrow%d" % i, [128, D], BF16, st=p5) for i in range(2)]
            for p in range(NP):
                b = p % 2
                load(hrow[b][:], H2_s[p * 128:(p + 1) * 128, :], 'hrow%d' % b, 'd_hrow%d' % b)
                for k in range(8):
                    op('pool', lambda p=p, k=k, b=b: G.indirect_dma_start(
                        out=XS_s, out_offset=bass.IndirectOffsetOnAxis(DESTI[:, p, k:k + 1], 0), in_=hrow[b][:], in_offset=None),
                       r=('hrow%d' % b, 'DESTI'), w=(), dma='d_scat%d' % b)
            P.barrier()

        if STOP < 6:
            return
        with ExitStack() as p6:
            xs = [sb("xs%d" % i, [128, D], BF16, st=p6) for i in range(3)]
            xT = sb("xT", [128, 8, 128], BF16, st=p6)
            wfa = [sb("wfa_%d" % i, [128, 6144], st=p6) for i in range(3)]
            wf = [[wfa[i][:, j * 2048:(j + 1) * 2048] for i in range(3)] for j in range(3)]
            wb = [[sb("wb%d_%d" % (j, i), [128, 2048], BF16, st=p6) for i in range(2)] for j in range(3)]
            sil6 = sb("sil6", [128, 256], st=p6); act6 = sb("act6", [128, 2, 128], BF16, st=p6)
            ysb = [sb("ysb%d" % i, [128, D], st=p6) for i in range(2)]
            pX = ps("pX6", [128, 1024], BF16, st=p6)
            pG = [ps("pG6_%d" % i, [128, 512], st=p6) for i in range(2)]
            pY = [ps("pY6_%d" % i, [128, 1024], st=p6) for i in range(2)]
            cast_eng = ('dve', 'act', 'act')

            def fetch(i):
                b = i % 3
                load(xs[b][:], XS_s[i * 128:(i + 1) * 128, :], 'xs%d' % b, 'd_xs%d' % b)
                op('pool', lambda b=b, i=i: G.indirect_dma_start(
                    out=wfa[b][:], out_offset=None, in_=L['wexp'], in_offset=bass.IndirectOffsetOnAxis(WIDX[:, i:i + 1], 0)),
                   r=('WIDX',), w=('wf0_%d' % b, 'wf1_%d' % b, 'wf2_%d' % b), dma='d_wf_%d' % b)
            fetch(0)
            fetch(1)
            for i in range(NB):
                b = i % 2
                bf = i % 3
                if i + 2 < NB:
                    fetch(i + 2)
                for j in range(3):
                    e_ = cast_eng[j]
                    if e_ == 'act':
                        op('act', lambda j=j, b=b: A.activation(out=wb[j][b][:], in_=wf[j][bf], func=AF.Copy),
                           r=('wf%d_%d' % (j, bf),), w=('wb%d_%d' % (j, b),))
                    else:
                        op(e_, lambda j=j, b=b, e_=e_: P.E[e_].tensor_copy(out=wb[j][b][:], in_=wf[j][bf]),
                           r=('wf%d_%d' % (j, bf),), w=('wb%d_%d' % (j, b),))
                xv = xs[bf][:].rearrange("p (q c) -> p c q", c=8)
                for c in range(8):
                    op('pe', lambda c=c: T.transpose(out=pX[:, c * 128:(c + 1) * 128], in_=xv[:, c, :], identity=identb[:]),
                       r=('xs%d' % bf, 'identb'), w=('pX6',))
                op('dve', lambda: V.tensor_copy(out=xT[:], in_=pX[:].rearrange("p (c t) -> p c t", c=8)), r=('pX6',), w=('xT',))
                pg_ = pG[b]; pgk = 'pG6_%d' % b
                for wi in range(2):
                    wv_ = wb[wi][b][:].rearrange("p (c f) -> p c f", c=8)
                    for fc in range(2):
                        c0 = (wi * 2 + fc) * 128
                        for c in range(8):
                            op('pe', lambda c=c, fc=fc, wv_=wv_, c0=c0: T.matmul(
                                pg_[:, c0:c0 + 128], lhsT=wv_[:, c, :].rearrange("p (m two) -> p two m", two=2)[:, fc, :],
                                rhs=xT[:, c, :], start=(c == 0), stop=(c == 7)),
                               r=('xT', 'wb%d_%d' % (wi, b)), w=(pgk,))
                op('act', lambda: A.activation(out=sil6[:], in_=pg_[:, 0:256], func=AF.Silu), r=(pgk,), w=('sil6',))
                op('dve', lambda: V.tensor_tensor(out=act6[:].rearrange("p a t -> p (a t)"), in0=sil6[:], in1=pg_[:, 256:512], op=ALU.mult),
                   r=('sil6', pgk), w=('act6',))
                w2v = wb[2][b][:].rearrange("p (c n) -> p c n", c=2)
                py_ = pY[b]; pyk = 'pY6_%d' % b
                for nh in range(2):
                    for fc in range(2):
                        op('pe', lambda nh=nh, fc=fc: T.matmul(py_[:, nh * 512:(nh + 1) * 512], lhsT=act6[:, fc, :],
                                                               rhs=w2v[:, fc, nh * 512:(nh + 1) * 512], start=(fc == 0), stop=(fc == 1)),
                           r=('act6', 'wb2_%d' % b), w=(pyk,))
                op('act', lambda: A.activation(out=ysb[b][:, 0:512], in_=py_[:, 0:512], func=AF.Copy), r=(pyk,), w=('ysb%d' % b,))
                op('dve', lambda: V.tensor_copy(out=ysb[b][:, 512:1024], in_=py_[:, 512:1024]), r=(pyk,), w=('ysb%d' % b,))
                op('sp', lambda i=i: SP.dma_start(out=YS_s[i * 128:(i + 1) * 128, :], in_=ysb[b][:]), r=('ysb%d' % b,), w=('YS_s',), dma='d_ys%d' % b)
            P.barrier()

        if STOP < 7:
            return
        with ExitStack() as p7:
            yg = [sb("yg%d" % i, [128, D], st=p7) for i in range(3)]
            acc = sb("acc7", [128, D], st=p7)
            base = [sb("base%d" % i, [128, D], st=p7) for i in range(2)]
            ob = [sb("ob%d" % i, [128, D], st=p7) for i in range(2)]
            ng = 0
            for p in range(NP):
                b = p % 2
                tsl = slice(p * 128, (p + 1) * 128)
                load(base[b][:], ACC_s[tsl, :], 'base%d' % b, 'd_base%d' % b)
                for k in range(8):
                    gi = ng % 3; ng += 1
                    op('pool', lambda p=p, k=k, gi=gi: G.indirect_dma_start(
                        out=yg[gi][:], out_offset=None, in_=YS_s, in_offset=bass.IndirectOffsetOnAxis(DESTI[:, p, k:k + 1], 0)),
                       r=('DESTI', 'YS_s'), w=('yg%d' % gi,), dma='d_yg%d' % gi)
                    if k == 0:
                        op('dve', lambda p=p, k=k, gi=gi: V.tensor_scalar(out=acc[:], in0=yg[gi][:], scalar1=GATE8[:, p, k:k + 1], scalar2=None,
                                                                          op0=ALU.mult), r=('yg%d' % gi, 'GATE8'), w=('acc7',))
                    else:
                        op('dve', lambda p=p, k=k, gi=gi: V.scalar_tensor_tensor(out=acc[:], in0=yg[gi][:], scalar=GATE8[:, p, k:k + 1], in1=acc[:],
                                                                                 op0=ALU.mult, op1=ALU.add), r=('yg%d' % gi, 'GATE8', 'acc7'), w=('acc7',))
                op('dve', lambda: V.tensor_tensor(out=ob[b][:], in0=acc[:], in1=GATE_F, op=ALU.mult), r=('acc7', 'mod'), w=('ob%d' % b,))
                op('pool', lambda: G.tensor_tensor(out=ob[b][:], in0=ob[b][:], in1=base[b][:], op=ALU.add), r=('ob%d' % b, 'base%d' % b), w=('ob%d' % b,))
                op('sp', lambda: SP.dma_start(out=out[tsl, :], in_=ob[b][:]), r=('ob%d' % b,), w=('out',), dma='d_out%d' % b)
            P.barrier()


def _consts(S, half):
    NBLK = S // 128
    TOWN = S // 2
    NB = TOWN * 8 // 128 + 256
    bf = ml_dtypes.bfloat16
    c = {}
    c['c_identb'] = np.eye(128, dtype=np.float32).astype(bf)
    c['c_identf'] = np.eye(128, dtype=np.float32)
    u = np.arange(128)
    c['c_tri'] = (u[:, None] <= u[None, :]).astype(np.float32)
    c['c_tristrict'] = (u[:, None] < u[None, :]).astype(np.float32).astype(bf)
    c['c_maskml'] = ((u[:, None] <= u[None, :]).astype(np.float32) * (128.0 ** -0.5)).astype(np.float32)
    causal = (u[:, None] <= u[None, :]).astype(np.float32)
    mk = np.zeros((128, 2, 128), np.float32)
    if half == 0:
        mk[:, 1, :] = causal; mk[:, 0, :] = 0.0
    else:
        mk[:, 1, :] = 1.0; mk[:, 0, :] = causal
    c['c_masku'] = mk.astype(bf)
    slopes = 2.0 ** (-8.0 * np.arange(1, 5) / 4.0)
    uu = np.arange(NBLK + 1)
    delta = (uu - 1) if half == 0 else uu
    tab = slopes[None, :, None] * u[:, None, None] - 128.0 * slopes[None, :, None] * delta[None, None, :]
    c['c_alibi'] = tab.astype(np.float32)
    c['c_alibi2'] = (tab + 256.0 * slopes[None, :, None]).astype(np.float32)
    s = np.zeros((128, 2), np.float32); s[:, half] = 1.0
    c['c_sel'] = s
    c['c_pidx'] = u.astype(np.float32)[:, None].copy()
    c['c_iota'] = np.tile(np.arange(512, dtype=np.float32)[None, :], (128, 1))
    c['c_thr'] = np.tile((128.0 * np.arange(32, dtype=np.float32))[None, :], (128, 1))
    return c


def _rep(v, n=128):
    return np.ascontiguousarray(np.broadcast_to(np.asarray(v, np.float32).reshape(1, -1), (n, np.asarray(v).size)))


def make_in_maps(inp, S, B):
    f = np.float32
    maps = []
    shared = {}
    shared['w_ada'] = np.ascontiguousarray(inp['w_ada'][0], f)
    shared['b_ada'] = _rep(inp['b_ada'][0])
    shared['w_in'] = np.ascontiguousarray(inp['w_in'][0], f)
    cw = np.asarray(inp['conv_w'][0], f)
    shared['convw'] = np.ascontiguousarray(cw.reshape(4, 8, 128).transpose(2, 1, 0))
    shared['convb'] = np.ascontiguousarray(np.asarray(inp['conv_b'][0], f).reshape(8, 128).T)
    shared['gate_b'] = _rep(inp['gate_b'][0])
    shared['mlg'] = _rep(np.tile(np.asarray(inp['ml_norm_g'][0], f), 4))
    shared['qg'] = _rep(inp['da_q_norm_g'][0])
    shared['kg'] = _rep(inp['da_k_norm_g'][0])
    lv = np.stack([inp['lambda_q1'][0], inp['lambda_k1'][0], inp['lambda_q2'][0], inp['lambda_k2'][0]]).astype(f)
    shared['lamv'] = np.ascontiguousarray(np.broadcast_to(lv[None], (128, 4, 64)))
    shared['dag'] = _rep(inp['da_norm_g'][0])
    shared['w_out'] = np.ascontiguousarray(inp['w_out'][0], f)
    shared['w_router'] = np.ascontiguousarray(inp['w_router'][0], f)
    shared['rbias'] = _rep(inp['router_bias'][0])
    shared['wexp'] = np.concatenate([np.asarray(inp['w1'][0], f).reshape(NE * 128, 2048),
                                     np.asarray(inp['w3'][0], f).reshape(NE * 128, 2048),
                                     np.asarray(inp['w2'][0], f).reshape(NE * 128, 2048)], axis=1)
    shared['ws1'] = np.ascontiguousarray(inp['ws1'][0], f)
    shared['ws3'] = np.ascontiguousarray(inp['ws3'][0], f)
    shared['ws2'] = np.ascontiguousarray(inp['ws2'][0], f)
    consts = [_consts(S, 0), _consts(S, 1)]
    xall = np.asarray(inp['x'], f)
    call = np.asarray(inp['c'], f)
    for b in range(B):
        for half in range(2):
            m = dict(shared)
            m.update(consts[half])
            m['x'] = np.ascontiguousarray(xall[b])
            m['xo'] = np.ascontiguousarray(xall[b].reshape(S // 256, 2, 128, D)[:, half].reshape(S // 2, D))
            m['ccol'] = np.ascontiguousarray(call[b].reshape(8, 128).T)
            maps.append(m)
    return maps


_NC_CACHE = {}


def run(inp, S, B, dbg=False):
    key = (S, dbg)
    if key not in _NC_CACHE:
        _NC_CACHE[key] = build_nc(S, dbg)
    nc = _NC_CACHE[key]
    maps = make_in_maps(inp, S, B)
    res = run_bass_kernel_spmd(nc, maps, core_ids=list(range(2 * B)))
    outf = np.zeros((B, S, D), np.float32)
    for b in range(B):
        for half in range(2):
            r = res.results[b * 2 + half]["out"]
            outf[b].reshape(S // 256, 2, 128, D)[:, half] = r.reshape(S // 256, 128, D)
    return outf, res


def kernel(**inputs):
    x = np.asarray(inputs['x'])
    B, S, _ = x.shape
    outf, _ = run(inputs, S, B, dbg=False)
    return outf
```
